# Optimizing a Trainium2 kernel written in Bass

```python
import jax, jax.numpy as jnp
from jax import lax
import numpy as np


D_MODEL = 1024
BATCH = 16
SEQ = 2048
DEPTH = 2

GRID_W = 64
CTX_LEN = 256
D_MIX = D_MODEL
EPS = 1e-6

A_GROUPS = 4
A_WIDTH = D_MIX // 2
A_GROUP_DIM = A_WIDTH // A_GROUPS
CHUNK_A = 128
ROWS_PER_CHUNK = CHUNK_A // GRID_W

B_HEADS = 4
B_WIDTH = D_MIX - A_WIDTH
B_DV = B_WIDTH // B_HEADS
B_DK = B_DV // 2
B_KEY_WIDTH = B_HEADS * B_DK
GATE_RANK = 16
GATE_TAU = 16.0
CHUNK_B = 64

N_GROUPS = 8
EXPERTS_PER_GROUP = 8
N_EXPERTS = N_GROUPS * EXPERTS_PER_GROUP
TOP_K = 2
D_EXPERT = D_MODEL // 2
MOE_BLOCK = 128

OFF_AU = 0
OFF_AV = OFF_AU + A_WIDTH
OFF_BQ = OFF_AV + A_WIDTH
OFF_BR = OFF_BQ + B_KEY_WIDTH
OFF_BK = OFF_BR + B_WIDTH
OFF_BV = OFF_BK + B_KEY_WIDTH
OFF_BG = OFF_BV + B_WIDTH
D_IN = OFF_BG + 2 * GATE_RANK

kernel_name = 'hymba_gmlp_gla_hmoe_dit'


def rmsnorm(x, g):
    xf = x.astype(jnp.float32)
    y = xf * lax.rsqrt(jnp.mean(xf * xf, axis=-1, keepdims=True) + EPS)
    return (y * g.astype(jnp.float32)).astype(x.dtype)


def modulate(h, shift, scale):
    return h * (1 + scale) + shift


def spatial_gating(zu, zv, n_chunks, ln_g, ln_b, w_sp, b_sp):
    Bn, T, _ = zu.shape
    u = jax.nn.gelu(zu)
    v = jax.nn.gelu(zv).reshape(Bn, n_chunks, CHUNK_A, A_GROUPS, A_GROUP_DIM).astype(jnp.float32)
    mu = jnp.mean(v, axis=-1, keepdims=True)
    var = jnp.mean(jnp.square(v - mu), axis=-1, keepdims=True)
    v = ((v - mu) * lax.rsqrt(var + EPS)).astype(zv.dtype)
    v = v * ln_g.reshape(A_GROUPS, A_GROUP_DIM) + ln_b.reshape(A_GROUPS, A_GROUP_DIM)
    s = jnp.einsum('gpq,bnqgc->bnpgc', w_sp, v) + b_sp.T[:, :, None]
    return u * s.reshape(Bn, T, A_WIDTH)


def heads(z, d):
    Bn, T, W = z.shape
    return z.reshape(Bn, T, W // d, d).transpose(0, 2, 1, 3).astype(jnp.float32)


def flip(t):
    return jnp.flip(t, axis=2)


def gla_logdecay(zg, w_up, b_up):
    Bn, T, _ = zg.shape
    logit = (zg @ w_up + b_up).astype(jnp.float32)
    la = jax.nn.log_sigmoid(logit) / GATE_TAU
    return la.reshape(Bn, T, B_HEADS, B_DK).transpose(0, 2, 1, 3)


def gla_inputs(zkvg, w_gate_up, b_gate):
    Bn, T, _ = zkvg.shape
    k = heads(zkvg[..., :B_KEY_WIDTH], B_DK)
    v = heads(zkvg[..., B_KEY_WIDTH:B_KEY_WIDTH + B_WIDTH], B_DV)
    zg = zkvg[..., B_KEY_WIDTH + B_WIDTH:].reshape(Bn, T, 2, GATE_RANK)
    la_f = gla_logdecay(zg[:, :, 0], w_gate_up[0], b_gate[0])
    la_b = gla_logdecay(zg[:, :, 1], w_gate_up[1], b_gate[1])
    return k, v, la_f, la_b


def to_chunks(t):
    Bn, H, T, d = t.shape
    return t.reshape(Bn, H, T // CHUNK_B, CHUNK_B, d)


def gla_states(k, v, la, s0):
    kc, vc, lc = to_chunks(k), to_chunks(v), to_chunks(la)
    b = jnp.cumsum(lc, axis=3)
    b_last = b[..., -1:, :]
    kv = jnp.einsum('bhncd,bhnce->bhnde', kc * jnp.exp(b_last - b), vc)
    decay = jnp.exp(b_last[..., 0, :])

    def step(s, inp):
        dec, kvn = inp
        return dec[..., None] * s + kvn, s

    s_final, s_prev = lax.scan(step, s0, (jnp.moveaxis(decay, 2, 0), jnp.moveaxis(kv, 2, 0)))
    return jnp.moveaxis(s_prev, 0, 2), s_final, b


def gla_direction(q, k, v, la, s0):
    s_prev, s_final, b = gla_states(k, v, la, s0)
    qc, kc, vc = to_chunks(q), to_chunks(k), to_chunks(v)
    qe = qc * jnp.exp(b)
    ke = kc * jnp.exp(-b)
    att = jnp.einsum('bhnid,bhnjd->bhnij', qe, ke)
    mask = jnp.tril(jnp.ones((CHUNK_B, CHUNK_B), dtype=bool))
    att = jnp.where(mask, att, 0.0)
    o = jnp.einsum('bhnij,bhnje->bhnie', att, vc) + jnp.einsum('bhnid,bhnde->bhnie', qe, s_prev)
    Bn, H, T, _ = q.shape
    return o.reshape(Bn, H, T, B_DV), s_final


def gla_bidir(q, k, v, la_f, la_b, s0_f, s0_b):
    o_f, s_f = gla_direction(q, k, v, la_f, s0_f)
    o_b, s_b = gla_direction(flip(q), flip(k), flip(v), flip(la_b), s0_b)
    return o_f + flip(o_b), s_f, s_b


def gla_merge(o, r, g_gla):
    o = o.transpose(0, 2, 1, 3)
    o = o * lax.rsqrt(jnp.mean(o * o, axis=-1, keepdims=True) + EPS) * g_gla.reshape(B_HEADS, B_DV).astype(jnp.float32)
    Bn, T = o.shape[0], o.shape[1]
    return o.reshape(Bn, T, B_WIDTH).astype(r.dtype) * jax.nn.silu(r)


def hier_moe(t, wrg, brg, wre, bre, w1, w3, w2):
    T, D = t.shape
    pg = jax.nn.softmax((t @ wrg + brg).astype(jnp.float32), axis=-1)
    p_top, g_idx = lax.top_k(pg, 1)
    le = (t @ wre + bre).astype(jnp.float32).reshape(T, N_GROUPS, EXPERTS_PER_GROUP)
    le_sel = jnp.take_along_axis(le, g_idx[:, :, None], axis=1)[:, 0]
    l_top, e_local = lax.top_k(le_sel, TOP_K)
    gate = p_top * jax.nn.softmax(l_top, axis=-1)
    expert = g_idx * EXPERTS_PER_GROUP + e_local
    M = T * TOP_K
    e_flat = expert.reshape(-1)
    tok_flat = jnp.repeat(jnp.arange(T, dtype=jnp.int32), TOP_K)
    gate_flat = gate.reshape(-1)
    order = jnp.argsort(e_flat)
    e_sorted = e_flat[order]
    counts = jnp.bincount(e_flat, length=N_EXPERTS)
    starts = jnp.cumsum(counts) - counts
    padded = (counts + MOE_BLOCK - 1) // MOE_BLOCK * MOE_BLOCK
    pad_ends = jnp.cumsum(padded)
    pad_starts = pad_ends - padded
    dest = pad_starts[e_sorted] + jnp.arange(M, dtype=jnp.int32) - starts[e_sorted]
    n_blocks = (M + N_EXPERTS * (MOE_BLOCK - 1) + MOE_BLOCK - 1) // MOE_BLOCK
    P = n_blocks * MOE_BLOCK
    buf_tok = jnp.full((P,), T, dtype=jnp.int32).at[dest].set(tok_flat[order])
    buf_gate = jnp.zeros((P,), jnp.float32).at[dest].set(gate_flat[order])
    block_start = jnp.arange(n_blocks, dtype=jnp.int32) * MOE_BLOCK
    block_expert = jnp.minimum(jnp.searchsorted(pad_ends, block_start, side='right'), N_EXPERTS - 1)
    t_pad = jnp.concatenate([t, jnp.zeros((1, D), t.dtype)], axis=0)
    xb = t_pad[buf_tok].reshape(n_blocks, MOE_BLOCK, D)

    def expert_block(args):
        xblk, e = args
        hblk = jax.nn.silu(xblk @ w1[e]) * (xblk @ w3[e])
        return hblk @ w2[e]

    yb = lax.map(expert_block, (xb, block_expert)).reshape(P, D)
    yb = yb * buf_gate[:, None].astype(yb.dtype)
    return jax.ops.segment_sum(yb, buf_tok, num_segments=T + 1)[:T]


def setup_inputs(seed: int = 0) -> dict:
    key = jax.random.key(seed)
    ks = jax.random.split(key, 32)
    L, D = DEPTH, D_MODEL
    nrm = lambda k, shape, s: jax.random.normal(k, shape, jnp.float32) * s
    return {
        'x': nrm(ks[0], (BATCH, SEQ, D), 1.0),
        'c': nrm(ks[1], (BATCH, D), 1.0),
        'ctx': nrm(ks[2], (BATCH, CTX_LEN, D), 1.0),
        'c_ctx': nrm(ks[3], (D,), 1.0),
        'w_mod': nrm(ks[4], (L, D, 6 * D), 0.5 * D ** -0.5),
        'b_mod': nrm(ks[5], (L, 6 * D), 0.01),
        'g_norm1': 1.0 + nrm(ks[6], (L, D), 0.01),
        'w_in': nrm(ks[7], (L, D, D_IN), D ** -0.5),
        'ln_v_g': 1.0 + nrm(ks[8], (L, A_WIDTH), 0.01),
        'ln_v_b': nrm(ks[9], (L, A_WIDTH), 0.01),
        'w_sp': nrm(ks[10], (L, A_GROUPS, CHUNK_A, CHUNK_A), CHUNK_A ** -0.5),
        'b_sp': 1.0 + nrm(ks[11], (L, A_GROUPS, CHUNK_A), 0.01),
        'w_gate_up': nrm(ks[12], (L, 2, GATE_RANK, B_KEY_WIDTH), GATE_RANK ** -0.5),
        'b_gate': nrm(ks[13], (L, 2, B_KEY_WIDTH), 0.01),
        'g_gla': 1.0 + nrm(ks[14], (L, B_WIDTH), 0.01),
        'w_out': nrm(ks[15], (L, D_MIX, D), D_MIX ** -0.5),
        'g_norm2': 1.0 + nrm(ks[16], (L, D), 0.01),
        'w_router_g': nrm(ks[17], (L, D, N_GROUPS), D ** -0.5),
        'b_router_g': nrm(ks[18], (L, N_GROUPS), 0.01),
        'w_router_e': nrm(ks[19], (L, D, N_EXPERTS), D ** -0.5),
        'b_router_e': nrm(ks[20], (L, N_EXPERTS), 0.01),
        'w1': nrm(ks[21], (L, N_EXPERTS, D, D_EXPERT), D ** -0.5),
        'w3': nrm(ks[22], (L, N_EXPERTS, D, D_EXPERT), D ** -0.5),
        'w2': nrm(ks[23], (L, N_EXPERTS, D_EXPERT, D), D_EXPERT ** -0.5),
        'g_final': 1.0 + nrm(ks[24], (D,), 0.01),
    }


def reference(x, c, ctx, c_ctx, w_mod, b_mod, g_norm1, w_in, ln_v_g, ln_v_b, w_sp, b_sp,
              w_gate_up, b_gate, g_gla, w_out, g_norm2, w_router_g, b_router_g,
              w_router_e, b_router_e, w1, w3, w2, g_final):
    Bn, S, D = x.shape
    Lc = ctx.shape[1]
    ROWS = S // GRID_W
    n_chunks_lat = ROWS // ROWS_PER_CHUNK
    n_chunks_ctx = Lc // CHUNK_A
    zero_state = jnp.zeros((Bn, B_HEADS, B_DK, B_DV), jnp.float32)
    h = x
    cx = ctx
    for l in range(DEPTH):
        last = l == DEPTH - 1
        m = jax.nn.silu(c) @ w_mod[l] + b_mod[l]
        sh1, sc1, g1, sh2, sc2, g2 = jnp.split(m[:, None, :], 6, axis=-1)
        mc = jax.nn.silu(c_ctx) @ w_mod[l] + b_mod[l]
        csh1, csc1, cg1, csh2, csc2, cg2 = jnp.split(mc, 6, axis=-1)

        hn = modulate(rmsnorm(h, g_norm1[l]), sh1, sc1)
        cn = modulate(rmsnorm(cx, g_norm1[l]), csh1, csc1)
        z = hn @ w_in[l]

        if last:
            zc_kvg = cn @ w_in[l][:, OFF_BK:]
            kc, vc, laf_c, lab_c = gla_inputs(zc_kvg, w_gate_up[l], b_gate[l])
            _, s_ctx_f, _ = gla_states(kc, vc, laf_c, zero_state)
            _, s_ctx_b, _ = gla_states(flip(kc), flip(vc), flip(lab_c), zero_state)
        else:
            zc = cn @ w_in[l]
            kc, vc, laf_c, lab_c = gla_inputs(zc[..., OFF_BK:], w_gate_up[l], b_gate[l])
            qc = heads(zc[..., OFF_BQ:OFF_BR], B_DK) * B_DK ** -0.5
            oc, s_ctx_f, s_ctx_b = gla_bidir(qc, kc, vc, laf_c, lab_c, zero_state, zero_state)
            bc_out = gla_merge(oc, zc[..., OFF_BR:OFF_BK], g_gla[l])
            ac_out = spatial_gating(zc[..., OFF_AU:OFF_AV], zc[..., OFF_AV:OFF_BQ], n_chunks_ctx,
                                    ln_v_g[l], ln_v_b[l], w_sp[l], b_sp[l])
            mix_c = jnp.concatenate([ac_out, bc_out], axis=-1) @ w_out[l]
            cx = cx + cg1 * mix_c

        a_out = spatial_gating(z[..., OFF_AU:OFF_AV], z[..., OFF_AV:OFF_BQ], n_chunks_lat,
                               ln_v_g[l], ln_v_b[l], w_sp[l], b_sp[l])
        q = heads(z[..., OFF_BQ:OFF_BR], B_DK) * B_DK ** -0.5
        k, v, la_f, la_b = gla_inputs(z[..., OFF_BK:], w_gate_up[l], b_gate[l])
        o, _, _ = gla_bidir(q, k, v, la_f, la_b, s_ctx_f, s_ctx_b)
        b_out = gla_merge(o, z[..., OFF_BR:OFF_BK], g_gla[l])
        mix = jnp.concatenate([a_out, b_out], axis=-1) @ w_out[l]
        h = h + g1 * mix

        hn2 = modulate(rmsnorm(h, g_norm2[l]), sh2, sc2).reshape(Bn * S, D)
        moe_args = (w_router_g[l], b_router_g[l], w_router_e[l], b_router_e[l], w1[l], w3[l], w2[l])
        if last:
            y = hier_moe(hn2, *moe_args)
            h = h + g2 * y.reshape(Bn, S, D)
        else:
            cn2 = modulate(rmsnorm(cx, g_norm2[l]), csh2, csc2).reshape(Bn * Lc, D)
            y = hier_moe(jnp.concatenate([hn2, cn2], axis=0), *moe_args)
            h = h + g2 * y[:Bn * S].reshape(Bn, S, D)
            cx = cx + cg2 * y[Bn * S:].reshape(Bn, Lc, D)
    return rmsnorm(h, g_final)
```

```python
import types
import numpy as np
from contextlib import ExitStack
import concourse.bass as bass
import concourse.mybir as mybir
from concourse.bass_utils import run_bass_kernel_spmd

F32 = mybir.dt.float32
BF16 = mybir.dt.bfloat16
I32 = mybir.dt.int32
AF = mybir.ActivationFunctionType
ALU = mybir.AluOpType
AX = mybir.AxisListType

NCORES = 8
L = 2
D = 1024
DIN = 2592
NS = 2
LC = 256
SEQ = 2048
TPS = 18
NT = NS * TPS
T = NT * 128
MB = 256
NBLK = (2 * T) // MB + 64
NSLOT = NBLK * MB
EPS = 1e-6
OFF_AU, OFF_AV, OFF_BQ, OFF_BR, OFF_BK, OFF_BV, OFF_BG = 0, 512, 1024, 1280, 1792, 2048, 2560

SAME_ENGINE_SYNC = True
_DBG = {}


def _freeze(fn):
    if fn.__closure__ is None:
        return fn
    cells = []
    for c in fn.__closure__:
        try:
            cells.append(types.CellType(c.cell_contents))
        except ValueError:
            cells.append(c)
    return types.FunctionType(fn.__code__, fn.__globals__, fn.__name__, fn.__defaults__, tuple(cells))


class Sched:
    ENGS = ['pe', 'act', 'dve', 'pool', 'sp']

    def __init__(self, nc, es, n_dma_sems=(('sp', 16), ('act', 4), ('pool', 16))):
        self.nc = nc
        self.prog = {e: [] for e in self.ENGS}
        self.esem = {e: es.enter_context(nc.semaphore('sem_' + e)) for e in self.ENGS}
        self.ecount = {e: 0 for e in self.ENGS}
        self.seen = {e: {} for e in self.ENGS}
        self.res = {}
        self.dpool, self.dnext, self.dcount, self.semobj = {}, {}, {}, {}
        for e in self.ENGS:
            self.semobj['E' + e] = self.esem[e]
        for e, n in n_dma_sems:
            self.dpool[e] = []
            for i in range(n):
                key = 'D%s%d' % (e, i)
                self.semobj[key] = es.enter_context(nc.semaphore('dsem_%s%d' % (e, i)))
                self.dpool[e].append(key)
                self.dcount[key] = 0
            self.dnext[e] = 0
        self.nops = 0
        self.nwaits = 0

    def _wait(self, eng, tok):
        s, v = tok
        if self.seen[eng].get(s, 0) >= v:
            return
        self.seen[eng][s] = v
        self.prog[eng].append(('wait', s, v))
        self.nwaits += 1

    def op(self, eng, fn, reads=(), writes=(), dma=False):
        if _DBG.get('maxops') and self.nops >= _DBG['maxops']:
            return None
        fn = _freeze(fn)
        writes = list(writes) + [r for r in reads if r.startswith('ps') and r not in writes]
        deps = []
        for r in reads:
            st = self.res.get(r)
            if st is not None and st['w'] is not None:
                deps.append(st['w'])
        for w in writes:
            st = self.res.get(w)
            if st is not None:
                if st['w'] is not None:
                    deps.append(st['w'])
                deps.extend(st['r'])
        if dma:
            pool = self.dpool[eng]
            key = pool[self.dnext[eng] % len(pool)]
            self.dnext[eng] += 1
            cnt = self.dcount[key]
            if cnt > 0:
                deps.append((key, cnt))
            self.dcount[key] = cnt + 16
            tok = (key, cnt + 16)
            inc = 16
        else:
            key = 'E' + eng
            self.ecount[eng] += 1
            tok = (key, self.ecount[eng])
            inc = 1
        own = 'E' + eng
        for d in deps:
            if d[0] == own and (eng == 'pe' or not SAME_ENGINE_SYNC):
                continue
            self._wait(eng, d)
        self.prog[eng].append(('op', fn, key, inc))
        self.nops += 1
        for r in reads:
            st = self.res.setdefault(r, {'w': None, 'r': []})
            st['r'].append(tok)
        for w in writes:
            self.res[w] = {'w': tok, 'r': []}
        return tok

    def barrier(self):
        for e in self.ENGS:
            for key, cnt in self.dcount.items():
                if cnt > 0:
                    self._wait(e, (key, cnt))
            for e2 in self.ENGS:
                if e2 != e and self.ecount[e2] > 0:
                    self._wait(e, ('E' + e2, self.ecount[e2]))
        self.res = {}

    def flush(self):
        if _DBG.get('verbose'):
            print('flush: nops', self.nops, 'nwaits', self.nwaits, flush=True)
        self.barrier()
        nc = self.nc
        with nc.Block() as block:
            def run(e):
                def f(eng):
                    for it in self.prog[e]:
                        if it[0] == 'wait':
                            eng.wait_ge(self.semobj[it[1]], it[2])
                        else:
                            ins = it[1](eng)
                            ins.then_inc(self.semobj[it[2]], it[3])
                return f
            block.tensor(run('pe'))
            block.scalar(run('act'))
            block.vector(run('dve'))
            block.gpsimd(run('pool'))
            block.sync(run('sp'))
        self.prog = {e: [] for e in self.ENGS}


def build_program(dbg=False, stop_after=None):
    nc = bass.Bass("TRN2", target_bir_lowering=False)

    def din(name, shape, dt=F32):
        return nc.dram_tensor(name, list(shape), dt, kind="ExternalInput").ap()

    def dscr(name, shape, dt):
        kind = "ExternalOutput" if dbg else "Internal"
        return nc.dram_tensor(name, list(shape), dt, kind=kind).ap()

    hin = din("hin", [T, D])
    cmod = din("cmod", [128, 8, 3])
    cst = din("cst", [128, 1024])
    w_mod = din("w_mod", [L, D, 6 * D])
    b_mod3 = din("b_mod3", [L, 3, 6 * D])
    gn1T = din("gn1T", [L, 128, 8])
    w_in = din("w_in", [L, D, DIN])
    ln_g = din("ln_v_g", [L, 512])
    ln_b = din("ln_v_b", [L, 512])
    w_spT = din("w_spT", [L, 4, 128, 128])
    b_sp = din("b_sp", [L, 512])
    w_gu = din("w_gate_up", [L, 2, 16, 256])
    b_gate = din("b_gate", [L, 2, 256])
    g_glaT = din("g_glaT", [L, 128, 4])
    w_out = din("w_out", [L, D, D])
    gn2 = din("g_norm2", [L, D])
    w_r = din("w_r", [L, D, 72])
    b_r = din("b_r", [L, 72])
    EW = 1 if stop_after in ("p0", "p1", "p2", "p3") else 64
    w1 = din("w1", [L, EW, D, 512])
    w3 = din("w3", [L, EW, D, 512])
    w2 = din("w2", [L, EW, 512, D])
    g_final = din("g_final", [1, D])
    out = nc.dram_tensor("out", [NS * SEQ, D], F32, kind="ExternalOutput").ap()

    H = dscr("H", [T, D], F32)
    MS = dscr("MS", [L, 3, 6 * D], F32)
    UT = dscr("UT", [4, 128, T], BF16)
    QT = dscr("QT", [2, 128, T], BF16)
    KT = dscr("KT", [2, 128, T], BF16)
    RT = dscr("RT", [4, 128, T], BF16)
    LA = dscr("LA", [T, 512], F32)
    Vd = dscr("Vd", [T, 512], BF16)
    Kd = dscr("Kd", [T, 256], BF16)
    VA = dscr("VA", [T, 512], BF16)
    XB = dscr("XB", [NSLOT, D], BF16)
    YB = dscr("YB", [NSLOT, D], F32)

    top = ExitStack()
    with top:
        S = Sched(nc, top)
        op = S.op

        def mset(tile_idx):
            s, j = divmod(tile_idx, TPS)
            return 2 if j < 2 else s

        def psb(name, shape, dt):
            return top.enter_context(nc.sbuf_tensor(name, shape, dt))
        cst32 = psb("cst32", [128, 1024], F32)
        cstb = psb("cstb", [128, 768], BF16)
        widx_i = psb("widx_i", [128, NBLK], I32)
        zt = psb("zt", [128, 4, D], BF16)
        dest_i = psb("dest_i", [128, NT * 2], I32)
        gate_f = psb("gate_f", [128, NT * 2], F32)
        IDENT, TRI, TRIT, SU, SLW, ONES = [slice(i * 128, (i + 1) * 128) for i in range(6)]
        op('sp', lambda e: e.dma_start(out=cst32[:], in_=cst[:, :]), writes=['cst32'], dma=True)
        op('dve', lambda e: e.tensor_copy(out=cstb[:], in_=cst32[:, 0:768]), reads=['cst32'], writes=['cstb'])
        op('dve', lambda e: e.memset(zt[:], 0.0), writes=['zt'])
        for zi in range(NSLOT // 512):
            op('sp', lambda e, zi=zi: e.dma_start(out=XB[zi * 512:(zi + 1) * 512, :].rearrange("(s p) d -> p s d", p=128), in_=zt[:]), reads=['zt'], writes=['XBz'], dma=True)
        S.flush()

        def rstd_from_ss(ss, rs, key_ss, key_rs, n):
            op('dve', lambda e: e.tensor_scalar(out=rs, in0=ss, scalar1=1.0 / n, scalar2=EPS, op0=ALU.mult, op1=ALU.add),
               reads=[key_ss], writes=[key_rs])
            op('act', lambda e: e.activation(out=rs, in_=rs, func=AF.Sqrt), reads=[key_rs], writes=[key_rs])
            op('dve', lambda e: e.reciprocal(out=rs, in_=rs), reads=[key_rs], writes=[key_rs])

        for l in range(L):
            Hsrc = hin if l == 0 else H
            with ExitStack() as es:
                def sb(name, shape, dt):
                    return es.enter_context(nc.sbuf_tensor(("L%d_p0_" % l) + name, shape, dt))
                scT = sb("scT", [128, 8, 3], F32)
                wm = [sb("wm%d" % i, [128, 8, 512], F32) for i in range(2)]
                bm = [sb("bm%d" % i, [3, 512], F32) for i in range(2)]
                mo = [sb("mo%d" % i, [3, 512], F32) for i in range(2)]
                psM = [es.enter_context(nc.psum_tensor("L%d_p0_psM%d" % (l, i), [128, 512], F32)) for i in range(2)]
                op('sp', lambda e: e.dma_start(out=scT[:], in_=cmod[:, :, :]), writes=['scT'], dma=True)
                op('act', lambda e: e.activation(out=scT[:], in_=scT[:], func=AF.Silu), reads=['scT'], writes=['scT'])
                for cg in range(12 if not _DBG.get('nop0') else 0):
                    i = cg % 2
                    cs = slice(cg * 512, (cg + 1) * 512)
                    op('sp', lambda e, i=i, cs=cs: e.dma_start(out=wm[i][:], in_=w_mod[l, :, cs].rearrange("(kc p) c -> p kc c", p=128)),
                       writes=['wm%d' % i], dma=True)
                    op('sp', lambda e, i=i, cs=cs: e.dma_start(out=bm[i][:], in_=b_mod3[l, :, cs]), writes=['bm%d' % i], dma=True)
                    for kc in range(8):
                        op('pe', lambda e, i=i, kc=kc: e.matmul(psM[i][0:3, :], lhsT=scT[:, kc, :], rhs=wm[i][:, kc, :],
                                                               start=(kc == 0), stop=(kc == 7)),
                           reads=['scT', 'wm%d' % i], writes=['psM%d' % i])
                    op('dve', lambda e, i=i: e.tensor_tensor(out=mo[i][:], in0=psM[i][0:3, :], in1=bm[i][:], op=ALU.add),
                       reads=['psM%d' % i, 'bm%d' % i], writes=['mo%d' % i])
                    op('sp', lambda e, i=i, cs=cs: e.dma_start(out=MS[l, :, cs], in_=mo[i][:]), reads=['mo%d' % i], writes=['MS'], dma=True)
                S.flush()

            if stop_after == 'p0':
                break
            with ExitStack() as es:
                def sb(name, shape, dt):
                    return es.enter_context(nc.sbuf_tensor(("L%d_p1_" % l) + name, shape, dt))

                def pst(name, shape, dt):
                    return es.enter_context(nc.psum_tensor(("L%d_p1_" % l) + name, shape, dt))
                winb = sb("winb", [128, 8, DIN], BF16)
                wup = sb("wup", [64, 512], F32)
                A1T = sb("A1T", [128, 3, 8], F32)
                sh1T = sb("sh1T", [128, 3, 8], F32)
                g1t = sb("g1t", [128, 8], F32)
                lng = sb("lng", [128, 512], F32)
                lnb = sb("lnb", [128, 512], F32)
                zgT = sb("zgT", [64, 256], F32)
                ht = [sb("ht%d" % i, [128, D], F32) for i in range(2)]
                junk = sb("junk", [128, D], BF16)
                hs = [sb("hs%d" % i, [128, D], BF16) for i in range(2)]
                hnT = [sb("hnT%d" % i, [128, 8, 256], BF16) for i in range(2)]
                st = [sb("st%d" % i, [128, 24], F32) for i in range(2)]
                fo = [sb("fo%d" % i, [128, 256], BF16) for i in range(4)]
                g32 = [sb("g32_%d" % i, [128, 512], F32) for i in range(2)]
                sq32 = [sb("sq32_%d" % i, [128, 512], F32) for i in range(2)]
                vab = [sb("vab%d" % i, [128, 512], BF16) for i in range(2)]
                vtb = [sb("vtb%d" % i, [128, 512], BF16) for i in range(2)]
                ktb = [sb("ktb%d" % i, [128, 256], BF16) for i in range(2)]
                la32 = [sb("la32_%d" % i, [128, 512], F32) for i in range(2)]
                psT = [pst("psT%d" % i, [128, 1024], BF16) for i in range(2)]
                psF = [pst("psF%d" % i, [128, 512], F32) for i in range(3)]
                psK = [pst("psK%d" % i, [128, 512], F32) for i in range(3)]

                for kc in range(8):
                    op('pool', lambda e, kc=kc: e.dma_start(out=winb[:, kc, :], in_=w_in[l, kc * 128:(kc + 1) * 128, :]),
                       writes=['winb'], dma=True)
                op('dve', lambda e: e.memset(wup[:], 0.0), writes=['wup'])
                op('sp', lambda e: e.dma_start(out=wup[0:16, 0:256], in_=w_gu[l, 0, :, :]), reads=['wup'], writes=['wup0'], dma=True)
                op('sp', lambda e: e.dma_start(out=wup[16:32, 256:512], in_=w_gu[l, 1, :, :]), reads=['wup'], writes=['wup1'], dma=True)
                op('sp', lambda e: e.dma_start(out=wup[32:33, 0:256], in_=b_gate[l, 0:1, :]), reads=['wup'], writes=['wup2'], dma=True)
                op('sp', lambda e: e.dma_start(out=wup[32:33, 256:512], in_=b_gate[l, 1:2, :]), reads=['wup'], writes=['wup3'], dma=True)
                WUPK = ['wup', 'wup0', 'wup1', 'wup2', 'wup3']
                op('dve', lambda e: e.memset(zgT[:], 1.0), writes=['zgT'])
                op('sp', lambda e: e.dma_start(out=g1t[:], in_=gn1T[l, :, :]), writes=['g1t'], dma=True)
                op('sp', lambda e: e.dma_start(out=lng[:], in_=ln_g[l:l + 1, :].to_broadcast([128, 512])), writes=['lng'], dma=True)
                op('sp', lambda e: e.dma_start(out=lnb[:], in_=ln_b[l:l + 1, :].to_broadcast([128, 512])), writes=['lnb'], dma=True)
                for ms in range(3):
                    op('sp', lambda e, ms=ms: e.dma_start(out=sh1T[:, ms, :], in_=MS[l, ms, 0:D].rearrange("(kc p) -> p kc", p=128),
                                                           allow_slow_non_contiguous=True), reads=['MS'], writes=['sh1T%d' % ms], dma=True)
                    op('sp', lambda e, ms=ms: e.dma_start(out=A1T[:, ms, :], in_=MS[l, ms, D:2 * D].rearrange("(kc p) -> p kc", p=128),
                                                           allow_slow_non_contiguous=True), reads=['MS'], writes=['A1T%d' % ms], dma=True)
                    op('dve', lambda e, ms=ms: e.scalar_tensor_tensor(out=A1T[:, ms, :], in0=A1T[:, ms, :], scalar=1.0, in1=g1t[:],
                                                                      op0=ALU.add, op1=ALU.mult),
                       reads=['A1T%d' % ms, 'g1t'], writes=['A1T%d' % ms])

                NG = NT // 2 if not _DBG.get('ng') else _DBG['ng']
                fcnt = [0]
                for gi in range(NG):
                    gb = gi % 2
                    t0 = gi * 256
                    ms = mset(gi * 2)
                    for ti in range(2):
                        tt = gi * 2 + ti
                        b = tt % 2
                        r0 = tt * 128
                        op('sp', lambda e, b=b, r0=r0: e.dma_start(out=ht[b][:], in_=Hsrc[r0:r0 + 128, :]), reads=['H'], writes=['ht%d' % b], dma=True)
                        op('dve', lambda e, b=b: e.memset(st[b][:, 0:1], 0.0), writes=['ss%d' % b])
                        op('act', lambda e, b=b: e.activation(out=junk[:], in_=ht[b][:], func=AF.Square, accum_out=st[b][:, 0:1]),
                           reads=['ht%d' % b, 'ss%d' % b], writes=['junk', 'ss%d' % b])
                        rstd_from_ss(st[b][:, 0:1], st[b][:, 1:2], 'ss%d' % b, 'rs%d' % b, D)
                        op('dve', lambda e, b=b: e.tensor_scalar(out=hs[b][:], in0=ht[b][:], scalar1=st[b][:, 1:2], scalar2=None, op0=ALU.mult),
                           reads=['ht%d' % b, 'rs%d' % b], writes=['hs%d' % b])
                        for kc in range(8):
                            op('pe', lambda e, b=b, kc=kc: e.transpose(out=psT[b][:, kc * 128:(kc + 1) * 128], in_=hs[b][:, kc * 128:(kc + 1) * 128],
                                                                      identity=cstb[:, IDENT]),
                               reads=['hs%d' % b, 'cstb'], writes=['psT%d' % b])
                        for kc in range(8):
                            eng = 'act' if kc % 2 == 0 else 'dve'
                            if eng == 'act':
                                f = lambda e, b=b, kc=kc, ti=ti: e.activation(out=hnT[gb][:, kc, ti * 128:(ti + 1) * 128], in_=psT[b][:, kc * 128:(kc + 1) * 128],
                                                                               func=AF.Identity, scale=A1T[:, ms, kc:kc + 1], bias=sh1T[:, ms, kc:kc + 1])
                            else:
                                f = lambda e, b=b, kc=kc, ti=ti: e.tensor_scalar(out=hnT[gb][:, kc, ti * 128:(ti + 1) * 128], in0=psT[b][:, kc * 128:(kc + 1) * 128],
                                                                                  scalar1=A1T[:, ms, kc:kc + 1], scalar2=sh1T[:, ms, kc:kc + 1],
                                                                                  op0=ALU.mult, op1=ALU.add)
                            op(eng, f, reads=['psT%d' % b, 'A1T%d' % ms, 'sh1T%d' % ms], writes=['hnT%d_%d_%d' % (gb, ti, kc)])
                    HK = ['hnT%d_%d_%d' % (gb, ti, kc) for ti in range(2) for kc in range(8)]
                    fm = [(OFF_AU + 128 * i, UT, i, AF.Gelu, 1.0) for i in range(4)]
                    fm += [(OFF_BQ + 128 * i, QT, i, AF.Copy, 0.125) for i in range(2)]
                    fm += [(OFF_BR + 128 * i, RT, i, AF.Silu, 1.0) for i in range(4)]
                    fm += [(OFF_BK + 128 * i, KT, i, AF.Copy, 1.0) for i in range(2)]
                    for (c0, dst, ci, func, scl) in fm:
                        n = fcnt[0]
                        fcnt[0] += 1
                        pb, ph = (n // 2) % 3, n % 2
                        pk = 'psF%d' % pb
                        pv = psF[pb][:, ph * 256:(ph + 1) * 256]
                        fb = n % 4
                        for kc in range(8):
                            op('pe', lambda e, pv=pv, kc=kc, c0=c0: e.matmul(pv, lhsT=winb[:, kc, c0:c0 + 128], rhs=hnT[gb][:, kc, :],
                                                                            start=(kc == 0), stop=(kc == 7)),
                               reads=['winb'] + HK, writes=[pk])
                        op('act', lambda e, pv=pv, fb=fb, func=func, scl=scl: e.activation(out=fo[fb][:], in_=pv, func=func, scale=scl),
                           reads=[pk], writes=['fo%d' % fb])
                        op('sp', lambda e, fb=fb, dst=dst, ci=ci: e.dma_start(out=dst[ci, :, t0:t0 + 256], in_=fo[fb][:]),
                           reads=['fo%d' % fb], writes=['z'], dma=True)
                    n = fcnt[0]
                    fcnt[0] += 1
                    pb, ph = (n // 2) % 3, n % 2
                    pk = 'psF%d' % pb
                    pvg = psF[pb][0:32, ph * 256:(ph + 1) * 256]
                    for kc in range(8):
                        op('pe', lambda e, pvg=pvg, kc=kc: e.matmul(pvg, lhsT=winb[:, kc, OFF_BG:OFF_BG + 32], rhs=hnT[gb][:, kc, :],
                                                                    start=(kc == 0), stop=(kc == 7)),
                           reads=['winb'] + HK, writes=[pk])
                    op('dve', lambda e, pvg=pvg: e.tensor_copy(out=zgT[0:32, :], in_=pvg), reads=[pk], writes=['zgT'])
                    for ti in range(2):
                        tt = gi * 2 + ti
                        b = tt % 2
                        r0 = tt * 128
                        HKt = ['hnT%d_%d_%d' % (gb, ti, kc) for kc in range(8)]
                        pz, pkk, pvv = psK[0], psK[1], psK[2]
                        for kc in range(8):
                            op('pe', lambda e, kc=kc, ti=ti: e.matmul(pz[:, :], lhsT=hnT[gb][:, kc, ti * 128:(ti + 1) * 128], rhs=winb[:, kc, OFF_AV:OFF_AV + 512],
                                                                      start=(kc == 0), stop=(kc == 7)), reads=['winb'] + HKt, writes=['psK0'])
                        for kc in range(8):
                            op('pe', lambda e, kc=kc, ti=ti: e.matmul(pvv[:, :], lhsT=hnT[gb][:, kc, ti * 128:(ti + 1) * 128], rhs=winb[:, kc, OFF_BV:OFF_BV + 512],
                                                                      start=(kc == 0), stop=(kc == 7)), reads=['winb'] + HKt, writes=['psK2'])
                        op('act', lambda e, b=b: e.activation(out=g32[b][:], in_=pz[:, :], func=AF.Gelu), reads=['psK0'], writes=['g32_%d' % b])
                        op('dve', lambda e, b=b: e.tensor_copy(out=vtb[b][:], in_=pvv[:, :]), reads=['psK2'], writes=['vtb%d' % b])
                        op('sp', lambda e, b=b, r0=r0: e.dma_start(out=Vd[r0:r0 + 128, :], in_=vtb[b][:]), reads=['vtb%d' % b], writes=['z'], dma=True)
                        for kc in range(8):
                            op('pe', lambda e, kc=kc, ti=ti: e.matmul(pkk[:, 0:256], lhsT=hnT[gb][:, kc, ti * 128:(ti + 1) * 128], rhs=winb[:, kc, OFF_BK:OFF_BK + 256],
                                                                      start=(kc == 0), stop=(kc == 7)), reads=['winb'] + HKt, writes=['psK1'])
                        op('act', lambda e, b=b: e.activation(out=ktb[b][:], in_=pkk[:, 0:256], func=AF.Copy), reads=['psK1'], writes=['ktb%d' % b])
                        op('sp', lambda e, b=b, r0=r0: e.dma_start(out=Kd[r0:r0 + 128, :], in_=ktb[b][:]), reads=['ktb%d' % b], writes=['z'], dma=True)
                        g3 = g32[b][:, :].rearrange("p (g c) -> p g c", g=4)
                        s3 = sq32[b][:, :].rearrange("p (g c) -> p g c", g=4)
                        stb = st[b]
                        op('pool', lambda e, b=b: e.tensor_tensor(out=sq32[b][:], in0=g32[b][:], in1=g32[b][:], op=ALU.mult),
                           reads=['g32_%d' % b], writes=['sq32_%d' % b])
                        op('dve', lambda e, g3=g3, stb=stb: e.tensor_reduce(out=stb[:, 4:8], in_=g3, axis=AX.X, op=ALU.add), reads=['g32_%d' % b], writes=['ln1_%d' % b])
                        op('dve', lambda e, s3=s3, stb=stb: e.tensor_reduce(out=stb[:, 8:12], in_=s3, axis=AX.X, op=ALU.add), reads=['sq32_%d' % b], writes=['ln2_%d' % b])
                        op('dve', lambda e, stb=stb: e.tensor_scalar(out=stb[:, 4:8], in0=stb[:, 4:8], scalar1=1.0 / 128, scalar2=None, op0=ALU.mult),
                           reads=['ln1_%d' % b], writes=['ln1_%d' % b])
                        op('dve', lambda e, stb=stb: e.tensor_tensor(out=stb[:, 12:16], in0=stb[:, 4:8], in1=stb[:, 4:8], op=ALU.mult),
                           reads=['ln1_%d' % b], writes=['ln3_%d' % b])
                        op('dve', lambda e, stb=stb: e.scalar_tensor_tensor(out=stb[:, 8:12], in0=stb[:, 8:12], scalar=1.0 / 128, in1=stb[:, 12:16],
                                                                            op0=ALU.mult, op1=ALU.subtract),
                           reads=['ln2_%d' % b, 'ln3_%d' % b], writes=['ln2_%d' % b])
                        op('dve', lambda e, stb=stb: e.tensor_scalar(out=stb[:, 8:12], in0=stb[:, 8:12], scalar1=EPS, scalar2=None, op0=ALU.add),
                           reads=['ln2_%d' % b], writes=['ln2_%d' % b])
                        op('act', lambda e, stb=stb: e.activation(out=stb[:, 8:12], in_=stb[:, 8:12], func=AF.Sqrt), reads=['ln2_%d' % b], writes=['ln2_%d' % b])
                        op('dve', lambda e, stb=stb: e.reciprocal(out=stb[:, 8:12], in_=stb[:, 8:12]), reads=['ln2_%d' % b], writes=['ln2_%d' % b])
                        op('dve', lambda e, g3=g3, stb=stb: e.tensor_tensor(out=g3, in0=g3, in1=stb[:, 4:8].unsqueeze(2).to_broadcast([128, 4, 128]), op=ALU.subtract),
                           reads=['g32_%d' % b, 'ln1_%d' % b, 'sq32_%d' % b], writes=['g32_%d' % b])
                        op('dve', lambda e, g3=g3, stb=stb: e.tensor_tensor(out=g3, in0=g3, in1=stb[:, 8:12].unsqueeze(2).to_broadcast([128, 4, 128]), op=ALU.mult),
                           reads=['g32_%d' % b, 'ln2_%d' % b], writes=['g32_%d' % b])
                        op('pool', lambda e, b=b: e.tensor_tensor(out=g32[b][:], in0=g32[b][:], in1=lng[:], op=ALU.mult),
                           reads=['g32_%d' % b, 'lng'], writes=['g32_%d' % b])
                        op('pool', lambda e, b=b: e.tensor_tensor(out=vab[b][:], in0=g32[b][:], in1=lnb[:], op=ALU.add),
                           reads=['g32_%d' % b, 'lnb'], writes=['vab%d' % b])
                        op('sp', lambda e, b=b, r0=r0: e.dma_start(out=VA[r0:r0 + 128, :], in_=vab[b][:]), reads=['vab%d' % b], writes=['z'], dma=True)
                        op('pe', lambda e, ti=ti: e.matmul(pkk[:, :], lhsT=zgT[0:64, ti * 128:(ti + 1) * 128], rhs=wup[0:64, :], start=True, stop=True),
                           reads=['zgT'] + WUPK + ['ktb%d' % b], writes=['psK1'])
                        op('act', lambda e, b=b: e.activation(out=la32[b][:], in_=pkk[:, :], func=AF.Exp, scale=-1.0), reads=['psK1'], writes=['la32_%d' % b])
                        op('act', lambda e, b=b: e.activation(out=la32[b][:], in_=la32[b][:], func=AF.Ln, bias=1.0), reads=['la32_%d' % b], writes=['la32_%d' % b])
                        op('pool', lambda e, b=b: e.tensor_scalar(out=la32[b][:], in0=la32[b][:], scalar1=-1.0 / 16, scalar2=None, op0=ALU.mult),
                           reads=['la32_%d' % b], writes=['la32_%d' % b])
                        op('sp', lambda e, b=b, r0=r0: e.dma_start(out=LA[r0:r0 + 128, :], in_=la32[b][:]), reads=['la32_%d' % b], writes=['z'], dma=True)
                S.flush()
            if stop_after == 'p1':
                break
            with ExitStack() as es:
                def sb(name, shape, dt):
                    return es.enter_context(nc.sbuf_tensor(("L%d_p2_" % l) + name, shape, dt))

                def pst(name, shape, dt):
                    return es.enter_context(nc.psum_tensor(("L%d_p2_" % l) + name, shape, dt))
                woutb = sb("woutb", [128, 8, D], BF16)
                wspb = sb("wspb", [128, 4, 128], BF16)
                bsp = sb("bsp", [128, 512], F32)
                ggla = sb("ggla", [128, 4], F32)
                g1rep = sb("g1rep", [128, 3, D], F32)
                KVs = sb("KVs", [128, TPS * 4, 128], F32)
                dec = sb("dec", [128, TPS * 4], F32)
                Sst = sb("Sst", [128, 4, 128], F32)
                Sprev = sb("Sprev", [128, TPS * 4, 128], BF16)
                la = [sb("la%d" % i, [128, 512], F32) for i in range(2)]
                ktm = [sb("ktm%d" % i, [128, 256], BF16) for i in range(2)]
                vtm = [sb("vtm%d" % i, [128, 512], BF16) for i in range(2)]
                eB = [sb("eB%d" % i, [128, 512], F32) for i in range(2)]
                kd = [sb("kd%d" % i, [128, 512], BF16) for i in range(2)]
                qTt = [sb("qT%d" % i, [128, 2, 128], BF16) for i in range(2)]
                kTt = [sb("kT%d" % i, [128, 2, 128], BF16) for i in range(2)]
                rTt = [sb("rT%d" % i, [128, 4, 128], BF16) for i in range(2)]
                uTt = [sb("uT%d" % i, [128, 4, 128], BF16) for i in range(2)]
                vat = [sb("va%d" % i, [128, 512], BF16) for i in range(2)]
                hh = [sb("hh%d" % i, [128, D], F32) for i in range(2)]
                Ep = [sb("Ep%d" % i, [128, 512], F32) for i in range(2)]
                Em = [sb("Em%d" % i, [128, 512], F32) for i in range(2)]
                qeL = [sb("qeL%d" % i, [128, 512], BF16) for i in range(2)]
                qeH = [sb("qeH%d" % i, [128, 512], BF16) for i in range(2)]
                keL = [sb("keL%d" % i, [128, 512], BF16) for i in range(2)]
                keH = [sb("keH%d" % i, [128, 512], BF16) for i in range(2)]
                atf = [sb("atf%d" % i, [128, 512], BF16) for i in range(2)]
                atb = [sb("atb%d" % i, [128, 512], BF16) for i in range(2)]
                sq = [sb("sq%d" % i, [128, 512], BF16) for i in range(2)]
                rs = [sb("rs%d" % i, [128, 512], F32) for i in range(2)]
                tmp = [sb("tmp%d" % i, [128, 512], F32) for i in range(2)]
                tmp2 = [sb("tmp2%d" % i, [128, 512], F32) for i in range(2)]
                bo = [sb("bo%d" % i, [128, 512], BF16) for i in range(2)]
                ao = [sb("ao%d" % i, [128, 512], BF16) for i in range(2)]
                hm = [sb("hm%d" % i, [128, D], F32) for i in range(2)]
                psB = pst("psB", [128, 512], F32)
                psKV = [pst("psKV%d" % i, [128, 512], F32) for i in range(2)]
                psD = pst("psD", [128, 512], F32)
                psO = pst("psO", [128, 512], F32)
                psP = pst("psP", [128, 512], F32)
                psM = [pst("psM%d" % i, [128, 512], F32) for i in range(2)]

                for i in range(2):
                    for (tl, nm) in ((qeL, 'qeL'), (qeH, 'qeH'), (keL, 'keL'), (keH, 'keH')):
                        op('dve', lambda e, tl=tl, i=i: e.memset(tl[i][:], 0.0), writes=['%s%d' % (nm, i)])
                for kc in range(8):
                    op('pool', lambda e, kc=kc: e.dma_start(out=woutb[:, kc, :], in_=w_out[l, kc * 128:(kc + 1) * 128, :]), writes=['woutb%d' % kc], dma=True)
                op('sp', lambda e: e.dma_start(out=ggla[:], in_=g_glaT[l, :, :]), writes=['ggla'], dma=True)
                for h4 in range(4):
                    op('dve', lambda e, h4=h4: e.tensor_scalar(out=woutb[:, 4 + h4, :], in0=woutb[:, 4 + h4, :], scalar1=ggla[:, h4:h4 + 1], scalar2=None, op0=ALU.mult),
                       reads=['ggla', 'woutb%d' % (4 + h4)], writes=['woutb%d' % (4 + h4)])
                WOK = ['woutb%d' % kc for kc in range(8)]
                op('pool', lambda e: e.dma_start(out=wspb[:], in_=w_spT[l, :, :, :].rearrange("g q p -> q g p")), writes=['wspb'], dma=True)
                op('sp', lambda e: e.dma_start(out=bsp[:], in_=b_sp[l:l + 1, :].to_broadcast([128, 512])), writes=['bsp'], dma=True)
                for ms in range(3):
                    op('sp', lambda e, ms=ms: e.dma_start(out=g1rep[:, ms, :], in_=MS[l, ms:ms + 1, 2 * D:3 * D].to_broadcast([128, D])), writes=['g1rep'], dma=True)

                for s in range(NS):
                    for j in range(TPS):
                        tt = s * TPS + j
                        r0 = tt * 128
                        b = j % 2
                        op('sp', lambda e, b=b, r0=r0: e.dma_start(out=la[b][:], in_=LA[r0:r0 + 128, :]), writes=['la%d' % b], dma=True)
                        op('sp', lambda e, b=b, r0=r0: e.dma_start(out=ktm[b][:], in_=Kd[r0:r0 + 128, :]), writes=['ktm%d' % b], dma=True)
                        op('sp', lambda e, b=b, r0=r0: e.dma_start(out=vtm[b][:], in_=Vd[r0:r0 + 128, :]), writes=['vtm%d' % b], dma=True)
                        op('pe', lambda e, b=b: e.matmul(psB[:, 0:256], lhsT=cst32[:, SU], rhs=la[b][:, 0:256], start=True, stop=True), reads=['la%d' % b, 'cst32'], writes=['psB'])
                        op('pe', lambda e, b=b: e.matmul(psB[:, 256:512], lhsT=cst32[:, SLW], rhs=la[b][:, 256:512], start=True, stop=True), reads=['la%d' % b, 'cst32'], writes=['psB'])
                        op('act', lambda e, b=b: e.activation(out=eB[b][:], in_=psB[:, :], func=AF.Exp), reads=['psB'], writes=['eB%d' % b])
                        op('dve', lambda e, b=b: e.tensor_tensor(out=kd[b][:, :].rearrange("p (a c) -> p a c", a=2), in0=eB[b][:, :].rearrange("p (a c) -> p a c", a=2),
                                                                 in1=ktm[b][:, :].unsqueeze(1).to_broadcast([128, 2, 256]), op=ALU.mult),
                           reads=['eB%d' % b, 'ktm%d' % b], writes=['kd%d' % b])
                        for idx in range(4):
                            dr, hp = divmod(idx, 2)
                            pv = psKV[dr][:, hp * 256:(hp + 1) * 256]
                            op('pe', lambda e, b=b, pv=pv, dr=dr, hp=hp: e.matmul(pv, lhsT=kd[b][:, dr * 256 + hp * 128: dr * 256 + hp * 128 + 128],
                                                                                  rhs=vtm[b][:, hp * 256:(hp + 1) * 256], start=True, stop=True),
                               reads=['kd%d' % b, 'vtm%d' % b], writes=['psKV%d' % dr])
                            op('act', lambda e, pv=pv, j=j, idx=idx: e.activation(out=KVs[0:64, j * 4 + idx, :], in_=pv[0:64, 0:128], func=AF.Copy),
                               reads=['psKV%d' % dr], writes=['KVa%d_%d' % (j, idx)])
                            op('dve', lambda e, pv=pv, j=j, idx=idx: e.tensor_copy(out=KVs[64:128, j * 4 + idx, :], in_=pv[64:128, 128:256]),
                               reads=['psKV%d' % dr], writes=['KVb%d_%d' % (j, idx)])
                            op('pe', lambda e, b=b, idx=idx, dr=dr, hp=hp: e.matmul(psD[:, idx:idx + 1], lhsT=la[b][:, dr * 256 + hp * 128: dr * 256 + hp * 128 + 128],
                                                                                    rhs=cst32[:, 5 * 128:5 * 128 + 1], start=True, stop=True),
                               reads=['la%d' % b, 'cst32'], writes=['psD'])
                        op('act', lambda e, j=j: e.activation(out=dec[:, j * 4:(j + 1) * 4], in_=psD[:, 0:4], func=AF.Exp), reads=['psD'], writes=['dec%d' % j])
                    op('dve', lambda e: e.memset(Sst[:], 0.0), writes=['Sst0', 'Sst1', 'Sst2', 'Sst3'])
                    order_f = list(range(TPS))
                    order_b = [1, 0] + list(range(TPS - 1, 1, -1))
                    for dr, order in ((0, order_f), (1, order_b)):
                        for j in order:
                            for hp in range(2):
                                idx = dr * 2 + hp
                                op('pool', lambda e, j=j, idx=idx: e.tensor_copy(out=Sprev[:, j * 4 + idx, :], in_=Sst[:, idx, :]),
                                   reads=['Sst%d' % idx], writes=['Sprev%d_%d' % (j, idx)])
                                op('dve', lambda e, j=j, idx=idx: e.scalar_tensor_tensor(out=Sst[:, idx, :], in0=Sst[:, idx, :], scalar=dec[:, j * 4 + idx: j * 4 + idx + 1],
                                                                                         in1=KVs[:, j * 4 + idx, :], op0=ALU.mult, op1=ALU.add),
                                   reads=['Sst%d' % idx, 'dec%d' % j, 'KVa%d_%d' % (j, idx), 'KVb%d_%d' % (j, idx)], writes=['Sst%d' % idx])
                    for j in range(TPS):
                        tt = s * TPS + j
                        r0 = tt * 128
                        b = j % 2
                        ms = mset(tt)
                        op('sp', lambda e, b=b, r0=r0: e.dma_start(out=la[b][:], in_=LA[r0:r0 + 128, :]), writes=['la%d' % b], dma=True)
                        op('sp', lambda e, b=b, r0=r0: e.dma_start(out=vtm[b][:], in_=Vd[r0:r0 + 128, :]), writes=['vtm%d' % b], dma=True)
                        op('sp', lambda e, b=b, r0=r0: e.dma_start(out=qTt[b][:], in_=QT[:, :, r0:r0 + 128].rearrange("h p t -> p h t")), writes=['qT%d' % b], dma=True)
                        op('sp', lambda e, b=b, r0=r0: e.dma_start(out=kTt[b][:], in_=KT[:, :, r0:r0 + 128].rearrange("h p t -> p h t")), writes=['kT%d' % b], dma=True)
                        op('sp', lambda e, b=b, r0=r0: e.dma_start(out=rTt[b][:], in_=RT[:, :, r0:r0 + 128].rearrange("h p t -> p h t")), writes=['rT%d' % b], dma=True)
                        op('sp', lambda e, b=b, r0=r0: e.dma_start(out=uTt[b][:], in_=UT[:, :, r0:r0 + 128].rearrange("h p t -> p h t")), writes=['uT%d' % b], dma=True)
                        op('sp', lambda e, b=b, r0=r0: e.dma_start(out=vat[b][:], in_=VA[r0:r0 + 128, :]), writes=['va%d' % b], dma=True)
                        op('sp', lambda e, b=b, r0=r0: e.dma_start(out=hh[b][:], in_=Hsrc[r0:r0 + 128, :]), writes=['hh%d' % b], dma=True)
                        for idx in range(4):
                            dr, hp = divmod(idx, 2)
                            msk = TRI if dr == 0 else TRIT
                            op('pe', lambda e, b=b, idx=idx, dr=dr, hp=hp, msk=msk: e.matmul(psB[:, idx * 128:(idx + 1) * 128],
                                                                                             lhsT=la[b][:, dr * 256 + hp * 128: dr * 256 + hp * 128 + 128],
                                                                                             rhs=cst32[:, msk], start=True, stop=True),
                               reads=['la%d' % b, 'cst32'], writes=['psB'])
                        op('act', lambda e, b=b: e.activation(out=Ep[b][:], in_=psB[:, :], func=AF.Exp), reads=['psB'], writes=['Ep%d' % b])
                        op('act', lambda e, b=b: e.activation(out=Em[b][:], in_=psB[:, :], func=AF.Exp, scale=-1.0), reads=['psB'], writes=['Em%d' % b])
                        for (rows_, qd, kd_, sfx) in ((slice(0, 64), qeL, keL, 'L'), (slice(64, 128), qeH, keH, 'H')):
                            op('dve', lambda e, b=b, rows_=rows_, qd=qd: e.tensor_tensor(out=qd[b][rows_, :].rearrange("p (a c) -> p a c", a=2),
                                                                                         in0=Ep[b][rows_, :].rearrange("p (a c) -> p a c", a=2),
                                                                                         in1=qTt[b][rows_, :, :].rearrange("p h t -> p (h t)").unsqueeze(1).to_broadcast([64, 2, 256]), op=ALU.mult),
                               reads=['Ep%d' % b, 'qT%d' % b], writes=['qe%s%d' % (sfx, b)])
                            op('pool', lambda e, b=b, rows_=rows_, kd_=kd_: e.tensor_tensor(out=kd_[b][rows_, :].rearrange("p (a c) -> p a c", a=2),
                                                                                            in0=Em[b][rows_, :].rearrange("p (a c) -> p a c", a=2),
                                                                                            in1=kTt[b][rows_, :, :].rearrange("p h t -> p (h t)").unsqueeze(1).to_broadcast([64, 2, 256]), op=ALU.mult),
                               reads=['Em%d' % b, 'kT%d' % b], writes=['ke%s%d' % (sfx, b)])
                        for dr in range(2):
                            for h4 in range(4):
                                hp, par = divmod(h4, 2)
                                rows = slice(par * 64, (par + 1) * 64)
                                cs = slice((dr * 2 + hp) * 128, (dr * 2 + hp + 1) * 128)
                                kx = keL if par == 0 else keH
                                qx = qeL if par == 0 else qeH
                                sfx = 'L' if par == 0 else 'H'
                                op('pe', lambda e, b=b, dr=dr, h4=h4, cs=cs, kx=kx, qx=qx: e.matmul(psKV[dr][:, h4 * 128:(h4 + 1) * 128], lhsT=kx[b][:, cs], rhs=qx[b][:, cs],
                                                                                                    start=True, stop=True),
                                   reads=['ke%s%d' % (sfx, b), 'qe%s%d' % (sfx, b)], writes=['psKV%d' % dr])
                        op('dve', lambda e, b=b: e.tensor_tensor(out=atf[b][:, :].rearrange("p (a c) -> p a c", a=4), in0=psKV[0][:, :].rearrange("p (a c) -> p a c", a=4),
                                                                 in1=cst32[:, TRI].unsqueeze(1).to_broadcast([128, 4, 128]), op=ALU.mult),
                           reads=['psKV0', 'cst32'], writes=['atf%d' % b])
                        op('dve', lambda e, b=b: e.tensor_tensor(out=atb[b][:, :].rearrange("p (a c) -> p a c", a=4), in0=psKV[1][:, :].rearrange("p (a c) -> p a c", a=4),
                                                                 in1=cst32[:, TRIT].unsqueeze(1).to_broadcast([128, 4, 128]), op=ALU.mult),
                           reads=['psKV1', 'cst32'], writes=['atb%d' % b])
                        for h4 in range(4):
                            hp, par = divmod(h4, 2)
                            rows = slice(par * 64, (par + 1) * 64)
                            hs_ = slice(h4 * 128, (h4 + 1) * 128)
                            op('pe', lambda e, b=b, hs_=hs_: e.matmul(psO[:, hs_], lhsT=vtm[b][:, hs_], rhs=atf[b][:, hs_], start=True, stop=False),
                               reads=['vtm%d' % b, 'atf%d' % b], writes=['psO'])
                            op('pe', lambda e, b=b, hs_=hs_: e.matmul(psO[:, hs_], lhsT=vtm[b][:, hs_], rhs=atb[b][:, hs_], start=False, stop=False),
                               reads=['vtm%d' % b, 'atb%d' % b], writes=['psO'])
                            for dr in range(2):
                                cs = slice((dr * 2 + hp) * 128, (dr * 2 + hp + 1) * 128)
                                qx = qeL if par == 0 else qeH
                                sfx = 'L' if par == 0 else 'H'
                                op('pe', lambda e, b=b, hs_=hs_, cs=cs, dr=dr, hp=hp, j=j, qx=qx: e.matmul(psO[:, hs_], lhsT=Sprev[:, j * 4 + dr * 2 + hp, :], rhs=qx[b][:, cs],
                                                                                                           start=False, stop=(dr == 1)),
                                   reads=['Sprev%d_%d' % (j, dr * 2 + hp), 'qe%s%d' % (sfx, b)], writes=['psO'])
                        op('act', lambda e, b=b: e.activation(out=sq[b][:], in_=psO[:, :], func=AF.Square), reads=['psO'], writes=['sq%d' % b])
                        op('pe', lambda e, b=b: e.matmul(psD[:, :], lhsT=cstb[:, ONES], rhs=sq[b][:], start=True, stop=True), reads=['sq%d' % b, 'cstb'], writes=['psD'])
                        op('dve', lambda e, b=b: e.tensor_scalar(out=rs[b][:], in0=psD[:, :], scalar1=1.0 / 128, scalar2=EPS, op0=ALU.mult, op1=ALU.add), reads=['psD'], writes=['rs%d' % b])
                        op('act', lambda e, b=b: e.activation(out=rs[b][:], in_=rs[b][:], func=AF.Sqrt), reads=['rs%d' % b], writes=['rs%d' % b])
                        op('dve', lambda e, b=b: e.reciprocal(out=rs[b][:], in_=rs[b][:]), reads=['rs%d' % b], writes=['rs%d' % b])
                        op('dve', lambda e, b=b: e.tensor_tensor(out=tmp[b][:], in0=psO[:, :], in1=rs[b][:], op=ALU.mult), reads=['psO', 'rs%d' % b], writes=['tmp%d' % b])
                        op('pool', lambda e, b=b: e.tensor_tensor(out=bo[b][:], in0=tmp[b][:], in1=rTt[b][:, :, :].rearrange("p h t -> p (h t)"), op=ALU.mult),
                           reads=['tmp%d' % b, 'rT%d' % b], writes=['bo%d' % b])
                        for g4 in range(4):
                            gs = slice(g4 * 128, (g4 + 1) * 128)
                            op('pe', lambda e, b=b, gs=gs, g4=g4: e.matmul(psP[:, gs], lhsT=vat[b][:, gs], rhs=wspb[:, g4, :], start=True, stop=True),
                               reads=['va%d' % b, 'wspb'], writes=['psP'])
                        op('dve', lambda e, b=b: e.tensor_tensor(out=tmp2[b][:], in0=psP[:, :], in1=bsp[:], op=ALU.add), reads=['psP', 'bsp'], writes=['tmp2%d' % b])
                        op('pool', lambda e, b=b: e.tensor_tensor(out=ao[b][:], in0=tmp2[b][:], in1=uTt[b][:, :, :].rearrange("p h t -> p (h t)"), op=ALU.mult),
                           reads=['tmp2%d' % b, 'uT%d' % b], writes=['ao%d' % b])
                        for half in range(2):
                            for kc in range(8):
                                src = ao[b] if kc < 4 else bo[b]
                                ks = slice((kc % 4) * 128, (kc % 4 + 1) * 128)
                                op('pe', lambda e, half=half, kc=kc, src=src, ks=ks: e.matmul(psM[half][:, :], lhsT=src[:, ks], rhs=woutb[:, kc, half * 512:(half + 1) * 512],
                                                                                              start=(kc == 0), stop=(kc == 7)),
                                   reads=['ao%d' % b, 'bo%d' % b] + WOK, writes=['psM%d' % half])
                            op('dve', lambda e, b=b, half=half, ms=ms: e.tensor_tensor(out=hm[b][:, half * 512:(half + 1) * 512], in0=psM[half][:, :],
                                                                                       in1=g1rep[:, ms, half * 512:(half + 1) * 512], op=ALU.mult),
                               reads=['psM%d' % half, 'g1rep'], writes=['hm%d_%d' % (b, half)])
                        op('pool', lambda e, b=b: e.tensor_tensor(out=hm[b][:], in0=hm[b][:], in1=hh[b][:], op=ALU.add),
                           reads=['hm%d_0' % b, 'hm%d_1' % b, 'hh%d' % b], writes=['hm%d_0' % b, 'hm%d_1' % b])
                        op('sp', lambda e, b=b, r0=r0: e.dma_start(out=H[r0:r0 + 128, :], in_=hm[b][:]), reads=['hm%d_0' % b, 'hm%d_1' % b], writes=['Hrow%d' % tt], dma=True)
                S.flush()
            if stop_after == 'p2':
                break
            with ExitStack() as es:
                def sb(name, shape, dt):
                    return es.enter_context(nc.sbuf_tensor(("L%d_p3_" % l) + name, shape, dt))

                def pst(name, shape, dt):
                    return es.enter_context(nc.psum_tensor(("L%d_p3_" % l) + name, shape, dt))
                wrb = sb("wrb", [128, 8, 72], BF16)
                brep = sb("brep", [128, 72], F32)
                A2rep = sb("A2rep", [128, 3, D], F32)
                sh2rep = sb("sh2rep", [128, 3, D], F32)
                gn2rep = sb("gn2rep", [128, D], F32)
                hn2all = sb("hn2all", [128, NT, D], BF16)
                ek = sb("ek", [128, NT * 2], F32)
                rank = sb("rank", [128, NT * 2], F32)
                base = sb("base", [128, 64], F32)
                big = sb("big", [128, NBLK * 64], F32)
                h2 = [sb("h2_%d" % i, [128, D], F32) for i in range(2)]
                hs2 = [sb("hs2_%d" % i, [128, D], F32) for i in range(2)]
                junk = sb("junk", [128, D], BF16)
                hT2 = [sb("hT2_%d" % i, [128, D], BF16) for i in range(2)]
                st = [sb("st%d" % i, [128, 4], F32) for i in range(2)]
                Lg = [sb("Lg%d" % i, [128, 72], F32) for i in range(2)]
                R = [sb("R%d" % i, [128, 16], F32) for i in range(2)]
                eg = [sb("eg%d" % i, [128, 8], F32) for i in range(2)]
                ohg = [sb("ohg%d" % i, [128, 8], F32) for i in range(2)]
                sel3 = [sb("sel3_%d" % i, [128, 64], F32) for i in range(2)]
                lsel = [sb("lsel%d" % i, [128, 8], F32) for i in range(2)]
                oh1 = [sb("oh1_%d" % i, [128, 8], F32) for i in range(2)]
                oh2 = [sb("oh2_%d" % i, [128, 8], F32) for i in range(2)]
                l2 = [sb("l2_%d" % i, [128, 8], F32) for i in range(2)]
                oh64 = [[sb("oh64_%d_%d" % (k, i), [128, 64], F32) for i in range(2)] for k in range(2)]
                t64 = [sb("t64_%d" % i, [128, 64], F32) for i in range(2)]
                Abf = [sb("Abf%d" % i, [128, 64], BF16) for i in range(2)]
                pos = [sb("pos%d" % i, [128, 64], F32) for i in range(2)]
                cntT = sb("cntT", [64, 128], F32)
                cmpT = sb("cmpT", [64, 128], F32)
                nblk = sb("nblk", [64, 1], F32)
                padT = sb("padT", [64, 128], F32)
                padse = sb("padse", [128, 128], F32)
                blke_f = sb("blke_f", [128, NBLK], F32)
                dest_f = sb("dest_f", [128, NT * 2], F32)
                psT = [pst("psT%d" % i, [128, D], BF16) for i in range(2)]
                psL = pst("psL", [128, 512], F32)
                psC = pst("psC", [128, 512], F32)
                psCT = pst("psCT", [128, 512], F32)
                psE = pst("psE", [128, 512], F32)
                IOTA = slice(768, 832)
                BLKS = slice(832, 832 + NBLK)
                THR = slice(832, 960)

                op('pool', lambda e: e.dma_start(out=wrb[:], in_=w_r[l, :, :].rearrange("(kc p) c -> p kc c", p=128)), writes=['wrb'], dma=True)
                op('sp', lambda e: e.dma_start(out=brep[:], in_=b_r[l:l + 1, :].to_broadcast([128, 72])), writes=['brep'], dma=True)
                op('sp', lambda e: e.dma_start(out=gn2rep[:], in_=gn2[l:l + 1, :].to_broadcast([128, D])), writes=['gn2rep'], dma=True)
                for ms in range(3):
                    op('sp', lambda e, ms=ms: e.dma_start(out=sh2rep[:, ms, :], in_=MS[l, ms:ms + 1, 3 * D:4 * D].to_broadcast([128, D])), writes=['sh2rep%d' % ms], dma=True)
                    op('sp', lambda e, ms=ms: e.dma_start(out=A2rep[:, ms, :], in_=MS[l, ms:ms + 1, 4 * D:5 * D].to_broadcast([128, D])), writes=['A2rep%d' % ms], dma=True)
                    op('dve', lambda e, ms=ms: e.scalar_tensor_tensor(out=A2rep[:, ms, :], in0=A2rep[:, ms, :], scalar=1.0, in1=gn2rep[:], op0=ALU.add, op1=ALU.mult),
                       reads=['A2rep%d' % ms, 'gn2rep'], writes=['A2rep%d' % ms])
                op('dve', lambda e: e.memset(base[:], 0.0), writes=['base'])
                for tt in range(NT):
                    b = tt % 2
                    r0 = tt * 128
                    ms = mset(tt)
                    Rb, Lgb = R[b], Lg[b]
                    rk = 'R%d' % b
                    op('sp', lambda e, b=b, r0=r0: e.dma_start(out=h2[b][:], in_=H[r0:r0 + 128, :]), writes=['h2_%d' % b], dma=True)
                    op('dve', lambda e, b=b: e.memset(st[b][:, 0:1], 0.0), writes=['ss%d' % b])
                    op('act', lambda e, b=b: e.activation(out=junk[:], in_=h2[b][:], func=AF.Square, accum_out=st[b][:, 0:1]),
                       reads=['h2_%d' % b, 'ss%d' % b], writes=['junk', 'ss%d' % b])
                    rstd_from_ss(st[b][:, 0:1], st[b][:, 1:2], 'ss%d' % b, 'rs%d' % b, D)
                    op('dve', lambda e, b=b: e.tensor_scalar(out=hs2[b][:], in0=h2[b][:], scalar1=st[b][:, 1:2], scalar2=None, op0=ALU.mult),
                       reads=['h2_%d' % b, 'rs%d' % b], writes=['hs2_%d' % b])
                    op('pool', lambda e, b=b, ms=ms: e.tensor_tensor(out=hs2[b][:], in0=hs2[b][:], in1=A2rep[:, ms, :], op=ALU.mult),
                       reads=['hs2_%d' % b, 'A2rep%d' % ms], writes=['hs2_%d' % b])
                    op('pool', lambda e, b=b, ms=ms, tt=tt: e.tensor_tensor(out=hn2all[:, tt, :], in0=hs2[b][:], in1=sh2rep[:, ms, :], op=ALU.add),
                       reads=['hs2_%d' % b, 'sh2rep%d' % ms], writes=['hn2_%d' % tt])
                    for kc in range(8):
                        op('pe', lambda e, b=b, kc=kc, tt=tt: e.transpose(out=psT[b][:, kc * 128:(kc + 1) * 128], in_=hn2all[:, tt, kc * 128:(kc + 1) * 128], identity=cstb[:, IDENT]),
                           reads=['hn2_%d' % tt, 'cstb'], writes=['psT%d' % b])
                    op('act', lambda e, b=b: e.activation(out=hT2[b][:], in_=psT[b][:, :], func=AF.Copy), reads=['psT%d' % b], writes=['hT2_%d' % b])
                    for kc in range(8):
                        op('pe', lambda e, b=b, kc=kc: e.matmul(psL[:, 0:72], lhsT=hT2[b][:, kc * 128:(kc + 1) * 128], rhs=wrb[:, kc, :], start=(kc == 0), stop=(kc == 7)),
                           reads=['hT2_%d' % b, 'wrb'], writes=['psL'])
                    op('dve', lambda e, Lgb=Lgb: e.tensor_tensor(out=Lgb[:], in0=psL[:, 0:72], in1=brep[:], op=ALU.add), reads=['psL', 'brep'], writes=['Lg%d' % b])
                    LK = 'Lg%d' % b
                    op('dve', lambda e, Rb=Rb, Lgb=Lgb: e.tensor_reduce(out=Rb[:, 0:1], in_=Lgb[:, 0:8], axis=AX.X, op=ALU.max), reads=[LK], writes=[rk])
                    op('dve', lambda e, Rb=Rb: e.tensor_scalar(out=Rb[:, 1:2], in0=Rb[:, 0:1], scalar1=-1.0, scalar2=None, op0=ALU.mult), reads=[rk], writes=[rk])
                    op('dve', lambda e, Rb=Rb: e.memset(Rb[:, 2:3], 0.0), reads=[rk], writes=[rk])
                    op('act', lambda e, Rb=Rb, Lgb=Lgb, b=b: e.activation(out=eg[b][:], in_=Lgb[:, 0:8], func=AF.Exp, bias=Rb[:, 1:2], scale=1.0, accum_out=Rb[:, 2:3]),
                       reads=[LK, rk], writes=[rk, 'eg%d' % b])
                    op('dve', lambda e, Rb=Rb: e.reciprocal(out=Rb[:, 3:4], in_=Rb[:, 2:3]), reads=[rk], writes=[rk])
                    op('dve', lambda e, Rb=Rb, Lgb=Lgb, b=b: e.tensor_scalar(out=ohg[b][:], in0=Lgb[:, 0:8], scalar1=Rb[:, 0:1], scalar2=None, op0=ALU.is_equal),
                       reads=[LK, rk], writes=['ohg%d' % b])
                    op('dve', lambda e, Lgb=Lgb, b=b: e.tensor_tensor(out=sel3[b][:, :].rearrange("p (g x) -> p g x", g=8), in0=Lgb[:, 8:72].rearrange("p (g x) -> p g x", g=8),
                                                                      in1=ohg[b][:, :].unsqueeze(2).to_broadcast([128, 8, 8]), op=ALU.mult),
                       reads=[LK, 'ohg%d' % b], writes=['sel3_%d' % b])
                    op('dve', lambda e, b=b: e.tensor_reduce(out=lsel[b][:], in_=sel3[b][:, :].rearrange("p (g x) -> p x g", g=8), axis=AX.X, op=ALU.add),
                       reads=['sel3_%d' % b], writes=['lsel%d' % b])
                    op('dve', lambda e, Rb=Rb, b=b: e.tensor_reduce(out=Rb[:, 4:5], in_=lsel[b][:], axis=AX.X, op=ALU.max), reads=['lsel%d' % b, rk], writes=[rk])
                    op('dve', lambda e, Rb=Rb, b=b: e.tensor_scalar(out=oh1[b][:], in0=lsel[b][:], scalar1=Rb[:, 4:5], scalar2=None, op0=ALU.is_equal),
                       reads=['lsel%d' % b, rk], writes=['oh1_%d' % b])
                    op('dve', lambda e, b=b: e.scalar_tensor_tensor(out=l2[b][:], in0=oh1[b][:], scalar=-1e30, in1=lsel[b][:], op0=ALU.mult, op1=ALU.add),
                       reads=['oh1_%d' % b, 'lsel%d' % b], writes=['l2_%d' % b])
                    op('dve', lambda e, Rb=Rb, b=b: e.tensor_reduce(out=Rb[:, 5:6], in_=l2[b][:], axis=AX.X, op=ALU.max), reads=['l2_%d' % b, rk], writes=[rk])
                    op('dve', lambda e, Rb=Rb, b=b: e.tensor_scalar(out=oh2[b][:], in0=l2[b][:], scalar1=Rb[:, 5:6], scalar2=None, op0=ALU.is_equal),
                       reads=['l2_%d' % b, rk], writes=['oh2_%d' % b])
                    op('dve', lambda e, Rb=Rb: e.tensor_tensor(out=Rb[:, 6:7], in0=Rb[:, 5:6], in1=Rb[:, 4:5], op=ALU.subtract), reads=[rk], writes=[rk])
                    op('act', lambda e, Rb=Rb: e.activation(out=Rb[:, 7:8], in_=Rb[:, 6:7], func=AF.Exp), reads=[rk], writes=[rk])
                    op('dve', lambda e, Rb=Rb: e.tensor_scalar(out=Rb[:, 7:8], in0=Rb[:, 7:8], scalar1=1.0, scalar2=None, op0=ALU.add), reads=[rk], writes=[rk])
                    op('dve', lambda e, Rb=Rb: e.reciprocal(out=Rb[:, 7:8], in_=Rb[:, 7:8]), reads=[rk], writes=[rk])
                    op('dve', lambda e, Rb=Rb, tt=tt: e.tensor_tensor(out=gate_f[:, tt * 2:tt * 2 + 1], in0=Rb[:, 7:8], in1=Rb[:, 3:4], op=ALU.mult), reads=[rk], writes=['gate%d' % tt])
                    op('dve', lambda e, Rb=Rb, tt=tt: e.tensor_tensor(out=gate_f[:, tt * 2 + 1:tt * 2 + 2], in0=Rb[:, 3:4], in1=gate_f[:, tt * 2:tt * 2 + 1], op=ALU.subtract),
                       reads=[rk, 'gate%d' % tt], writes=['gate%d' % tt])
                    for k, ohk, ohkey in ((0, oh1, 'oh1_%d' % b), (1, oh2, 'oh2_%d' % b)):
                        op('dve', lambda e, b=b, k=k, ohk=ohk: e.tensor_tensor(out=oh64[k][b][:, :].rearrange("p (g x) -> p g x", g=8),
                                                                                in0=ohg[b][:, :].unsqueeze(2).to_broadcast([128, 8, 8]),
                                                                                in1=ohk[b][:, :].unsqueeze(1).to_broadcast([128, 8, 8]), op=ALU.mult),
                           reads=['ohg%d' % b, ohkey], writes=['oh64_%d_%d' % (k, b)])
                        op('dve', lambda e, b=b, k=k: e.tensor_tensor(out=t64[b][:], in0=oh64[k][b][:], in1=cst32[:, IOTA], op=ALU.mult),
                           reads=['oh64_%d_%d' % (k, b), 'cst32'], writes=['t64_%d' % b])
                        op('dve', lambda e, b=b, k=k, tt=tt: e.tensor_reduce(out=ek[:, tt * 2 + k:tt * 2 + k + 1], in_=t64[b][:], axis=AX.X, op=ALU.add),
                           reads=['t64_%d' % b], writes=['ek%d_%d' % (tt, k)])
                    op('dve', lambda e, b=b: e.tensor_tensor(out=Abf[b][:], in0=oh64[0][b][:], in1=oh64[1][b][:], op=ALU.add),
                       reads=['oh64_0_%d' % b, 'oh64_1_%d' % b], writes=['Abf%d' % b])
                    op('pe', lambda e, b=b: e.matmul(psC[:, 0:64], lhsT=cstb[:, SLW], rhs=Abf[b][:], start=True, stop=True), reads=['Abf%d' % b, 'cstb'], writes=['psC'])
                    op('pe', lambda e, b=b: e.matmul(psC[:, 64:128], lhsT=cstb[:, ONES], rhs=Abf[b][:], start=True, stop=True), reads=['Abf%d' % b, 'cstb'], writes=['psC'])
                    op('pe', lambda e, b=b, tt=tt: e.matmul(psCT[0:64, 0:128], lhsT=Abf[b][:], rhs=cstb[:, ONES], start=(tt == 0), stop=(tt == NT - 1)),
                       reads=['Abf%d' % b, 'cstb'], writes=['psCT'])
                    op('dve', lambda e, b=b: e.tensor_tensor(out=pos[b][:], in0=psC[:, 0:64], in1=base[:], op=ALU.add), reads=['psC', 'base'], writes=['pos%d' % b])
                    for k in range(2):
                        op('dve', lambda e, b=b, k=k: e.tensor_tensor(out=t64[b][:], in0=oh64[k][b][:], in1=pos[b][:], op=ALU.mult),
                           reads=['oh64_%d_%d' % (k, b), 'pos%d' % b], writes=['t64_%d' % b])
                        op('dve', lambda e, b=b, k=k, tt=tt: e.tensor_reduce(out=rank[:, tt * 2 + k:tt * 2 + k + 1], in_=t64[b][:], axis=AX.X, op=ALU.add),
                           reads=['t64_%d' % b], writes=['rank%d_%d' % (tt, k)])
                    op('dve', lambda e: e.tensor_tensor(out=base[:], in0=psC[:, 64:128], in1=base[:], op=ALU.add), reads=['psC', 'base'], writes=['base'])
                op('dve', lambda e: e.tensor_copy(out=cntT[:], in_=psCT[0:64, 0:128]), reads=['psCT'], writes=['cntT'])
                op('dve', lambda e: e.tensor_tensor(out=cmpT[:], in0=cntT[:], in1=cst32[0:64, THR], op=ALU.is_gt), reads=['cntT', 'cst32'], writes=['cmpT'])
                op('dve', lambda e: e.tensor_reduce(out=nblk[:], in_=cmpT[:], axis=AX.X, op=ALU.add), reads=['cmpT'], writes=['nblk'])
                op('dve', lambda e: e.tensor_scalar(out=padT[:], in0=cst32[0:64, ONES], scalar1=nblk[:, 0:1], scalar2=float(MB), op0=ALU.mult, op1=ALU.mult),
                   reads=['nblk', 'cst32'], writes=['padT'])
                op('pe', lambda e: e.matmul(psE[:, 0:64], lhsT=padT[:], rhs=cst32[0:64, 4 * 128:4 * 128 + 64], start=True, stop=True), reads=['padT', 'cst32'], writes=['psE'])
                op('pe', lambda e: e.matmul(psE[:, 64:128], lhsT=padT[:], rhs=cst32[0:64, 128:192], start=True, stop=True), reads=['padT', 'cst32'], writes=['psE'])
                op('dve', lambda e: e.tensor_copy(out=padse[:], in_=psE[:, 0:128]), reads=['psE'], writes=['padse'])
                op('dve', lambda e: e.tensor_tensor(out=big[:, 0:NBLK * 64].rearrange("p (b x) -> p b x", x=64),
                                                    in0=padse[:, 64:128].unsqueeze(1).to_broadcast([128, NBLK, 64]),
                                                    in1=cst32[:, BLKS].unsqueeze(2).to_broadcast([128, NBLK, 64]), op=ALU.is_le),
                   reads=['padse', 'cst32'], writes=['big'])
                op('dve', lambda e: e.tensor_reduce(out=blke_f[:], in_=big[:, 0:NBLK * 64].rearrange("p (b x) -> p b x", x=64), axis=AX.X, op=ALU.add),
                   reads=['big'], writes=['blke_f'])
                op('dve', lambda e: e.tensor_scalar(out=blke_f[:], in0=blke_f[:], scalar1=63.0, scalar2=None, op0=ALU.min), reads=['blke_f'], writes=['blke_f'])
                op('dve', lambda e: e.tensor_scalar(out=blke_f[:], in0=blke_f[:], scalar1=128.0, scalar2=cst32[:, 960:961], op0=ALU.mult, op1=ALU.add),
                   reads=['blke_f', 'cst32'], writes=['blke_f'])
                op('dve', lambda e: e.tensor_scalar(out=blke_f[:], in0=blke_f[:], scalar1=float(l * 64 * 128), scalar2=None, op0=ALU.add), reads=['blke_f'], writes=['blke_f'])
                op('dve', lambda e: e.tensor_copy(out=widx_i[:], in_=blke_f[:]), reads=['blke_f'], writes=['widx_i'])
                op('dve', lambda e: e.tensor_tensor(out=big[:, 0:NT * 2 * 64].rearrange("p (b x) -> p b x", x=64),
                                                    in0=cst32[:, IOTA].unsqueeze(1).to_broadcast([128, NT * 2, 64]),
                                                    in1=ek[:, :].unsqueeze(2).to_broadcast([128, NT * 2, 64]), op=ALU.is_equal),
                   reads=['big', 'cst32'] + ['ek%d_%d' % (tt, k) for tt in range(NT) for k in range(2)], writes=['big'])
                op('dve', lambda e: e.tensor_tensor(out=big[:, 0:NT * 2 * 64].rearrange("p (b x) -> p b x", x=64),
                                                    in0=big[:, 0:NT * 2 * 64].rearrange("p (b x) -> p b x", x=64),
                                                    in1=padse[:, 0:64].unsqueeze(1).to_broadcast([128, NT * 2, 64]), op=ALU.mult),
                   reads=['big', 'padse'], writes=['big'])
                op('dve', lambda e: e.tensor_reduce(out=dest_f[:], in_=big[:, 0:NT * 2 * 64].rearrange("p (b x) -> p b x", x=64), axis=AX.X, op=ALU.add),
                   reads=['big'], writes=['dest_f'])
                op('dve', lambda e: e.tensor_tensor(out=dest_f[:], in0=dest_f[:], in1=rank[:], op=ALU.add),
                   reads=['dest_f'] + ['rank%d_%d' % (tt, k) for tt in range(NT) for k in range(2)], writes=['dest_f'])
                op('dve', lambda e: e.tensor_copy(out=dest_i[:], in_=dest_f[:]), reads=['dest_f'], writes=['dest_i'])
                for tt in range(NT):
                    for k in range(2):
                        op('pool', lambda e, tt=tt, k=k: e.indirect_dma_start(out=XB[:, :], out_offset=bass.IndirectOffsetOnAxis(ap=dest_i[:, tt * 2 + k:tt * 2 + k + 1], axis=0),
                                                                              in_=hn2all[:, tt, :], in_offset=None),
                           reads=['dest_i', 'hn2_%d' % tt], writes=['XBs%d_%d' % (tt, k)], dma=True)
                S.flush()
            if stop_after == 'p3':
                break

            with ExitStack() as es:
                def sb(name, shape, dt):
                    return es.enter_context(nc.sbuf_tensor(("L%d_p4_" % l) + name, shape, dt))

                def pst(name, shape, dt):
                    return es.enter_context(nc.psum_tensor(("L%d_p4_" % l) + name, shape, dt))
                NWB = 3
                w1b = [sb("w1b%d" % i, [128, 4096], BF16) for i in range(NWB)]
                w3b = [sb("w3b%d" % i, [128, 4096], BF16) for i in range(NWB)]
                w2b = [sb("w2b%d" % i, [128, 4096], BF16) for i in range(NWB)]
                xb = [sb("xb%d" % i, [128, 2, D], BF16) for i in range(2)]
                xT = [sb("xT%d" % i, [128, 8, MB], BF16) for i in range(2)]
                hT = [sb("hT%d" % i, [128, 4, MB], BF16) for i in range(2)]
                s1 = [sb("s1_%d" % i, [128, MB], F32) for i in range(2)]
                yst = [sb("yst%d" % i, [128, D], F32) for i in range(2)]
                psX = [pst("psX%d" % i, [128, D], BF16) for i in range(2)]
                psH = [pst("psH%d" % i, [128, 512], F32) for i in range(2)]
                psY = [pst("psY%d" % i, [128, 512], F32) for i in range(3)]
                w1v = w1.rearrange("l e (p kc) f -> (l e p) (kc f)", kc=8)
                w3v = w3.rearrange("l e (p kc) f -> (l e p) (kc f)", kc=8)
                w2v = w2.rearrange("l e (p fc) d -> (l e p) (fc d)", fc=4)
                ycnt = 0
                for blk in range(NBLK):
                    b = blk % 2
                    wb = blk % NWB
                    for (wv, wt, nm) in ((w1v, w1b[wb], 'w1b%d' % wb), (w3v, w3b[wb], 'w3b%d' % wb), (w2v, w2b[wb], 'w2b%d' % wb)):
                        op('pool', lambda e, wv=wv, wt=wt, blk=blk: e.indirect_dma_start(out=wt[:, :], out_offset=None, in_=wv[:, :],
                                                                                         in_offset=bass.IndirectOffsetOnAxis(ap=widx_i[:, blk:blk + 1], axis=0)),
                           reads=['widx_i'], writes=[nm], dma=True)
                    op('sp', lambda e, b=b, blk=blk: e.dma_start(out=xb[b][:], in_=XB[blk * MB:(blk + 1) * MB, :].rearrange("(s p) d -> p s d", p=128)),
                       writes=['xb%d' % b], dma=True)
                    w1b3 = w1b[wb][:, :].rearrange("p (kc f) -> p kc f", kc=8)
                    w3b3 = w3b[wb][:, :].rearrange("p (kc f) -> p kc f", kc=8)
                    w2b3 = w2b[wb][:, :].rearrange("p (fc d) -> p fc d", fc=4)
                    for s_ in range(2):
                        for kc in range(8):
                            op('pe', lambda e, b=b, s_=s_, kc=kc: e.transpose(out=psX[s_][:, kc * 128:(kc + 1) * 128], in_=xb[b][:, s_, kc:D:8], identity=cstb[:, IDENT]),
                               reads=['xb%d' % b, 'cstb'], writes=['psX%d' % s_])
                        eng = 'act' if s_ == 0 else 'dve'
                        if eng == 'act':
                            f = lambda e, b=b, s_=s_: e.activation(out=xT[b][:, :, s_ * 128:(s_ + 1) * 128], in_=psX[s_][:, :].rearrange("p (kc t) -> p kc t", kc=8), func=AF.Copy)
                        else:
                            f = lambda e, b=b, s_=s_: e.tensor_copy(out=xT[b][:, :, s_ * 128:(s_ + 1) * 128], in_=psX[s_][:, :].rearrange("p (kc t) -> p kc t", kc=8))
                        op(eng, f, reads=['psX%d' % s_], writes=['xT%d_%d' % (b, s_)])
                    XK = ['xT%d_0' % b, 'xT%d_1' % b]
                    for fc in range(4):
                        hb = fc % 2
                        for kc in range(8):
                            op('pe', lambda e, b=b, fc=fc, kc=kc, hb=hb, w1b3=w1b3: e.matmul(psH[hb][:, 0:MB], lhsT=w1b3[:, kc, fc:512:4], rhs=xT[b][:, kc, :], start=(kc == 0), stop=(kc == 7)),
                               reads=XK + ['w1b%d' % wb], writes=['psH%d' % hb])
                        for kc in range(8):
                            op('pe', lambda e, b=b, fc=fc, kc=kc, hb=hb, w3b3=w3b3: e.matmul(psH[hb][:, MB:2 * MB], lhsT=w3b3[:, kc, fc:512:4], rhs=xT[b][:, kc, :], start=(kc == 0), stop=(kc == 7)),
                               reads=XK + ['w3b%d' % wb], writes=['psH%d' % hb])
                        op('act', lambda e, hb=hb: e.activation(out=s1[hb][:], in_=psH[hb][:, 0:MB], func=AF.Silu), reads=['psH%d' % hb], writes=['s1_%d' % hb])
                        op('dve', lambda e, b=b, hb=hb, fc=fc: e.tensor_tensor(out=hT[b][:, fc, :], in0=s1[hb][:], in1=psH[hb][:, MB:2 * MB], op=ALU.mult),
                           reads=['s1_%d' % hb, 'psH%d' % hb], writes=['hT%d_%d' % (b, fc)])
                    HKs = ['hT%d_%d' % (b, fc) for fc in range(4)]
                    for s_ in range(2):
                        for half in range(2):
                            yb_ = ycnt % 3
                            ycnt += 1
                            for fc in range(4):
                                op('pe', lambda e, b=b, s_=s_, half=half, fc=fc, yb_=yb_, w2b3=w2b3: e.matmul(psY[yb_][:, :], lhsT=hT[b][:, fc, s_ * 128:(s_ + 1) * 128],
                                                                                                               rhs=w2b3[:, fc, half * 512:(half + 1) * 512], start=(fc == 0), stop=(fc == 3)),
                                   reads=HKs + ['w2b%d' % wb], writes=['psY%d' % yb_])
                            if half == 0:
                                op('act', lambda e, s_=s_, yb_=yb_: e.activation(out=yst[s_][:, 0:512], in_=psY[yb_][:, :], func=AF.Copy), reads=['psY%d' % yb_], writes=['yst%d_0' % s_])
                            else:
                                op('dve', lambda e, s_=s_, yb_=yb_: e.tensor_copy(out=yst[s_][:, 512:1024], in_=psY[yb_][:, :]), reads=['psY%d' % yb_], writes=['yst%d_1' % s_])
                        op('sp', lambda e, s_=s_, blk=blk: e.dma_start(out=YB[blk * MB + s_ * 128: blk * MB + (s_ + 1) * 128, :], in_=yst[s_][:]),
                           reads=['yst%d_0' % s_, 'yst%d_1' % s_], writes=['YBrow'], dma=True)
                S.flush()
            if stop_after == 'p4':
                break

            last = (l == L - 1)
            with ExitStack() as es:
                def sb(name, shape, dt):
                    return es.enter_context(nc.sbuf_tensor(("L%d_p5_" % l) + name, shape, dt))
                g2rep = sb("g2rep", [128, 3, D], F32)
                gfin = sb("gfin", [128, D], F32)
                y0 = [sb("y0_%d" % i, [128, D], F32) for i in range(2)]
                y1 = [sb("y1_%d" % i, [128, D], F32) for i in range(2)]
                h5 = [sb("h5_%d" % i, [128, D], F32) for i in range(2)]
                junk = sb("junk", [128, D], BF16)
                st = [sb("st%d" % i, [128, 4], F32) for i in range(2)]
                for ms in range(3):
                    op('sp', lambda e, ms=ms: e.dma_start(out=g2rep[:, ms, :], in_=MS[l, ms:ms + 1, 5 * D:6 * D].to_broadcast([128, D])), writes=['g2rep'], dma=True)
                op('sp', lambda e: e.dma_start(out=gfin[:], in_=g_final[0:1, :].to_broadcast([128, D])), writes=['gfin'], dma=True)
                for tt in range(NT):
                    s_, j = divmod(tt, TPS)
                    if last and j < 2:
                        continue
                    b = tt % 2
                    r0 = tt * 128
                    ms = mset(tt)
                    op('pool', lambda e, b=b, tt=tt: e.indirect_dma_start(out=y0[b][:, :], out_offset=None, in_=YB[:, :],
                                                                          in_offset=bass.IndirectOffsetOnAxis(ap=dest_i[:, tt * 2:tt * 2 + 1], axis=0)),
                       writes=['y0_%d' % b], dma=True)
                    op('pool', lambda e, b=b, tt=tt: e.indirect_dma_start(out=y1[b][:, :], out_offset=None, in_=YB[:, :],
                                                                          in_offset=bass.IndirectOffsetOnAxis(ap=dest_i[:, tt * 2 + 1:tt * 2 + 2], axis=0)),
                       writes=['y1_%d' % b], dma=True)
                    op('sp', lambda e, b=b, r0=r0: e.dma_start(out=h5[b][:], in_=H[r0:r0 + 128, :]), writes=['h5_%d' % b], dma=True)
                    op('dve', lambda e, b=b, tt=tt: e.tensor_scalar(out=y0[b][:], in0=y0[b][:], scalar1=gate_f[:, tt * 2:tt * 2 + 1], scalar2=None, op0=ALU.mult),
                       reads=['y0_%d' % b], writes=['y0_%d' % b])
                    op('dve', lambda e, b=b, tt=tt: e.scalar_tensor_tensor(out=y0[b][:], in0=y1[b][:], scalar=gate_f[:, tt * 2 + 1:tt * 2 + 2], in1=y0[b][:], op0=ALU.mult, op1=ALU.add),
                       reads=['y0_%d' % b, 'y1_%d' % b], writes=['y0_%d' % b])
                    op('pool', lambda e, b=b, ms=ms: e.tensor_tensor(out=y0[b][:], in0=y0[b][:], in1=g2rep[:, ms, :], op=ALU.mult), reads=['y0_%d' % b, 'g2rep'], writes=['y0_%d' % b])
                    op('pool', lambda e, b=b: e.tensor_tensor(out=h5[b][:], in0=h5[b][:], in1=y0[b][:], op=ALU.add), reads=['y0_%d' % b, 'h5_%d' % b], writes=['h5_%d' % b])
                    if not last:
                        op('sp', lambda e, b=b, r0=r0: e.dma_start(out=H[r0:r0 + 128, :], in_=h5[b][:]), reads=['h5_%d' % b], writes=['Hrow%d' % tt], dma=True)
                    else:
                        op('dve', lambda e, b=b: e.memset(st[b][:, 0:1], 0.0), writes=['ss%d' % b])
                        op('act', lambda e, b=b: e.activation(out=junk[:], in_=h5[b][:], func=AF.Square, accum_out=st[b][:, 0:1]),
                           reads=['h5_%d' % b, 'ss%d' % b], writes=['junk', 'ss%d' % b])
                        rstd_from_ss(st[b][:, 0:1], st[b][:, 1:2], 'ss%d' % b, 'rs%d' % b, D)
                        op('dve', lambda e, b=b: e.tensor_scalar(out=h5[b][:], in0=h5[b][:], scalar1=st[b][:, 1:2], scalar2=None, op0=ALU.mult),
                           reads=['h5_%d' % b, 'rs%d' % b], writes=['h5_%d' % b])
                        op('pool', lambda e, b=b: e.tensor_tensor(out=h5[b][:], in0=h5[b][:], in1=gfin[:], op=ALU.mult), reads=['h5_%d' % b, 'gfin'], writes=['h5_%d' % b])
                        orow = s_ * SEQ + (j - 2) * 128
                        op('sp', lambda e, b=b, orow=orow: e.dma_start(out=out[orow:orow + 128, :], in_=h5[b][:]), reads=['h5_%d' % b], writes=['orow%d' % tt], dma=True)
                S.flush()
            if stop_after == 'p5':
                break
    return nc


def _prep_inputs(inputs):
    f32 = np.float32
    x, c, ctx, c_ctx = inputs['x'], inputs['c'], inputs['ctx'], inputs['c_ctx']
    j = np.arange(128)
    ident = np.eye(128, dtype=f32)
    tri = (j[:, None] <= j[None, :]).astype(f32)
    trit = (j[:, None] >= j[None, :]).astype(f32)
    su = (j[:, None] > j[None, :]).astype(f32)
    slw = (j[:, None] < j[None, :]).astype(f32)
    ones = np.ones((128, 128), f32)
    iota = np.broadcast_to(np.arange(64, dtype=f32)[None, :], (128, 64))
    blks = np.broadcast_to((np.arange(128, dtype=f32) * MB)[None, :], (128, 128))
    pidx = np.arange(128, dtype=f32)[:, None]
    pad = np.zeros((128, 1024 - 961), f32)
    cst = np.ascontiguousarray(np.concatenate([ident, tri, trit, su, slw, ones, iota, blks, pidx, pad], axis=1))
    shared = {
        'cst': cst,
        'w_mod': inputs['w_mod'],
        'b_mod3': np.ascontiguousarray(np.broadcast_to(inputs['b_mod'][:, None, :], (L, 3, 6 * D))),
        'gn1T': np.ascontiguousarray(inputs['g_norm1'].reshape(L, 8, 128).transpose(0, 2, 1)),
        'w_in': inputs['w_in'],
        'ln_v_g': inputs['ln_v_g'], 'ln_v_b': inputs['ln_v_b'],
        'w_spT': np.ascontiguousarray(inputs['w_sp'].transpose(0, 1, 3, 2)),
        'b_sp': np.ascontiguousarray(inputs['b_sp'].reshape(L, 512)),
        'w_gate_up': inputs['w_gate_up'], 'b_gate': inputs['b_gate'],
        'g_glaT': np.ascontiguousarray(inputs['g_gla'].reshape(L, 4, 128).transpose(0, 2, 1)),
        'w_out': inputs['w_out'], 'g_norm2': inputs['g_norm2'],
        'w_r': np.ascontiguousarray(np.concatenate([inputs['w_router_g'], inputs['w_router_e']], axis=2)),
        'b_r': np.ascontiguousarray(np.concatenate([inputs['b_router_g'], inputs['b_router_e']], axis=1)),
        'w1': inputs['w1'], 'w3': inputs['w3'], 'w2': inputs['w2'],
        'g_final': np.ascontiguousarray(inputs['g_final'].reshape(1, D)),
    }
    in_maps = []
    for core in range(NCORES):
        rows, cm = [], []
        for s in range(NS):
            b = core * NS + s
            rows.append(ctx[b])
            rows.append(x[b])
            cm.append(c[b])
        cm.append(c_ctx)
        cmod = np.ascontiguousarray(np.stack(cm, axis=0).reshape(3, 8, 128).transpose(2, 1, 0)).astype(f32)
        m = dict(shared)
        m['hin'] = np.ascontiguousarray(np.concatenate(rows, axis=0)).astype(f32)
        m['cmod'] = cmod
        in_maps.append(m)
    return in_maps


def kernel(**inputs):
    inputs = {k: np.asarray(v) for k, v in inputs.items()}
    in_maps = _prep_inputs(inputs)
    nc = build_program()
    res = run_bass_kernel_spmd(nc, in_maps, core_ids=list(range(NCORES)))
    outs = [r["out"].reshape(NS, SEQ, D) for r in res.results]
    return np.concatenate(outs, axis=0).astype(np.float32)
```

```python
import types
import numpy as np
from contextlib import ExitStack
import concourse.bass as bass
import concourse.mybir as mybir
from concourse.bass_utils import run_bass_kernel_spmd

F32 = mybir.dt.float32
BF16 = mybir.dt.bfloat16
I32 = mybir.dt.int32
AF = mybir.ActivationFunctionType
ALU = mybir.AluOpType
AX = mybir.AxisListType

NCORES = 8
L = 2
D = 1024
DIN = 2592
NS = 2
LC = 256
SEQ = 2048
TPS = 18
NT = NS * TPS
T = NT * 128
MB = 256
NBLK = (2 * T) // MB + 64
NSLOT = NBLK * MB
EPS = 1e-6
OFF_AU, OFF_AV, OFF_BQ, OFF_BR, OFF_BK, OFF_BV, OFF_BG = 0, 512, 1024, 1280, 1792, 2048, 2560

SAME_ENGINE_SYNC = True
_DBG = {}


def _freeze(fn):
    if fn.__closure__ is None:
        return fn
    cells = []
    for c in fn.__closure__:
        try:
            cells.append(types.CellType(c.cell_contents))
        except ValueError:
            cells.append(c)
    return types.FunctionType(fn.__code__, fn.__globals__, fn.__name__, fn.__defaults__, tuple(cells))


class Sched:
    ENGS = ['pe', 'act', 'dve', 'pool', 'sp']

    def __init__(self, nc, es, n_dma_sems=(('sp', 16), ('act', 4), ('pool', 16))):
        self.nc = nc
        self.prog = {e: [] for e in self.ENGS}
        self.esem = {e: es.enter_context(nc.semaphore('sem_' + e)) for e in self.ENGS}
        self.ecount = {e: 0 for e in self.ENGS}
        self.seen = {e: {} for e in self.ENGS}
        self.res = {}
        self.dpool, self.dnext, self.dcount, self.semobj = {}, {}, {}, {}
        for e in self.ENGS:
            self.semobj['E' + e] = self.esem[e]
        for e, n in n_dma_sems:
            self.dpool[e] = []
            for i in range(n):
                key = 'D%s%d' % (e, i)
                self.semobj[key] = es.enter_context(nc.semaphore('dsem_%s%d' % (e, i)))
                self.dpool[e].append(key)
                self.dcount[key] = 0
            self.dnext[e] = 0
        self.nops = 0
        self.nwaits = 0

    def _wait(self, eng, tok):
        s, v = tok
        if self.seen[eng].get(s, 0) >= v:
            return
        self.seen[eng][s] = v
        self.prog[eng].append(('wait', s, v))
        self.nwaits += 1

    def op(self, eng, fn, reads=(), writes=(), dma=False, sig=True):
        if _DBG.get('maxops') and self.nops >= _DBG['maxops']:
            return None
        fn = _freeze(fn)
        writes = list(writes) + [r for r in reads if r.startswith('ps') and r not in writes]
        deps = []
        for r in reads:
            st = self.res.get(r)
            if st is not None and st['w'] is not None:
                deps.append(st['w'])
        for w in writes:
            st = self.res.get(w)
            if st is not None:
                if st['w'] is not None:
                    deps.append(st['w'])
                deps.extend(st['r'])
        if dma:
            pool = self.dpool[eng]
            key = pool[self.dnext[eng] % len(pool)]
            self.dnext[eng] += 1
            cnt = self.dcount[key]
            if cnt > 0:
                deps.append((key, cnt))
            self.dcount[key] = cnt + 16
            tok = (key, cnt + 16)
            inc = 16
        elif not sig:
            key = 'E' + eng
            tok = (key, self.ecount[eng] + 1)
            inc = 0
        else:
            key = 'E' + eng
            self.ecount[eng] += 1
            tok = (key, self.ecount[eng])
            inc = 1
        own = 'E' + eng
        for d in deps:
            if d[0] == own and (eng == 'pe' or not SAME_ENGINE_SYNC):
                continue
            self._wait(eng, d)
        self.prog[eng].append(('op', fn, key, inc))
        self.nops += 1
        for r in reads:
            st = self.res.setdefault(r, {'w': None, 'r': []})
            st['r'].append(tok)
        for w in writes:
            self.res[w] = {'w': tok, 'r': []}
        return tok

    def barrier(self):
        for e in self.ENGS:
            for key, cnt in self.dcount.items():
                if cnt > 0:
                    self._wait(e, (key, cnt))
            for e2 in self.ENGS:
                if e2 != e and self.ecount[e2] > 0:
                    self._wait(e, ('E' + e2, self.ecount[e2]))
        self.res = {}

    def flush(self):
        if _DBG.get('verbose'):
            print('flush: nops', self.nops, 'nwaits', self.nwaits, flush=True)
        self.barrier()
        nc = self.nc
        with nc.Block() as block:
            def run(e):
                def f(eng):
                    for it in self.prog[e]:
                        if it[0] == 'wait':
                            eng.wait_ge(self.semobj[it[1]], it[2])
                        else:
                            ins = it[1](eng)
                            if it[3]:
                                ins.then_inc(self.semobj[it[2]], it[3])
                return f
            block.tensor(run('pe'))
            block.scalar(run('act'))
            block.vector(run('dve'))
            block.gpsimd(run('pool'))
            block.sync(run('sp'))
        self.prog = {e: [] for e in self.ENGS}


def build_program(dbg=False, stop_after=None):
    nc = bass.Bass("TRN2", target_bir_lowering=False)

    def din(name, shape, dt=F32):
        return nc.dram_tensor(name, list(shape), dt, kind="ExternalInput").ap()

    def dscr(name, shape, dt):
        kind = "ExternalOutput" if dbg else "Internal"
        return nc.dram_tensor(name, list(shape), dt, kind=kind).ap()

    hin = din("hin", [T, D])
    cmod = din("cmod", [128, 8, 3])
    cst = din("cst", [128, 1024])
    w_mod = din("w_mod", [L, D, 6 * D])
    b_mod3 = din("b_mod3", [L, 3, 6 * D])
    gn1T = din("gn1T", [L, 128, 8])
    w_in = din("w_in", [L, D, DIN])
    ln_g = din("ln_v_g", [L, 512])
    ln_b = din("ln_v_b", [L, 512])
    w_spT = din("w_spT", [L, 4, 128, 128])
    b_sp = din("b_sp", [L, 512])
    w_gu = din("w_gate_up", [L, 2, 16, 256])
    b_gate = din("b_gate", [L, 2, 256])
    g_glaT = din("g_glaT", [L, 128, 4])
    w_out = din("w_out", [L, D, D])
    gn2 = din("g_norm2", [L, D])
    w_r = din("w_r", [L, D, 72])
    b_r = din("b_r", [L, 72])
    EW = 1 if stop_after in ("p0", "p1", "p2", "p3") else 64
    w1 = din("w1", [L, EW, D, 512])
    w3 = din("w3", [L, EW, D, 512])
    w2 = din("w2", [L, EW, 512, D])
    g_final = din("g_final", [1, D])
    out = nc.dram_tensor("out", [NS * SEQ, D], F32, kind="ExternalOutput").ap()

    H = dscr("H", [T, D], F32)
    MS = dscr("MS", [L, 3, 6 * D], F32)
    UT = dscr("UT", [4, 128, T], BF16)
    QT = dscr("QT", [2, 128, T], BF16)
    KT = dscr("KT", [2, 128, T], BF16)
    RT = dscr("RT", [4, 128, T], BF16)
    LA = dscr("LA", [T, 512], F32)
    Vd = dscr("Vd", [T, 512], BF16)
    Kd = dscr("Kd", [T, 256], BF16)
    VA = dscr("VA", [T, 512], BF16)
    XB = dscr("XB", [NSLOT, D], BF16)
    YB = dscr("YB", [NSLOT, D], F32)

    top = ExitStack()
    with top:
        S = Sched(nc, top)
        op = S.op

        def mset(tile_idx):
            s, j = divmod(tile_idx, TPS)
            return 2 if j < 2 else s

        def psb(name, shape, dt):
            return top.enter_context(nc.sbuf_tensor(name, shape, dt))
        cst32 = psb("cst32", [128, 1024], F32)
        cstb = psb("cstb", [128, 768], BF16)
        widx_i = psb("widx_i", [128, NBLK], I32)
        zt = psb("zt", [128, 4, D], BF16)
        dest_i = psb("dest_i", [128, NT * 2], I32)
        gate_f = psb("gate_f", [128, NT * 2], F32)
        IDENT, TRI, TRIT, SU, SLW, ONES = [slice(i * 128, (i + 1) * 128) for i in range(6)]
        op('sp', lambda e: e.dma_start(out=cst32[:], in_=cst[:, :]), writes=['cst32'], dma=True)
        op('dve', lambda e: e.tensor_copy(out=cstb[:], in_=cst32[:, 0:768]), reads=['cst32'], writes=['cstb'])
        op('dve', lambda e: e.memset(zt[:], 0.0), writes=['zt'])
        for zi in range(NSLOT // 512):
            op('sp', lambda e, zi=zi: e.dma_start(out=XB[zi * 512:(zi + 1) * 512, :].rearrange("(s p) d -> p s d", p=128), in_=zt[:]), reads=['zt'], writes=['XBz'], dma=True)
        S.flush()

        def rstd_from_ss(ss, rs, key_ss, key_rs, n):
            op('dve', lambda e: e.tensor_scalar(out=rs, in0=ss, scalar1=1.0 / n, scalar2=EPS, op0=ALU.mult, op1=ALU.add),
               reads=[key_ss], writes=[key_rs])
            op('act', lambda e: e.activation(out=rs, in_=rs, func=AF.Sqrt), reads=[key_rs], writes=[key_rs])
            op('dve', lambda e: e.reciprocal(out=rs, in_=rs), reads=[key_rs], writes=[key_rs])

        for l in range(L):
            Hsrc = hin if l == 0 else H
            with ExitStack() as es:
                def sb(name, shape, dt):
                    return es.enter_context(nc.sbuf_tensor(("L%d_p0_" % l) + name, shape, dt))
                scT = sb("scT", [128, 8, 3], F32)
                wm = [sb("wm%d" % i, [128, 8, 512], F32) for i in range(2)]
                bm = [sb("bm%d" % i, [3, 512], F32) for i in range(2)]
                mo = [sb("mo%d" % i, [3, 512], F32) for i in range(2)]
                psM = [es.enter_context(nc.psum_tensor("L%d_p0_psM%d" % (l, i), [128, 512], F32)) for i in range(2)]
                op('sp', lambda e: e.dma_start(out=scT[:], in_=cmod[:, :, :]), writes=['scT'], dma=True)
                op('act', lambda e: e.activation(out=scT[:], in_=scT[:], func=AF.Silu), reads=['scT'], writes=['scT'])
                for cg in range(12 if not _DBG.get('nop0') else 0):
                    i = cg % 2
                    cs = slice(cg * 512, (cg + 1) * 512)
                    op('sp', lambda e, i=i, cs=cs: e.dma_start(out=wm[i][:], in_=w_mod[l, :, cs].rearrange("(kc p) c -> p kc c", p=128)),
                       writes=['wm%d' % i], dma=True)
                    op('sp', lambda e, i=i, cs=cs: e.dma_start(out=bm[i][:], in_=b_mod3[l, :, cs]), writes=['bm%d' % i], dma=True)
                    for kc in range(8):
                        op('pe', lambda e, i=i, kc=kc: e.matmul(psM[i][0:3, :], lhsT=scT[:, kc, :], rhs=wm[i][:, kc, :],
                                                               start=(kc == 0), stop=(kc == 7)),
                           sig=(kc == 7), reads=['scT', 'wm%d' % i], writes=['psM%d' % i])
                    op('dve', lambda e, i=i: e.tensor_tensor(out=mo[i][:], in0=psM[i][0:3, :], in1=bm[i][:], op=ALU.add),
                       reads=['psM%d' % i, 'bm%d' % i], writes=['mo%d' % i])
                    op('sp', lambda e, i=i, cs=cs: e.dma_start(out=MS[l, :, cs], in_=mo[i][:]), reads=['mo%d' % i], writes=['MS'], dma=True)
                S.flush()

            if stop_after == 'p0':
                break
            with ExitStack() as es:
                def sb(name, shape, dt):
                    return es.enter_context(nc.sbuf_tensor(("L%d_p1_" % l) + name, shape, dt))

                def pst(name, shape, dt):
                    return es.enter_context(nc.psum_tensor(("L%d_p1_" % l) + name, shape, dt))
                winb = sb("winb", [128, 8, DIN], BF16)
                wup = sb("wup", [64, 512], F32)
                A1T = sb("A1T", [128, 3, 8], F32)
                sh1T = sb("sh1T", [128, 3, 8], F32)
                g1t = sb("g1t", [128, 8], F32)
                lng = sb("lng", [128, 512], F32)
                lnb = sb("lnb", [128, 512], F32)
                zgT = sb("zgT", [64, 256], F32)
                ht = [sb("ht%d" % i, [128, D], F32) for i in range(2)]
                junk = sb("junk", [128, D], BF16)
                hs = [sb("hs%d" % i, [128, D], BF16) for i in range(2)]
                hnT = [sb("hnT%d" % i, [128, 8, 256], BF16) for i in range(2)]
                st = [sb("st%d" % i, [128, 24], F32) for i in range(2)]
                fo = [sb("fo%d" % i, [128, 256], BF16) for i in range(4)]
                g32 = [sb("g32_%d" % i, [128, 512], F32) for i in range(2)]
                sq32 = [sb("sq32_%d" % i, [128, 512], F32) for i in range(2)]
                vab = [sb("vab%d" % i, [128, 512], BF16) for i in range(2)]
                vtb = [sb("vtb%d" % i, [128, 512], BF16) for i in range(2)]
                ktb = [sb("ktb%d" % i, [128, 256], BF16) for i in range(2)]
                la32 = [sb("la32_%d" % i, [128, 512], F32) for i in range(2)]
                psT = [pst("psT%d" % i, [128, 1024], BF16) for i in range(2)]
                psF = [pst("psF%d" % i, [128, 512], F32) for i in range(3)]
                psK = [pst("psK%d" % i, [128, 512], F32) for i in range(3)]

                for kc in range(8):
                    op('pool', lambda e, kc=kc: e.dma_start(out=winb[:, kc, :], in_=w_in[l, kc * 128:(kc + 1) * 128, :]),
                       writes=['winb'], dma=True)
                op('dve', lambda e: e.memset(wup[:], 0.0), writes=['wup'])
                op('sp', lambda e: e.dma_start(out=wup[0:16, 0:256], in_=w_gu[l, 0, :, :]), reads=['wup'], writes=['wup0'], dma=True)
                op('sp', lambda e: e.dma_start(out=wup[16:32, 256:512], in_=w_gu[l, 1, :, :]), reads=['wup'], writes=['wup1'], dma=True)
                op('sp', lambda e: e.dma_start(out=wup[32:33, 0:256], in_=b_gate[l, 0:1, :]), reads=['wup'], writes=['wup2'], dma=True)
                op('sp', lambda e: e.dma_start(out=wup[32:33, 256:512], in_=b_gate[l, 1:2, :]), reads=['wup'], writes=['wup3'], dma=True)
                WUPK = ['wup', 'wup0', 'wup1', 'wup2', 'wup3']
                op('dve', lambda e: e.memset(zgT[:], 1.0), writes=['zgT'])
                op('sp', lambda e: e.dma_start(out=g1t[:], in_=gn1T[l, :, :]), writes=['g1t'], dma=True)
                op('sp', lambda e: e.dma_start(out=lng[:], in_=ln_g[l:l + 1, :].to_broadcast([128, 512])), writes=['lng'], dma=True)
                op('sp', lambda e: e.dma_start(out=lnb[:], in_=ln_b[l:l + 1, :].to_broadcast([128, 512])), writes=['lnb'], dma=True)
                for ms in range(3):
                    op('sp', lambda e, ms=ms: e.dma_start(out=sh1T[:, ms, :], in_=MS[l, ms, 0:D].rearrange("(kc p) -> p kc", p=128),
                                                           allow_slow_non_contiguous=True), reads=['MS'], writes=['sh1T%d' % ms], dma=True)
                    op('sp', lambda e, ms=ms: e.dma_start(out=A1T[:, ms, :], in_=MS[l, ms, D:2 * D].rearrange("(kc p) -> p kc", p=128),
                                                           allow_slow_non_contiguous=True), reads=['MS'], writes=['A1T%d' % ms], dma=True)
                    op('dve', lambda e, ms=ms: e.scalar_tensor_tensor(out=A1T[:, ms, :], in0=A1T[:, ms, :], scalar=1.0, in1=g1t[:],
                                                                      op0=ALU.add, op1=ALU.mult),
                       reads=['A1T%d' % ms, 'g1t'], writes=['A1T%d' % ms])

                NG = NT // 2 if not _DBG.get('ng') else _DBG['ng']
                fcnt = [0]
                for gi in range(NG):
                    gb = gi % 2
                    t0 = gi * 256
                    ms = mset(gi * 2)
                    for ti in range(2):
                        tt = gi * 2 + ti
                        b = tt % 2
                        r0 = tt * 128
                        op('sp', lambda e, b=b, r0=r0: e.dma_start(out=ht[b][:], in_=Hsrc[r0:r0 + 128, :]), reads=['H'], writes=['ht%d' % b], dma=True)
                        op('dve', lambda e, b=b: e.memset(st[b][:, 0:1], 0.0), writes=['ss%d' % b])
                        op('act', lambda e, b=b: e.activation(out=junk[:], in_=ht[b][:], func=AF.Square, accum_out=st[b][:, 0:1]),
                           reads=['ht%d' % b, 'ss%d' % b], writes=['junk', 'ss%d' % b])
                        rstd_from_ss(st[b][:, 0:1], st[b][:, 1:2], 'ss%d' % b, 'rs%d' % b, D)
                        op('dve', lambda e, b=b: e.tensor_scalar(out=hs[b][:], in0=ht[b][:], scalar1=st[b][:, 1:2], scalar2=None, op0=ALU.mult),
                           reads=['ht%d' % b, 'rs%d' % b], writes=['hs%d' % b])
                        for kc in range(8):
                            op('pe', lambda e, b=b, kc=kc: e.transpose(out=psT[b][:, kc * 128:(kc + 1) * 128], in_=hs[b][:, kc * 128:(kc + 1) * 128],
                                                                      identity=cstb[:, IDENT]),
                               reads=['hs%d' % b, 'cstb'], writes=['psT%d' % b])
                        for kc in range(8):
                            eng = 'act' if ti == 0 else 'dve'
                            if eng == 'act':
                                f = lambda e, b=b, kc=kc, ti=ti: e.activation(out=hnT[gb][:, kc, ti * 128:(ti + 1) * 128], in_=psT[b][:, kc * 128:(kc + 1) * 128],
                                                                               func=AF.Identity, scale=A1T[:, ms, kc:kc + 1], bias=sh1T[:, ms, kc:kc + 1])
                            else:
                                f = lambda e, b=b, kc=kc, ti=ti: e.tensor_scalar(out=hnT[gb][:, kc, ti * 128:(ti + 1) * 128], in0=psT[b][:, kc * 128:(kc + 1) * 128],
                                                                                  scalar1=A1T[:, ms, kc:kc + 1], scalar2=sh1T[:, ms, kc:kc + 1],
                                                                                  op0=ALU.mult, op1=ALU.add)
                            op(eng, f, reads=['psT%d' % b, 'A1T%d' % ms, 'sh1T%d' % ms], writes=['hnT%d_%d_%d' % (gb, ti, kc)])
                    HK = ['hnT%d_%d_%d' % (gb, ti, kc) for ti in range(2) for kc in range(8)]
                    fm = [(OFF_AU + 128 * i, UT, i, AF.Gelu, 1.0) for i in range(4)]
                    fm += [(OFF_BQ + 128 * i, QT, i, AF.Copy, 0.125) for i in range(2)]
                    fm += [(OFF_BR + 128 * i, RT, i, AF.Silu, 1.0) for i in range(4)]
                    fm += [(OFF_BK + 128 * i, KT, i, AF.Copy, 1.0) for i in range(2)]
                    for (c0, dst, ci, func, scl) in fm:
                        n = fcnt[0]
                        fcnt[0] += 1
                        pb, ph = (n // 2) % 3, n % 2
                        pk = 'psF%d' % pb
                        pv = psF[pb][:, ph * 256:(ph + 1) * 256]
                        fb = n % 4
                        for kc in range(8):
                            op('pe', lambda e, pv=pv, kc=kc, c0=c0: e.matmul(pv, lhsT=winb[:, kc, c0:c0 + 128], rhs=hnT[gb][:, kc, :],
                                                                            start=(kc == 0), stop=(kc == 7)),
                               sig=(kc == 7), reads=['winb'] + HK, writes=[pk])
                        op('act', lambda e, pv=pv, fb=fb, func=func, scl=scl: e.activation(out=fo[fb][:], in_=pv, func=func, scale=scl),
                           reads=[pk], writes=['fo%d' % fb])
                        op('sp', lambda e, fb=fb, dst=dst, ci=ci: e.dma_start(out=dst[ci, :, t0:t0 + 256], in_=fo[fb][:]),
                           reads=['fo%d' % fb], writes=['z'], dma=True)
                    n = fcnt[0]
                    fcnt[0] += 1
                    pb, ph = (n // 2) % 3, n % 2
                    pk = 'psF%d' % pb
                    pvg = psF[pb][0:32, ph * 256:(ph + 1) * 256]
                    for kc in range(8):
                        op('pe', lambda e, pvg=pvg, kc=kc: e.matmul(pvg, lhsT=winb[:, kc, OFF_BG:OFF_BG + 32], rhs=hnT[gb][:, kc, :],
                                                                    start=(kc == 0), stop=(kc == 7)),
                           sig=(kc == 7), reads=['winb'] + HK, writes=[pk])
                    op('dve', lambda e, pvg=pvg: e.tensor_copy(out=zgT[0:32, :], in_=pvg), reads=[pk], writes=['zgT'])
                    for ti in range(2):
                        tt = gi * 2 + ti
                        b = tt % 2
                        r0 = tt * 128
                        HKt = ['hnT%d_%d_%d' % (gb, ti, kc) for kc in range(8)]
                        pz, pkk, pvv = psK[0], psK[1], psK[2]
                        for kc in range(8):
                            op('pe', lambda e, kc=kc, ti=ti: e.matmul(pz[:, :], lhsT=hnT[gb][:, kc, ti * 128:(ti + 1) * 128], rhs=winb[:, kc, OFF_AV:OFF_AV + 512],
                                                                      start=(kc == 0), stop=(kc == 7)), sig=(kc == 7), reads=['winb'] + HKt, writes=['psK0'])
                        for kc in range(8):
                            op('pe', lambda e, kc=kc, ti=ti: e.matmul(pvv[:, :], lhsT=hnT[gb][:, kc, ti * 128:(ti + 1) * 128], rhs=winb[:, kc, OFF_BV:OFF_BV + 512],
                                                                      start=(kc == 0), stop=(kc == 7)), sig=(kc == 7), reads=['winb'] + HKt, writes=['psK2'])
                        op('act', lambda e, b=b: e.activation(out=g32[b][:], in_=pz[:, :], func=AF.Gelu), reads=['psK0'], writes=['g32_%d' % b])
                        op('dve', lambda e, b=b: e.tensor_copy(out=vtb[b][:], in_=pvv[:, :]), reads=['psK2'], writes=['vtb%d' % b])
                        op('sp', lambda e, b=b, r0=r0: e.dma_start(out=Vd[r0:r0 + 128, :], in_=vtb[b][:]), reads=['vtb%d' % b], writes=['z'], dma=True)
                        for kc in range(8):
                            op('pe', lambda e, kc=kc, ti=ti: e.matmul(pkk[:, 0:256], lhsT=hnT[gb][:, kc, ti * 128:(ti + 1) * 128], rhs=winb[:, kc, OFF_BK:OFF_BK + 256],
                                                                      start=(kc == 0), stop=(kc == 7)), sig=(kc == 7), reads=['winb'] + HKt, writes=['psK1'])
                        op('act', lambda e, b=b: e.activation(out=ktb[b][:], in_=pkk[:, 0:256], func=AF.Copy), reads=['psK1'], writes=['ktb%d' % b])
                        op('sp', lambda e, b=b, r0=r0: e.dma_start(out=Kd[r0:r0 + 128, :], in_=ktb[b][:]), reads=['ktb%d' % b], writes=['z'], dma=True)
                        g3 = g32[b][:, :].rearrange("p (g c) -> p g c", g=4)
                        s3 = sq32[b][:, :].rearrange("p (g c) -> p g c", g=4)
                        stb = st[b]
                        op('pool', lambda e, b=b: e.tensor_tensor(out=sq32[b][:], in0=g32[b][:], in1=g32[b][:], op=ALU.mult),
                           reads=['g32_%d' % b], writes=['sq32_%d' % b])
                        op('dve', lambda e, g3=g3, stb=stb: e.tensor_reduce(out=stb[:, 4:8], in_=g3, axis=AX.X, op=ALU.add), reads=['g32_%d' % b], writes=['ln1_%d' % b])
                        op('dve', lambda e, s3=s3, stb=stb: e.tensor_reduce(out=stb[:, 8:12], in_=s3, axis=AX.X, op=ALU.add), reads=['sq32_%d' % b], writes=['ln2_%d' % b])
                        op('dve', lambda e, stb=stb: e.tensor_scalar(out=stb[:, 4:8], in0=stb[:, 4:8], scalar1=1.0 / 128, scalar2=None, op0=ALU.mult),
                           reads=['ln1_%d' % b], writes=['ln1_%d' % b])
                        op('dve', lambda e, stb=stb: e.tensor_tensor(out=stb[:, 12:16], in0=stb[:, 4:8], in1=stb[:, 4:8], op=ALU.mult),
                           reads=['ln1_%d' % b], writes=['ln3_%d' % b])
                        op('dve', lambda e, stb=stb: e.scalar_tensor_tensor(out=stb[:, 8:12], in0=stb[:, 8:12], scalar=1.0 / 128, in1=stb[:, 12:16],
                                                                            op0=ALU.mult, op1=ALU.subtract),
                           reads=['ln2_%d' % b, 'ln3_%d' % b], writes=['ln2_%d' % b])
                        op('dve', lambda e, stb=stb: e.tensor_scalar(out=stb[:, 8:12], in0=stb[:, 8:12], scalar1=EPS, scalar2=None, op0=ALU.add),
                           reads=['ln2_%d' % b], writes=['ln2_%d' % b])
                        op('act', lambda e, stb=stb: e.activation(out=stb[:, 8:12], in_=stb[:, 8:12], func=AF.Sqrt), reads=['ln2_%d' % b], writes=['ln2_%d' % b])
                        op('dve', lambda e, stb=stb: e.reciprocal(out=stb[:, 8:12], in_=stb[:, 8:12]), reads=['ln2_%d' % b], writes=['ln2_%d' % b])
                        op('dve', lambda e, g3=g3, stb=stb: e.tensor_tensor(out=g3, in0=g3, in1=stb[:, 4:8].unsqueeze(2).to_broadcast([128, 4, 128]), op=ALU.subtract),
                           reads=['g32_%d' % b, 'ln1_%d' % b, 'sq32_%d' % b], writes=['g32_%d' % b])
                        op('dve', lambda e, g3=g3, stb=stb: e.tensor_tensor(out=g3, in0=g3, in1=stb[:, 8:12].unsqueeze(2).to_broadcast([128, 4, 128]), op=ALU.mult),
                           reads=['g32_%d' % b, 'ln2_%d' % b], writes=['g32_%d' % b])
                        op('pool', lambda e, b=b: e.tensor_tensor(out=g32[b][:], in0=g32[b][:], in1=lng[:], op=ALU.mult),
                           reads=['g32_%d' % b, 'lng'], writes=['g32_%d' % b])
                        op('pool', lambda e, b=b: e.tensor_tensor(out=vab[b][:], in0=g32[b][:], in1=lnb[:], op=ALU.add),
                           reads=['g32_%d' % b, 'lnb'], writes=['vab%d' % b])
                        op('sp', lambda e, b=b, r0=r0: e.dma_start(out=VA[r0:r0 + 128, :], in_=vab[b][:]), reads=['vab%d' % b], writes=['z'], dma=True)
                        op('pe', lambda e, ti=ti: e.matmul(pkk[:, :], lhsT=zgT[0:64, ti * 128:(ti + 1) * 128], rhs=wup[0:64, :], start=True, stop=True),
                           reads=['zgT'] + WUPK + ['ktb%d' % b], writes=['psK1'])
                        op('act', lambda e, b=b: e.activation(out=la32[b][:], in_=pkk[:, :], func=AF.Exp, scale=-1.0), reads=['psK1'], writes=['la32_%d' % b])
                        op('act', lambda e, b=b: e.activation(out=la32[b][:], in_=la32[b][:], func=AF.Ln, bias=1.0), reads=['la32_%d' % b], writes=['la32_%d' % b])
                        op('pool', lambda e, b=b: e.tensor_scalar(out=la32[b][:], in0=la32[b][:], scalar1=-1.0 / 16, scalar2=None, op0=ALU.mult),
                           reads=['la32_%d' % b], writes=['la32_%d' % b])
                        op('sp', lambda e, b=b, r0=r0: e.dma_start(out=LA[r0:r0 + 128, :], in_=la32[b][:]), reads=['la32_%d' % b], writes=['z'], dma=True)
                S.flush()
            if stop_after == 'p1':
                break
            with ExitStack() as es:
                def sb(name, shape, dt):
                    return es.enter_context(nc.sbuf_tensor(("L%d_p2_" % l) + name, shape, dt))

                def pst(name, shape, dt):
                    return es.enter_context(nc.psum_tensor(("L%d_p2_" % l) + name, shape, dt))
                woutb = sb("woutb", [128, 8, D], BF16)
                wspb = sb("wspb", [128, 4, 128], BF16)
                bsp = sb("bsp", [128, 512], F32)
                ggla = sb("ggla", [128, 4], F32)
                g1rep = sb("g1rep", [128, 3, D], F32)
                KVs = sb("KVs", [128, TPS * 4, 128], F32)
                dec = sb("dec", [128, TPS * 4], F32)
                Sst = sb("Sst", [128, 4, 128], F32)
                Sprev = sb("Sprev", [128, TPS * 4, 128], BF16)
                la = [sb("la%d" % i, [128, 512], F32) for i in range(2)]
                ktm = [sb("ktm%d" % i, [128, 256], BF16) for i in range(2)]
                vtm = [sb("vtm%d" % i, [128, 512], BF16) for i in range(2)]
                eB = [sb("eB%d" % i, [128, 512], F32) for i in range(2)]
                kd = [sb("kd%d" % i, [128, 512], BF16) for i in range(2)]
                qTt = [sb("qT%d" % i, [128, 2, 128], BF16) for i in range(2)]
                kTt = [sb("kT%d" % i, [128, 2, 128], BF16) for i in range(2)]
                rTt = [sb("rT%d" % i, [128, 4, 128], BF16) for i in range(2)]
                uTt = [sb("uT%d" % i, [128, 4, 128], BF16) for i in range(2)]
                vat = [sb("va%d" % i, [128, 512], BF16) for i in range(2)]
                hh = [sb("hh%d" % i, [128, D], F32) for i in range(2)]
                Ep = [sb("Ep%d" % i, [128, 512], F32) for i in range(2)]
                Em = [sb("Em%d" % i, [128, 512], F32) for i in range(2)]
                qeL = [sb("qeL%d" % i, [128, 512], BF16) for i in range(2)]
                qeH = [sb("qeH%d" % i, [128, 512], BF16) for i in range(2)]
                keL = [sb("keL%d" % i, [128, 512], BF16) for i in range(2)]
                keH = [sb("keH%d" % i, [128, 512], BF16) for i in range(2)]
                atf = [sb("atf%d" % i, [128, 512], BF16) for i in range(2)]
                atb = [sb("atb%d" % i, [128, 512], BF16) for i in range(2)]
                sq = [sb("sq%d" % i, [128, 512], BF16) for i in range(2)]
                rs = [sb("rs%d" % i, [128, 512], F32) for i in range(2)]
                tmp = [sb("tmp%d" % i, [128, 512], F32) for i in range(2)]
                tmp2 = [sb("tmp2%d" % i, [128, 512], F32) for i in range(2)]
                bo = [sb("bo%d" % i, [128, 512], BF16) for i in range(2)]
                ao = [sb("ao%d" % i, [128, 512], BF16) for i in range(2)]
                hm = [sb("hm%d" % i, [128, D], F32) for i in range(2)]
                psB = pst("psB", [128, 512], F32)
                psKV = [pst("psKV%d" % i, [128, 512], F32) for i in range(2)]
                psD = pst("psD", [128, 512], F32)
                psO = pst("psO", [128, 512], F32)
                psP = pst("psP", [128, 512], F32)
                psM = [pst("psM%d" % i, [128, 512], F32) for i in range(2)]

                for i in range(2):
                    for (tl, nm) in ((qeL, 'qeL'), (qeH, 'qeH'), (keL, 'keL'), (keH, 'keH')):
                        op('dve', lambda e, tl=tl, i=i: e.memset(tl[i][:], 0.0), writes=['%s%d' % (nm, i)])
                for kc in range(8):
                    op('pool', lambda e, kc=kc: e.dma_start(out=woutb[:, kc, :], in_=w_out[l, kc * 128:(kc + 1) * 128, :]), writes=['woutb%d' % kc], dma=True)
                op('sp', lambda e: e.dma_start(out=ggla[:], in_=g_glaT[l, :, :]), writes=['ggla'], dma=True)
                for h4 in range(4):
                    op('dve', lambda e, h4=h4: e.tensor_scalar(out=woutb[:, 4 + h4, :], in0=woutb[:, 4 + h4, :], scalar1=ggla[:, h4:h4 + 1], scalar2=None, op0=ALU.mult),
                       reads=['ggla', 'woutb%d' % (4 + h4)], writes=['woutb%d' % (4 + h4)])
                WOK = ['woutb%d' % kc for kc in range(8)]
                op('pool', lambda e: e.dma_start(out=wspb[:], in_=w_spT[l, :, :, :].rearrange("g q p -> q g p")), writes=['wspb'], dma=True)
                op('sp', lambda e: e.dma_start(out=bsp[:], in_=b_sp[l:l + 1, :].to_broadcast([128, 512])), writes=['bsp'], dma=True)
                for ms in range(3):
                    op('sp', lambda e, ms=ms: e.dma_start(out=g1rep[:, ms, :], in_=MS[l, ms:ms + 1, 2 * D:3 * D].to_broadcast([128, D])), writes=['g1rep'], dma=True)

                for s in range(NS):
                    for j in range(TPS):
                        tt = s * TPS + j
                        r0 = tt * 128
                        b = j % 2
                        op('sp', lambda e, b=b, r0=r0: e.dma_start(out=la[b][:], in_=LA[r0:r0 + 128, :]), writes=['la%d' % b], dma=True)
                        op('sp', lambda e, b=b, r0=r0: e.dma_start(out=ktm[b][:], in_=Kd[r0:r0 + 128, :]), writes=['ktm%d' % b], dma=True)
                        op('sp', lambda e, b=b, r0=r0: e.dma_start(out=vtm[b][:], in_=Vd[r0:r0 + 128, :]), writes=['vtm%d' % b], dma=True)
                        op('pe', lambda e, b=b: e.matmul(psB[:, 0:256], lhsT=cst32[:, SU], rhs=la[b][:, 0:256], start=True, stop=True), reads=['la%d' % b, 'cst32'], writes=['psB'])
                        op('pe', lambda e, b=b: e.matmul(psB[:, 256:512], lhsT=cst32[:, SLW], rhs=la[b][:, 256:512], start=True, stop=True), reads=['la%d' % b, 'cst32'], writes=['psB'])
                        op('act', lambda e, b=b: e.activation(out=eB[b][:], in_=psB[:, :], func=AF.Exp), reads=['psB'], writes=['eB%d' % b])
                        op('dve', lambda e, b=b: e.tensor_tensor(out=kd[b][:, :].rearrange("p (a c) -> p a c", a=2), in0=eB[b][:, :].rearrange("p (a c) -> p a c", a=2),
                                                                 in1=ktm[b][:, :].unsqueeze(1).to_broadcast([128, 2, 256]), op=ALU.mult),
                           reads=['eB%d' % b, 'ktm%d' % b], writes=['kd%d' % b])
                        for idx in range(4):
                            dr, hp = divmod(idx, 2)
                            pv = psKV[dr][:, hp * 256:(hp + 1) * 256]
                            op('pe', lambda e, b=b, pv=pv, dr=dr, hp=hp: e.matmul(pv, lhsT=kd[b][:, dr * 256 + hp * 128: dr * 256 + hp * 128 + 128],
                                                                                  rhs=vtm[b][:, hp * 256:(hp + 1) * 256], start=True, stop=True),
                               reads=['kd%d' % b, 'vtm%d' % b], writes=['psKV%d' % dr])
                            if dr == 0:
                                op('act', lambda e, pv=pv, j=j, idx=idx: e.activation(out=KVs[0:64, j * 4 + idx, :], in_=pv[0:64, 0:128], func=AF.Copy),
                                   reads=['psKV%d' % dr], writes=['KVa%d_%d' % (j, idx)])
                                op('act', lambda e, pv=pv, j=j, idx=idx: e.activation(out=KVs[64:128, j * 4 + idx, :], in_=pv[64:128, 128:256], func=AF.Copy),
                                   reads=['psKV%d' % dr], writes=['KVb%d_%d' % (j, idx)])
                            else:
                                op('dve', lambda e, pv=pv, j=j, idx=idx: e.tensor_copy(out=KVs[0:64, j * 4 + idx, :], in_=pv[0:64, 0:128]),
                                   reads=['psKV%d' % dr], writes=['KVa%d_%d' % (j, idx)])
                                op('dve', lambda e, pv=pv, j=j, idx=idx: e.tensor_copy(out=KVs[64:128, j * 4 + idx, :], in_=pv[64:128, 128:256]),
                                   reads=['psKV%d' % dr], writes=['KVb%d_%d' % (j, idx)])
                            op('pe', lambda e, b=b, idx=idx, dr=dr, hp=hp: e.matmul(psD[:, idx:idx + 1], lhsT=la[b][:, dr * 256 + hp * 128: dr * 256 + hp * 128 + 128],
                                                                                    rhs=cst32[:, 5 * 128:5 * 128 + 1], start=True, stop=True),
                               reads=['la%d' % b, 'cst32'], writes=['psD'])
                        op('act', lambda e, j=j: e.activation(out=dec[:, j * 4:(j + 1) * 4], in_=psD[:, 0:4], func=AF.Exp), reads=['psD'], writes=['dec%d' % j])
                    op('dve', lambda e: e.memset(Sst[:], 0.0), writes=['Sst0', 'Sst1', 'Sst2', 'Sst3'])
                    order_f = list(range(TPS))
                    order_b = [1, 0] + list(range(TPS - 1, 1, -1))
                    for dr, order in ((0, order_f), (1, order_b)):
                        for j in order:
                            for hp in range(2):
                                idx = dr * 2 + hp
                                op('pool', lambda e, j=j, idx=idx: e.tensor_copy(out=Sprev[:, j * 4 + idx, :], in_=Sst[:, idx, :]),
                                   reads=['Sst%d' % idx], writes=['Sprev%d_%d' % (j, idx)])
                                op('dve', lambda e, j=j, idx=idx: e.scalar_tensor_tensor(out=Sst[:, idx, :], in0=Sst[:, idx, :], scalar=dec[:, j * 4 + idx: j * 4 + idx + 1],
                                                                                         in1=KVs[:, j * 4 + idx, :], op0=ALU.mult, op1=ALU.add),
                                   reads=['Sst%d' % idx, 'dec%d' % j, 'KVa%d_%d' % (j, idx), 'KVb%d_%d' % (j, idx)], writes=['Sst%d' % idx])
                    for j in range(TPS):
                        tt = s * TPS + j
                        r0 = tt * 128
                        b = j % 2
                        ms = mset(tt)
                        op('sp', lambda e, b=b, r0=r0: e.dma_start(out=la[b][:], in_=LA[r0:r0 + 128, :]), writes=['la%d' % b], dma=True)
                        op('sp', lambda e, b=b, r0=r0: e.dma_start(out=vtm[b][:], in_=Vd[r0:r0 + 128, :]), writes=['vtm%d' % b], dma=True)
                        op('sp', lambda e, b=b, r0=r0: e.dma_start(out=qTt[b][:], in_=QT[:, :, r0:r0 + 128].rearrange("h p t -> p h t")), writes=['qT%d' % b], dma=True)
                        op('sp', lambda e, b=b, r0=r0: e.dma_start(out=kTt[b][:], in_=KT[:, :, r0:r0 + 128].rearrange("h p t -> p h t")), writes=['kT%d' % b], dma=True)
                        op('sp', lambda e, b=b, r0=r0: e.dma_start(out=rTt[b][:], in_=RT[:, :, r0:r0 + 128].rearrange("h p t -> p h t")), writes=['rT%d' % b], dma=True)
                        op('sp', lambda e, b=b, r0=r0: e.dma_start(out=uTt[b][:], in_=UT[:, :, r0:r0 + 128].rearrange("h p t -> p h t")), writes=['uT%d' % b], dma=True)
                        op('sp', lambda e, b=b, r0=r0: e.dma_start(out=vat[b][:], in_=VA[r0:r0 + 128, :]), writes=['va%d' % b], dma=True)
                        op('sp', lambda e, b=b, r0=r0: e.dma_start(out=hh[b][:], in_=Hsrc[r0:r0 + 128, :]), writes=['hh%d' % b], dma=True)
                        for idx in range(4):
                            dr, hp = divmod(idx, 2)
                            msk = TRI if dr == 0 else TRIT
                            op('pe', lambda e, b=b, idx=idx, dr=dr, hp=hp, msk=msk: e.matmul(psB[:, idx * 128:(idx + 1) * 128],
                                                                                             lhsT=la[b][:, dr * 256 + hp * 128: dr * 256 + hp * 128 + 128],
                                                                                             rhs=cst32[:, msk], start=True, stop=True),
                               reads=['la%d' % b, 'cst32'], writes=['psB'])
                        op('act', lambda e, b=b: e.activation(out=Ep[b][:], in_=psB[:, :], func=AF.Exp), reads=['psB'], writes=['Ep%d' % b])
                        op('act', lambda e, b=b: e.activation(out=Em[b][:], in_=psB[:, :], func=AF.Exp, scale=-1.0), reads=['psB'], writes=['Em%d' % b])
                        for (rows_, qd, kd_, sfx) in ((slice(0, 64), qeL, keL, 'L'), (slice(64, 128), qeH, keH, 'H')):
                            op('dve', lambda e, b=b, rows_=rows_, qd=qd: e.tensor_tensor(out=qd[b][rows_, :].rearrange("p (a c) -> p a c", a=2),
                                                                                         in0=Ep[b][rows_, :].rearrange("p (a c) -> p a c", a=2),
                                                                                         in1=qTt[b][rows_, :, :].rearrange("p h t -> p (h t)").unsqueeze(1).to_broadcast([64, 2, 256]), op=ALU.mult),
                               reads=['Ep%d' % b, 'qT%d' % b], writes=['qe%s%d' % (sfx, b)])
                            op('pool', lambda e, b=b, rows_=rows_, kd_=kd_: e.tensor_tensor(out=kd_[b][rows_, :].rearrange("p (a c) -> p a c", a=2),
                                                                                            in0=Em[b][rows_, :].rearrange("p (a c) -> p a c", a=2),
                                                                                            in1=kTt[b][rows_, :, :].rearrange("p h t -> p (h t)").unsqueeze(1).to_broadcast([64, 2, 256]), op=ALU.mult),
                               reads=['Em%d' % b, 'kT%d' % b], writes=['ke%s%d' % (sfx, b)])
                        for dr in range(2):
                            for h4 in range(4):
                                hp, par = divmod(h4, 2)
                                rows = slice(par * 64, (par + 1) * 64)
                                cs = slice((dr * 2 + hp) * 128, (dr * 2 + hp + 1) * 128)
                                kx = keL if par == 0 else keH
                                qx = qeL if par == 0 else qeH
                                sfx = 'L' if par == 0 else 'H'
                                op('pe', lambda e, b=b, dr=dr, h4=h4, cs=cs, kx=kx, qx=qx: e.matmul(psKV[dr][:, h4 * 128:(h4 + 1) * 128], lhsT=kx[b][:, cs], rhs=qx[b][:, cs],
                                                                                                    start=True, stop=True),
                                   reads=['ke%s%d' % (sfx, b), 'qe%s%d' % (sfx, b)], writes=['psKV%d' % dr])
                        op('dve', lambda e, b=b: e.tensor_tensor(out=atf[b][:, :].rearrange("p (a c) -> p a c", a=4), in0=psKV[0][:, :].rearrange("p (a c) -> p a c", a=4),
                                                                 in1=cst32[:, TRI].unsqueeze(1).to_broadcast([128, 4, 128]), op=ALU.mult),
                           reads=['psKV0', 'cst32'], writes=['atf%d' % b])
                        op('dve', lambda e, b=b: e.tensor_tensor(out=atb[b][:, :].rearrange("p (a c) -> p a c", a=4), in0=psKV[1][:, :].rearrange("p (a c) -> p a c", a=4),
                                                                 in1=cst32[:, TRIT].unsqueeze(1).to_broadcast([128, 4, 128]), op=ALU.mult),
                           reads=['psKV1', 'cst32'], writes=['atb%d' % b])
                        for h4 in range(4):
                            hp, par = divmod(h4, 2)
                            rows = slice(par * 64, (par + 1) * 64)
                            hs_ = slice(h4 * 128, (h4 + 1) * 128)
                            op('pe', lambda e, b=b, hs_=hs_: e.matmul(psO[:, hs_], lhsT=vtm[b][:, hs_], rhs=atf[b][:, hs_], start=True, stop=False),
                               reads=['vtm%d' % b, 'atf%d' % b], writes=['psO'])
                            op('pe', lambda e, b=b, hs_=hs_: e.matmul(psO[:, hs_], lhsT=vtm[b][:, hs_], rhs=atb[b][:, hs_], start=False, stop=False),
                               reads=['vtm%d' % b, 'atb%d' % b], writes=['psO'])
                            for dr in range(2):
                                cs = slice((dr * 2 + hp) * 128, (dr * 2 + hp + 1) * 128)
                                qx = qeL if par == 0 else qeH
                                sfx = 'L' if par == 0 else 'H'
                                op('pe', lambda e, b=b, hs_=hs_, cs=cs, dr=dr, hp=hp, j=j, qx=qx: e.matmul(psO[:, hs_], lhsT=Sprev[:, j * 4 + dr * 2 + hp, :], rhs=qx[b][:, cs],
                                                                                                           start=False, stop=(dr == 1)),
                                   reads=['Sprev%d_%d' % (j, dr * 2 + hp), 'qe%s%d' % (sfx, b)], writes=['psO'])
                        op('act', lambda e, b=b: e.activation(out=sq[b][:], in_=psO[:, :], func=AF.Square), reads=['psO'], writes=['sq%d' % b])
                        op('pe', lambda e, b=b: e.matmul(psD[:, :], lhsT=cstb[:, ONES], rhs=sq[b][:], start=True, stop=True), reads=['sq%d' % b, 'cstb'], writes=['psD'])
                        op('dve', lambda e, b=b: e.tensor_scalar(out=rs[b][:], in0=psD[:, :], scalar1=1.0 / 128, scalar2=EPS, op0=ALU.mult, op1=ALU.add), reads=['psD'], writes=['rs%d' % b])
                        op('act', lambda e, b=b: e.activation(out=rs[b][:], in_=rs[b][:], func=AF.Sqrt), reads=['rs%d' % b], writes=['rs%d' % b])
                        op('dve', lambda e, b=b: e.reciprocal(out=rs[b][:], in_=rs[b][:]), reads=['rs%d' % b], writes=['rs%d' % b])
                        op('dve', lambda e, b=b: e.tensor_tensor(out=tmp[b][:], in0=psO[:, :], in1=rs[b][:], op=ALU.mult), reads=['psO', 'rs%d' % b], writes=['tmp%d' % b])
                        op('pool', lambda e, b=b: e.tensor_tensor(out=bo[b][:], in0=tmp[b][:], in1=rTt[b][:, :, :].rearrange("p h t -> p (h t)"), op=ALU.mult),
                           reads=['tmp%d' % b, 'rT%d' % b], writes=['bo%d' % b])
                        for g4 in range(4):
                            gs = slice(g4 * 128, (g4 + 1) * 128)
                            op('pe', lambda e, b=b, gs=gs, g4=g4: e.matmul(psP[:, gs], lhsT=vat[b][:, gs], rhs=wspb[:, g4, :], start=True, stop=True),
                               reads=['va%d' % b, 'wspb'], writes=['psP'])
                        op('dve', lambda e, b=b: e.tensor_tensor(out=tmp2[b][:], in0=psP[:, :], in1=bsp[:], op=ALU.add), reads=['psP', 'bsp'], writes=['tmp2%d' % b])
                        op('pool', lambda e, b=b: e.tensor_tensor(out=ao[b][:], in0=tmp2[b][:], in1=uTt[b][:, :, :].rearrange("p h t -> p (h t)"), op=ALU.mult),
                           reads=['tmp2%d' % b, 'uT%d' % b], writes=['ao%d' % b])
                        for half in range(2):
                            for kc in range(8):
                                src = ao[b] if kc < 4 else bo[b]
                                ks = slice((kc % 4) * 128, (kc % 4 + 1) * 128)
                                op('pe', lambda e, half=half, kc=kc, src=src, ks=ks: e.matmul(psM[half][:, :], lhsT=src[:, ks], rhs=woutb[:, kc, half * 512:(half + 1) * 512],
                                                                                              start=(kc == 0), stop=(kc == 7)),
                                   sig=(kc == 7), reads=['ao%d' % b, 'bo%d' % b] + WOK, writes=['psM%d' % half])
                            op('dve', lambda e, b=b, half=half, ms=ms: e.tensor_tensor(out=hm[b][:, half * 512:(half + 1) * 512], in0=psM[half][:, :],
                                                                                       in1=g1rep[:, ms, half * 512:(half + 1) * 512], op=ALU.mult),
                               reads=['psM%d' % half, 'g1rep'], writes=['hm%d_%d' % (b, half)])
                        op('pool', lambda e, b=b: e.tensor_tensor(out=hm[b][:], in0=hm[b][:], in1=hh[b][:], op=ALU.add),
                           reads=['hm%d_0' % b, 'hm%d_1' % b, 'hh%d' % b], writes=['hm%d_0' % b, 'hm%d_1' % b])
                        op('sp', lambda e, b=b, r0=r0: e.dma_start(out=H[r0:r0 + 128, :], in_=hm[b][:]), reads=['hm%d_0' % b, 'hm%d_1' % b], writes=['Hrow%d' % tt], dma=True)
                S.flush()
            if stop_after == 'p2':
                break
            with ExitStack() as es:
                def sb(name, shape, dt):
                    return es.enter_context(nc.sbuf_tensor(("L%d_p3_" % l) + name, shape, dt))

                def pst(name, shape, dt):
                    return es.enter_context(nc.psum_tensor(("L%d_p3_" % l) + name, shape, dt))
                wrb = sb("wrb", [128, 8, 72], BF16)
                brep = sb("brep", [128, 72], F32)
                A2rep = sb("A2rep", [128, 3, D], F32)
                sh2rep = sb("sh2rep", [128, 3, D], F32)
                gn2rep = sb("gn2rep", [128, D], F32)
                hn2all = sb("hn2all", [128, NT, D], BF16)
                ek = sb("ek", [128, NT * 2], F32)
                rank = sb("rank", [128, NT * 2], F32)
                base = sb("base", [128, 64], F32)
                big = sb("big", [128, NBLK * 64], F32)
                h2 = [sb("h2_%d" % i, [128, D], F32) for i in range(2)]
                hs2 = [sb("hs2_%d" % i, [128, D], F32) for i in range(2)]
                junk = sb("junk", [128, D], BF16)
                hT2 = [sb("hT2_%d" % i, [128, D], BF16) for i in range(2)]
                st = [sb("st%d" % i, [128, 4], F32) for i in range(2)]
                Lg = [sb("Lg%d" % i, [128, 72], F32) for i in range(2)]
                R = [sb("R%d" % i, [128, 16], F32) for i in range(2)]
                eg = [sb("eg%d" % i, [128, 8], F32) for i in range(2)]
                ohg = [sb("ohg%d" % i, [128, 8], F32) for i in range(2)]
                sel3 = [sb("sel3_%d" % i, [128, 64], F32) for i in range(2)]
                lsel = [sb("lsel%d" % i, [128, 8], F32) for i in range(2)]
                oh1 = [sb("oh1_%d" % i, [128, 8], F32) for i in range(2)]
                oh2 = [sb("oh2_%d" % i, [128, 8], F32) for i in range(2)]
                l2 = [sb("l2_%d" % i, [128, 8], F32) for i in range(2)]
                oh64 = [[sb("oh64_%d_%d" % (k, i), [128, 64], F32) for i in range(2)] for k in range(2)]
                t64 = [sb("t64_%d" % i, [128, 64], F32) for i in range(2)]
                Abf = [sb("Abf%d" % i, [128, 64], BF16) for i in range(2)]
                pos = [sb("pos%d" % i, [128, 64], F32) for i in range(2)]
                cntT = sb("cntT", [64, 128], F32)
                cmpT = sb("cmpT", [64, 128], F32)
                nblk = sb("nblk", [64, 1], F32)
                padT = sb("padT", [64, 128], F32)
                padse = sb("padse", [128, 128], F32)
                blke_f = sb("blke_f", [128, NBLK], F32)
                dest_f = sb("dest_f", [128, NT * 2], F32)
                psT = [pst("psT%d" % i, [128, D], BF16) for i in range(2)]
                psL = pst("psL", [128, 512], F32)
                psC = pst("psC", [128, 512], F32)
                psCT = pst("psCT", [128, 512], F32)
                psE = pst("psE", [128, 512], F32)
                IOTA = slice(768, 832)
                BLKS = slice(832, 832 + NBLK)
                THR = slice(832, 960)

                op('pool', lambda e: e.dma_start(out=wrb[:], in_=w_r[l, :, :].rearrange("(kc p) c -> p kc c", p=128)), writes=['wrb'], dma=True)
                op('sp', lambda e: e.dma_start(out=brep[:], in_=b_r[l:l + 1, :].to_broadcast([128, 72])), writes=['brep'], dma=True)
                op('sp', lambda e: e.dma_start(out=gn2rep[:], in_=gn2[l:l + 1, :].to_broadcast([128, D])), writes=['gn2rep'], dma=True)
                for ms in range(3):
                    op('sp', lambda e, ms=ms: e.dma_start(out=sh2rep[:, ms, :], in_=MS[l, ms:ms + 1, 3 * D:4 * D].to_broadcast([128, D])), writes=['sh2rep%d' % ms], dma=True)
                    op('sp', lambda e, ms=ms: e.dma_start(out=A2rep[:, ms, :], in_=MS[l, ms:ms + 1, 4 * D:5 * D].to_broadcast([128, D])), writes=['A2rep%d' % ms], dma=True)
                    op('dve', lambda e, ms=ms: e.scalar_tensor_tensor(out=A2rep[:, ms, :], in0=A2rep[:, ms, :], scalar=1.0, in1=gn2rep[:], op0=ALU.add, op1=ALU.mult),
                       reads=['A2rep%d' % ms, 'gn2rep'], writes=['A2rep%d' % ms])
                op('dve', lambda e: e.memset(base[:], 0.0), writes=['base'])
                for tt in range(NT):
                    b = tt % 2
                    r0 = tt * 128
                    ms = mset(tt)
                    Rb, Lgb = R[b], Lg[b]
                    rk = 'R%d' % b
                    op('sp', lambda e, b=b, r0=r0: e.dma_start(out=h2[b][:], in_=H[r0:r0 + 128, :]), writes=['h2_%d' % b], dma=True)
                    op('dve', lambda e, b=b: e.memset(st[b][:, 0:1], 0.0), writes=['ss%d' % b])
                    op('act', lambda e, b=b: e.activation(out=junk[:], in_=h2[b][:], func=AF.Square, accum_out=st[b][:, 0:1]),
                       reads=['h2_%d' % b, 'ss%d' % b], writes=['junk', 'ss%d' % b])
                    rstd_from_ss(st[b][:, 0:1], st[b][:, 1:2], 'ss%d' % b, 'rs%d' % b, D)
                    op('dve', lambda e, b=b: e.tensor_scalar(out=hs2[b][:], in0=h2[b][:], scalar1=st[b][:, 1:2], scalar2=None, op0=ALU.mult),
                       reads=['h2_%d' % b, 'rs%d' % b], writes=['hs2_%d' % b])
                    op('pool', lambda e, b=b, ms=ms: e.tensor_tensor(out=hs2[b][:], in0=hs2[b][:], in1=A2rep[:, ms, :], op=ALU.mult),
                       reads=['hs2_%d' % b, 'A2rep%d' % ms], writes=['hs2_%d' % b])
                    op('pool', lambda e, b=b, ms=ms, tt=tt: e.tensor_tensor(out=hn2all[:, tt, :], in0=hs2[b][:], in1=sh2rep[:, ms, :], op=ALU.add),
                       reads=['hs2_%d' % b, 'sh2rep%d' % ms], writes=['hn2_%d' % tt])
                    for kc in range(8):
                        op('pe', lambda e, b=b, kc=kc, tt=tt: e.transpose(out=psT[b][:, kc * 128:(kc + 1) * 128], in_=hn2all[:, tt, kc * 128:(kc + 1) * 128], identity=cstb[:, IDENT]),
                           reads=['hn2_%d' % tt, 'cstb'], writes=['psT%d' % b])
                    op('act', lambda e, b=b: e.activation(out=hT2[b][:], in_=psT[b][:, :], func=AF.Copy), reads=['psT%d' % b], writes=['hT2_%d' % b])
                    for kc in range(8):
                        op('pe', lambda e, b=b, kc=kc: e.matmul(psL[:, 0:72], lhsT=hT2[b][:, kc * 128:(kc + 1) * 128], rhs=wrb[:, kc, :], start=(kc == 0), stop=(kc == 7)),
                           sig=(kc == 7), reads=['hT2_%d' % b, 'wrb'], writes=['psL'])
                    op('dve', lambda e, Lgb=Lgb: e.tensor_tensor(out=Lgb[:], in0=psL[:, 0:72], in1=brep[:], op=ALU.add), reads=['psL', 'brep'], writes=['Lg%d' % b])
                    LK = 'Lg%d' % b
                    op('dve', lambda e, Rb=Rb, Lgb=Lgb: e.tensor_reduce(out=Rb[:, 0:1], in_=Lgb[:, 0:8], axis=AX.X, op=ALU.max), reads=[LK], writes=[rk])
                    op('dve', lambda e, Rb=Rb: e.tensor_scalar(out=Rb[:, 1:2], in0=Rb[:, 0:1], scalar1=-1.0, scalar2=None, op0=ALU.mult), reads=[rk], writes=[rk])
                    op('dve', lambda e, Rb=Rb: e.memset(Rb[:, 2:3], 0.0), reads=[rk], writes=[rk])
                    op('act', lambda e, Rb=Rb, Lgb=Lgb, b=b: e.activation(out=eg[b][:], in_=Lgb[:, 0:8], func=AF.Exp, bias=Rb[:, 1:2], scale=1.0, accum_out=Rb[:, 2:3]),
                       reads=[LK, rk], writes=[rk, 'eg%d' % b])
                    op('dve', lambda e, Rb=Rb: e.reciprocal(out=Rb[:, 3:4], in_=Rb[:, 2:3]), reads=[rk], writes=[rk])
                    op('dve', lambda e, Rb=Rb, Lgb=Lgb, b=b: e.tensor_scalar(out=ohg[b][:], in0=Lgb[:, 0:8], scalar1=Rb[:, 0:1], scalar2=None, op0=ALU.is_equal),
                       reads=[LK, rk], writes=['ohg%d' % b])
                    op('dve', lambda e, Lgb=Lgb, b=b: e.tensor_tensor(out=sel3[b][:, :].rearrange("p (g x) -> p g x", g=8), in0=Lgb[:, 8:72].rearrange("p (g x) -> p g x", g=8),
                                                                      in1=ohg[b][:, :].unsqueeze(2).to_broadcast([128, 8, 8]), op=ALU.mult),
                       reads=[LK, 'ohg%d' % b], writes=['sel3_%d' % b])
                    op('dve', lambda e, b=b: e.tensor_reduce(out=lsel[b][:], in_=sel3[b][:, :].rearrange("p (g x) -> p x g", g=8), axis=AX.X, op=ALU.add),
                       reads=['sel3_%d' % b], writes=['lsel%d' % b])
                    op('dve', lambda e, Rb=Rb, b=b: e.tensor_reduce(out=Rb[:, 4:5], in_=lsel[b][:], axis=AX.X, op=ALU.max), reads=['lsel%d' % b, rk], writes=[rk])
                    op('dve', lambda e, Rb=Rb, b=b: e.tensor_scalar(out=oh1[b][:], in0=lsel[b][:], scalar1=Rb[:, 4:5], scalar2=None, op0=ALU.is_equal),
                       reads=['lsel%d' % b, rk], writes=['oh1_%d' % b])
                    op('dve', lambda e, b=b: e.scalar_tensor_tensor(out=l2[b][:], in0=oh1[b][:], scalar=-1e30, in1=lsel[b][:], op0=ALU.mult, op1=ALU.add),
                       reads=['oh1_%d' % b, 'lsel%d' % b], writes=['l2_%d' % b])
                    op('dve', lambda e, Rb=Rb, b=b: e.tensor_reduce(out=Rb[:, 5:6], in_=l2[b][:], axis=AX.X, op=ALU.max), reads=['l2_%d' % b, rk], writes=[rk])
                    op('dve', lambda e, Rb=Rb, b=b: e.tensor_scalar(out=oh2[b][:], in0=l2[b][:], scalar1=Rb[:, 5:6], scalar2=None, op0=ALU.is_equal),
                       reads=['l2_%d' % b, rk], writes=['oh2_%d' % b])
                    op('dve', lambda e, Rb=Rb: e.tensor_tensor(out=Rb[:, 6:7], in0=Rb[:, 5:6], in1=Rb[:, 4:5], op=ALU.subtract), reads=[rk], writes=[rk])
                    op('act', lambda e, Rb=Rb: e.activation(out=Rb[:, 7:8], in_=Rb[:, 6:7], func=AF.Exp), reads=[rk], writes=[rk])
                    op('dve', lambda e, Rb=Rb: e.tensor_scalar(out=Rb[:, 7:8], in0=Rb[:, 7:8], scalar1=1.0, scalar2=None, op0=ALU.add), reads=[rk], writes=[rk])
                    op('dve', lambda e, Rb=Rb: e.reciprocal(out=Rb[:, 7:8], in_=Rb[:, 7:8]), reads=[rk], writes=[rk])
                    op('dve', lambda e, Rb=Rb, tt=tt: e.tensor_tensor(out=gate_f[:, tt * 2:tt * 2 + 1], in0=Rb[:, 7:8], in1=Rb[:, 3:4], op=ALU.mult), reads=[rk], writes=['gate%d' % tt])
                    op('dve', lambda e, Rb=Rb, tt=tt: e.tensor_tensor(out=gate_f[:, tt * 2 + 1:tt * 2 + 2], in0=Rb[:, 3:4], in1=gate_f[:, tt * 2:tt * 2 + 1], op=ALU.subtract),
                       reads=[rk, 'gate%d' % tt], writes=['gate%d' % tt])
                    for k, ohk, ohkey in ((0, oh1, 'oh1_%d' % b), (1, oh2, 'oh2_%d' % b)):
                        op('dve', lambda e, b=b, k=k, ohk=ohk: e.tensor_tensor(out=oh64[k][b][:, :].rearrange("p (g x) -> p g x", g=8),
                                                                                in0=ohg[b][:, :].unsqueeze(2).to_broadcast([128, 8, 8]),
                                                                                in1=ohk[b][:, :].unsqueeze(1).to_broadcast([128, 8, 8]), op=ALU.mult),
                           reads=['ohg%d' % b, ohkey], writes=['oh64_%d_%d' % (k, b)])
                        op('dve', lambda e, b=b, k=k: e.tensor_tensor(out=t64[b][:], in0=oh64[k][b][:], in1=cst32[:, IOTA], op=ALU.mult),
                           reads=['oh64_%d_%d' % (k, b), 'cst32'], writes=['t64_%d' % b])
                        op('dve', lambda e, b=b, k=k, tt=tt: e.tensor_reduce(out=ek[:, tt * 2 + k:tt * 2 + k + 1], in_=t64[b][:], axis=AX.X, op=ALU.add),
                           reads=['t64_%d' % b], writes=['ek%d_%d' % (tt, k)])
                    op('dve', lambda e, b=b: e.tensor_tensor(out=Abf[b][:], in0=oh64[0][b][:], in1=oh64[1][b][:], op=ALU.add),
                       reads=['oh64_0_%d' % b, 'oh64_1_%d' % b], writes=['Abf%d' % b])
                    op('pe', lambda e, b=b: e.matmul(psC[:, 0:64], lhsT=cstb[:, SLW], rhs=Abf[b][:], start=True, stop=True), reads=['Abf%d' % b, 'cstb'], writes=['psC'])
                    op('pe', lambda e, b=b: e.matmul(psC[:, 64:128], lhsT=cstb[:, ONES], rhs=Abf[b][:], start=True, stop=True), reads=['Abf%d' % b, 'cstb'], writes=['psC'])
                    op('pe', lambda e, b=b, tt=tt: e.matmul(psCT[0:64, 0:128], lhsT=Abf[b][:], rhs=cstb[:, ONES], start=(tt == 0), stop=(tt == NT - 1)),
                       reads=['Abf%d' % b, 'cstb'], writes=['psCT'])
                    op('dve', lambda e, b=b: e.tensor_tensor(out=pos[b][:], in0=psC[:, 0:64], in1=base[:], op=ALU.add), reads=['psC', 'base'], writes=['pos%d' % b])
                    for k in range(2):
                        op('dve', lambda e, b=b, k=k: e.tensor_tensor(out=t64[b][:], in0=oh64[k][b][:], in1=pos[b][:], op=ALU.mult),
                           reads=['oh64_%d_%d' % (k, b), 'pos%d' % b], writes=['t64_%d' % b])
                        op('dve', lambda e, b=b, k=k, tt=tt: e.tensor_reduce(out=rank[:, tt * 2 + k:tt * 2 + k + 1], in_=t64[b][:], axis=AX.X, op=ALU.add),
                           reads=['t64_%d' % b], writes=['rank%d_%d' % (tt, k)])
                    op('dve', lambda e: e.tensor_tensor(out=base[:], in0=psC[:, 64:128], in1=base[:], op=ALU.add), reads=['psC', 'base'], writes=['base'])
                op('dve', lambda e: e.tensor_copy(out=cntT[:], in_=psCT[0:64, 0:128]), reads=['psCT'], writes=['cntT'])
                op('dve', lambda e: e.tensor_tensor(out=cmpT[:], in0=cntT[:], in1=cst32[0:64, THR], op=ALU.is_gt), reads=['cntT', 'cst32'], writes=['cmpT'])
                op('dve', lambda e: e.tensor_reduce(out=nblk[:], in_=cmpT[:], axis=AX.X, op=ALU.add), reads=['cmpT'], writes=['nblk'])
                op('dve', lambda e: e.tensor_scalar(out=padT[:], in0=cst32[0:64, ONES], scalar1=nblk[:, 0:1], scalar2=float(MB), op0=ALU.mult, op1=ALU.mult),
                   reads=['nblk', 'cst32'], writes=['padT'])
                op('pe', lambda e: e.matmul(psE[:, 0:64], lhsT=padT[:], rhs=cst32[0:64, 4 * 128:4 * 128 + 64], start=True, stop=True), reads=['padT', 'cst32'], writes=['psE'])
                op('pe', lambda e: e.matmul(psE[:, 64:128], lhsT=padT[:], rhs=cst32[0:64, 128:192], start=True, stop=True), reads=['padT', 'cst32'], writes=['psE'])
                op('dve', lambda e: e.tensor_copy(out=padse[:], in_=psE[:, 0:128]), reads=['psE'], writes=['padse'])
                op('dve', lambda e: e.tensor_tensor(out=big[:, 0:NBLK * 64].rearrange("p (b x) -> p b x", x=64),
                                                    in0=padse[:, 64:128].unsqueeze(1).to_broadcast([128, NBLK, 64]),
                                                    in1=cst32[:, BLKS].unsqueeze(2).to_broadcast([128, NBLK, 64]), op=ALU.is_le),
                   reads=['padse', 'cst32'], writes=['big'])
                op('dve', lambda e: e.tensor_reduce(out=blke_f[:], in_=big[:, 0:NBLK * 64].rearrange("p (b x) -> p b x", x=64), axis=AX.X, op=ALU.add),
                   reads=['big'], writes=['blke_f'])
                op('dve', lambda e: e.tensor_scalar(out=blke_f[:], in0=blke_f[:], scalar1=63.0, scalar2=None, op0=ALU.min), reads=['blke_f'], writes=['blke_f'])
                op('dve', lambda e: e.tensor_scalar(out=blke_f[:], in0=blke_f[:], scalar1=128.0, scalar2=cst32[:, 960:961], op0=ALU.mult, op1=ALU.add),
                   reads=['blke_f', 'cst32'], writes=['blke_f'])
                op('dve', lambda e: e.tensor_scalar(out=blke_f[:], in0=blke_f[:], scalar1=float(l * 64 * 128), scalar2=None, op0=ALU.add), reads=['blke_f'], writes=['blke_f'])
                op('dve', lambda e: e.tensor_copy(out=widx_i[:], in_=blke_f[:]), reads=['blke_f'], writes=['widx_i'])
                op('dve', lambda e: e.tensor_tensor(out=big[:, 0:NT * 2 * 64].rearrange("p (b x) -> p b x", x=64),
                                                    in0=cst32[:, IOTA].unsqueeze(1).to_broadcast([128, NT * 2, 64]),
                                                    in1=ek[:, :].unsqueeze(2).to_broadcast([128, NT * 2, 64]), op=ALU.is_equal),
                   reads=['big', 'cst32'] + ['ek%d_%d' % (tt, k) for tt in range(NT) for k in range(2)], writes=['big'])
                op('dve', lambda e: e.tensor_tensor(out=big[:, 0:NT * 2 * 64].rearrange("p (b x) -> p b x", x=64),
                                                    in0=big[:, 0:NT * 2 * 64].rearrange("p (b x) -> p b x", x=64),
                                                    in1=padse[:, 0:64].unsqueeze(1).to_broadcast([128, NT * 2, 64]), op=ALU.mult),
                   reads=['big', 'padse'], writes=['big'])
                op('dve', lambda e: e.tensor_reduce(out=dest_f[:], in_=big[:, 0:NT * 2 * 64].rearrange("p (b x) -> p b x", x=64), axis=AX.X, op=ALU.add),
                   reads=['big'], writes=['dest_f'])
                op('dve', lambda e: e.tensor_tensor(out=dest_f[:], in0=dest_f[:], in1=rank[:], op=ALU.add),
                   reads=['dest_f'] + ['rank%d_%d' % (tt, k) for tt in range(NT) for k in range(2)], writes=['dest_f'])
                op('dve', lambda e: e.tensor_copy(out=dest_i[:], in_=dest_f[:]), reads=['dest_f'], writes=['dest_i'])
                for tt in range(NT):
                    for k in range(2):
                        op('pool', lambda e, tt=tt, k=k: e.indirect_dma_start(out=XB[:, :], out_offset=bass.IndirectOffsetOnAxis(ap=dest_i[:, tt * 2 + k:tt * 2 + k + 1], axis=0),
                                                                              in_=hn2all[:, tt, :], in_offset=None),
                           reads=['dest_i', 'hn2_%d' % tt], writes=['XBs%d_%d' % (tt, k)], dma=True)
                S.flush()
            if stop_after == 'p3':
                break

            with ExitStack() as es:
                def sb(name, shape, dt):
                    return es.enter_context(nc.sbuf_tensor(("L%d_p4_" % l) + name, shape, dt))

                def pst(name, shape, dt):
                    return es.enter_context(nc.psum_tensor(("L%d_p4_" % l) + name, shape, dt))
                NWB = 3
                w1b = [sb("w1b%d" % i, [128, 4096], BF16) for i in range(NWB)]
                w3b = [sb("w3b%d" % i, [128, 4096], BF16) for i in range(NWB)]
                w2b = [sb("w2b%d" % i, [128, 4096], BF16) for i in range(NWB)]
                xb = [sb("xb%d" % i, [128, 2, D], BF16) for i in range(2)]
                xT = [sb("xT%d" % i, [128, 8, MB], BF16) for i in range(2)]
                hT = [sb("hT%d" % i, [128, 4, MB], BF16) for i in range(2)]
                s1 = [sb("s1_%d" % i, [128, MB], F32) for i in range(2)]
                yst = [sb("yst%d" % i, [128, D], F32) for i in range(2)]
                psX = [pst("psX%d" % i, [128, D], BF16) for i in range(2)]
                psH = [pst("psH%d" % i, [128, 512], F32) for i in range(3)]
                psY = [pst("psY%d" % i, [128, 512], F32) for i in range(2)]
                w1v = w1.rearrange("l e (p kc) f -> (l e p) (kc f)", kc=8)
                w3v = w3.rearrange("l e (p kc) f -> (l e p) (kc f)", kc=8)
                w2v = w2.rearrange("l e (p fc) d -> (l e p) (fc d)", fc=4)
                ycnt = [0]

                def emit_loads(blk):
                    b = blk % 2
                    wb = blk % NWB
                    for (wv, wt, nm) in (((w1v, w1b[wb], 'w1b%d' % wb), (w3v, w3b[wb], 'w3b%d' % wb), (w2v, w2b[wb], 'w2b%d' % wb)) if not _DBG.get('nogather') else ()):
                        op('pool', lambda e, wv=wv, wt=wt, blk=blk: e.indirect_dma_start(out=wt[:, :], out_offset=None, in_=wv[:, :],
                                                                                         in_offset=bass.IndirectOffsetOnAxis(ap=widx_i[:, blk:blk + 1], axis=0)),
                           reads=['widx_i'], writes=[nm], dma=True)
                    op('sp', lambda e, b=b, blk=blk: e.dma_start(out=xb[b][:], in_=XB[blk * MB:(blk + 1) * MB, :].rearrange("(s p) d -> p s d", p=128)),
                       writes=['xb%d' % b], dma=True)

                def emit_T(blk):
                    b = blk % 2
                    for s_ in range(2):
                        for kc in range(8):
                            op('pe', lambda e, b=b, s_=s_, kc=kc: e.transpose(out=psX[s_][:, kc * 128:(kc + 1) * 128], in_=xb[b][:, s_, kc:D:8], identity=cstb[:, IDENT]),
                               reads=['xb%d' % b, 'cstb'], writes=['psX%d' % s_], sig=(kc == 7))
                        if s_ == 0:
                            f = lambda e, b=b, s_=s_: e.activation(out=xT[b][:, :, s_ * 128:(s_ + 1) * 128], in_=psX[s_][:, :].rearrange("p (kc t) -> p kc t", kc=8), func=AF.Copy)
                        else:
                            f = lambda e, b=b, s_=s_: e.tensor_copy(out=xT[b][:, :, s_ * 128:(s_ + 1) * 128], in_=psX[s_][:, :].rearrange("p (kc t) -> p kc t", kc=8))
                        op('act' if s_ == 0 else 'dve', f, reads=['psX%d' % s_], writes=['xT%d_%d' % (b, s_)])

                def emit_H(blk):
                    b = blk % 2
                    wb = blk % NWB
                    w1b3 = w1b[wb][:, :].rearrange("p (kc f) -> p kc f", kc=8)
                    w3b3 = w3b[wb][:, :].rearrange("p (kc f) -> p kc f", kc=8)
                    XK = ['xT%d_0' % b, 'xT%d_1' % b]
                    for fc in range(4):
                        hb = (blk * 4 + fc) % 3
                        for kc in range(8):
                            op('pe', lambda e, b=b, fc=fc, kc=kc, hb=hb, w1b3=w1b3: e.matmul(psH[hb][:, 0:MB], lhsT=w1b3[:, kc, fc:512:4], rhs=xT[b][:, kc, :], start=(kc == 0), stop=(kc == 7)),
                               sig=(kc == 7), reads=XK + ['w1b%d' % wb], writes=['psH%d' % hb])
                        for kc in range(8):
                            op('pe', lambda e, b=b, fc=fc, kc=kc, hb=hb, w3b3=w3b3: e.matmul(psH[hb][:, MB:2 * MB], lhsT=w3b3[:, kc, fc:512:4], rhs=xT[b][:, kc, :], start=(kc == 0), stop=(kc == 7)),
                               sig=(kc == 7), reads=XK + ['w3b%d' % wb], writes=['psH%d' % hb])
                        sb_ = (blk * 4 + fc) % 2
                        op('act', lambda e, hb=hb, sb_=sb_: e.activation(out=s1[sb_][:], in_=psH[hb][:, 0:MB], func=AF.Silu), reads=['psH%d' % hb], writes=['s1_%d' % sb_])
                        op('dve', lambda e, b=b, hb=hb, fc=fc, sb_=sb_: e.tensor_tensor(out=hT[b][:, fc, :], in0=s1[sb_][:], in1=psH[hb][:, MB:2 * MB], op=ALU.mult),
                           reads=['s1_%d' % sb_, 'psH%d' % hb], writes=['hT%d_%d' % (b, fc)])

                def emit_Y(blk):
                    b = blk % 2
                    wb = blk % NWB
                    w2b3 = w2b[wb][:, :].rearrange("p (fc d) -> p fc d", fc=4)
                    HKs = ['hT%d_%d' % (b, fc) for fc in range(4)]
                    for s_ in range(2):
                        for half in range(2):
                            yb_ = ycnt[0] % 2
                            ycnt[0] += 1
                            for fc in range(4):
                                op('pe', lambda e, b=b, s_=s_, half=half, fc=fc, yb_=yb_, w2b3=w2b3: e.matmul(psY[yb_][:, :], lhsT=hT[b][:, fc, s_ * 128:(s_ + 1) * 128],
                                                                                                               rhs=w2b3[:, fc, half * 512:(half + 1) * 512], start=(fc == 0), stop=(fc == 3)),
                                   sig=(fc == 3), reads=HKs + ['w2b%d' % wb], writes=['psY%d' % yb_])
                            if half == 0:
                                op('act', lambda e, s_=s_, yb_=yb_: e.activation(out=yst[s_][:, 0:512], in_=psY[yb_][:, :], func=AF.Copy), reads=['psY%d' % yb_], writes=['yst%d_0' % s_])
                            else:
                                op('dve', lambda e, s_=s_, yb_=yb_: e.tensor_copy(out=yst[s_][:, 512:1024], in_=psY[yb_][:, :]), reads=['psY%d' % yb_], writes=['yst%d_1' % s_])
                        op('sp', lambda e, s_=s_, blk=blk: e.dma_start(out=YB[blk * MB + s_ * 128: blk * MB + (s_ + 1) * 128, :], in_=yst[s_][:]),
                           reads=['yst%d_0' % s_, 'yst%d_1' % s_], writes=['YBrow'], dma=True)

                emit_loads(0)
                emit_T(0)
                emit_loads(1)
                for blk in range(NBLK):
                    emit_H(blk)
                    if blk + 1 < NBLK:
                        emit_T(blk + 1)
                    if blk + 2 < NBLK:
                        emit_loads(blk + 2)
                    emit_Y(blk)
                S.flush()
            if stop_after == 'p4':
                break

            last = (l == L - 1)
            with ExitStack() as es:
                def sb(name, shape, dt):
                    return es.enter_context(nc.sbuf_tensor(("L%d_p5_" % l) + name, shape, dt))
                g2rep = sb("g2rep", [128, 3, D], F32)
                gfin = sb("gfin", [128, D], F32)
                y0 = [sb("y0_%d" % i, [128, D], F32) for i in range(2)]
                y1 = [sb("y1_%d" % i, [128, D], F32) for i in range(2)]
                h5 = [sb("h5_%d" % i, [128, D], F32) for i in range(2)]
                junk = sb("junk", [128, D], BF16)
                st = [sb("st%d" % i, [128, 4], F32) for i in range(2)]
                for ms in range(3):
                    op('sp', lambda e, ms=ms: e.dma_start(out=g2rep[:, ms, :], in_=MS[l, ms:ms + 1, 5 * D:6 * D].to_broadcast([128, D])), writes=['g2rep'], dma=True)
                op('sp', lambda e: e.dma_start(out=gfin[:], in_=g_final[0:1, :].to_broadcast([128, D])), writes=['gfin'], dma=True)
                for tt in range(NT):
                    s_, j = divmod(tt, TPS)
                    if last and j < 2:
                        continue
                    b = tt % 2
                    r0 = tt * 128
                    ms = mset(tt)
                    op('pool', lambda e, b=b, tt=tt: e.indirect_dma_start(out=y0[b][:, :], out_offset=None, in_=YB[:, :],
                                                                          in_offset=bass.IndirectOffsetOnAxis(ap=dest_i[:, tt * 2:tt * 2 + 1], axis=0)),
                       writes=['y0_%d' % b], dma=True)
                    op('pool', lambda e, b=b, tt=tt: e.indirect_dma_start(out=y1[b][:, :], out_offset=None, in_=YB[:, :],
                                                                          in_offset=bass.IndirectOffsetOnAxis(ap=dest_i[:, tt * 2 + 1:tt * 2 + 2], axis=0)),
                       writes=['y1_%d' % b], dma=True)
                    op('sp', lambda e, b=b, r0=r0: e.dma_start(out=h5[b][:], in_=H[r0:r0 + 128, :]), writes=['h5_%d' % b], dma=True)
                    op('dve', lambda e, b=b, tt=tt: e.tensor_scalar(out=y0[b][:], in0=y0[b][:], scalar1=gate_f[:, tt * 2:tt * 2 + 1], scalar2=None, op0=ALU.mult),
                       reads=['y0_%d' % b], writes=['y0_%d' % b])
                    op('dve', lambda e, b=b, tt=tt: e.scalar_tensor_tensor(out=y0[b][:], in0=y1[b][:], scalar=gate_f[:, tt * 2 + 1:tt * 2 + 2], in1=y0[b][:], op0=ALU.mult, op1=ALU.add),
                       reads=['y0_%d' % b, 'y1_%d' % b], writes=['y0_%d' % b])
                    op('pool', lambda e, b=b, ms=ms: e.tensor_tensor(out=y0[b][:], in0=y0[b][:], in1=g2rep[:, ms, :], op=ALU.mult), reads=['y0_%d' % b, 'g2rep'], writes=['y0_%d' % b])
                    op('pool', lambda e, b=b: e.tensor_tensor(out=h5[b][:], in0=h5[b][:], in1=y0[b][:], op=ALU.add), reads=['y0_%d' % b, 'h5_%d' % b], writes=['h5_%d' % b])
                    if not last:
                        op('sp', lambda e, b=b, r0=r0: e.dma_start(out=H[r0:r0 + 128, :], in_=h5[b][:]), reads=['h5_%d' % b], writes=['Hrow%d' % tt], dma=True)
                    else:
                        op('dve', lambda e, b=b: e.memset(st[b][:, 0:1], 0.0), writes=['ss%d' % b])
                        op('act', lambda e, b=b: e.activation(out=junk[:], in_=h5[b][:], func=AF.Square, accum_out=st[b][:, 0:1]),
                           reads=['h5_%d' % b, 'ss%d' % b], writes=['junk', 'ss%d' % b])
                        rstd_from_ss(st[b][:, 0:1], st[b][:, 1:2], 'ss%d' % b, 'rs%d' % b, D)
                        op('dve', lambda e, b=b: e.tensor_scalar(out=h5[b][:], in0=h5[b][:], scalar1=st[b][:, 1:2], scalar2=None, op0=ALU.mult),
                           reads=['h5_%d' % b, 'rs%d' % b], writes=['h5_%d' % b])
                        op('pool', lambda e, b=b: e.tensor_tensor(out=h5[b][:], in0=h5[b][:], in1=gfin[:], op=ALU.mult), reads=['h5_%d' % b, 'gfin'], writes=['h5_%d' % b])
                        orow = s_ * SEQ + (j - 2) * 128
                        op('sp', lambda e, b=b, orow=orow: e.dma_start(out=out[orow:orow + 128, :], in_=h5[b][:]), reads=['h5_%d' % b], writes=['orow%d' % tt], dma=True)
                S.flush()
            if stop_after == 'p5':
                break
    return nc


def _prep_inputs(inputs):
    f32 = np.float32
    x, c, ctx, c_ctx = inputs['x'], inputs['c'], inputs['ctx'], inputs['c_ctx']
    j = np.arange(128)
    ident = np.eye(128, dtype=f32)
    tri = (j[:, None] <= j[None, :]).astype(f32)
    trit = (j[:, None] >= j[None, :]).astype(f32)
    su = (j[:, None] > j[None, :]).astype(f32)
    slw = (j[:, None] < j[None, :]).astype(f32)
    ones = np.ones((128, 128), f32)
    iota = np.broadcast_to(np.arange(64, dtype=f32)[None, :], (128, 64))
    blks = np.broadcast_to((np.arange(128, dtype=f32) * MB)[None, :], (128, 128))
    pidx = np.arange(128, dtype=f32)[:, None]
    pad = np.zeros((128, 1024 - 961), f32)
    cst = np.ascontiguousarray(np.concatenate([ident, tri, trit, su, slw, ones, iota, blks, pidx, pad], axis=1))
    shared = {
        'cst': cst,
        'w_mod': inputs['w_mod'],
        'b_mod3': np.ascontiguousarray(np.broadcast_to(inputs['b_mod'][:, None, :], (L, 3, 6 * D))),
        'gn1T': np.ascontiguousarray(inputs['g_norm1'].reshape(L, 8, 128).transpose(0, 2, 1)),
        'w_in': inputs['w_in'],
        'ln_v_g': inputs['ln_v_g'], 'ln_v_b': inputs['ln_v_b'],
        'w_spT': np.ascontiguousarray(inputs['w_sp'].transpose(0, 1, 3, 2)),
        'b_sp': np.ascontiguousarray(inputs['b_sp'].reshape(L, 512)),
        'w_gate_up': inputs['w_gate_up'], 'b_gate': inputs['b_gate'],
        'g_glaT': np.ascontiguousarray(inputs['g_gla'].reshape(L, 4, 128).transpose(0, 2, 1)),
        'w_out': inputs['w_out'], 'g_norm2': inputs['g_norm2'],
        'w_r': np.ascontiguousarray(np.concatenate([inputs['w_router_g'], inputs['w_router_e']], axis=2)),
        'b_r': np.ascontiguousarray(np.concatenate([inputs['b_router_g'], inputs['b_router_e']], axis=1)),
        'w1': inputs['w1'], 'w3': inputs['w3'], 'w2': inputs['w2'],
        'g_final': np.ascontiguousarray(inputs['g_final'].reshape(1, D)),
    }
    in_maps = []
    for core in range(NCORES):
        rows, cm = [], []
        for s in range(NS):
            b = core * NS + s
            rows.append(ctx[b])
            rows.append(x[b])
            cm.append(c[b])
        cm.append(c_ctx)
        cmod = np.ascontiguousarray(np.stack(cm, axis=0).reshape(3, 8, 128).transpose(2, 1, 0)).astype(f32)
        m = dict(shared)
        m['hin'] = np.ascontiguousarray(np.concatenate(rows, axis=0)).astype(f32)
        m['cmod'] = cmod
        in_maps.append(m)
    return in_maps


def kernel(**inputs):
    inputs = {k: np.asarray(v) for k, v in inputs.items()}
    in_maps = _prep_inputs(inputs)
    nc = build_program()
    res = run_bass_kernel_spmd(nc, in_maps, core_ids=list(range(NCORES)))
    outs = [r["out"].reshape(NS, SEQ, D) for r in res.results]
    return np.concatenate(outs, axis=0).astype(np.float32)
```

```python
import types
import numpy as np
from contextlib import ExitStack
import concourse.bass as bass
import concourse.mybir as mybir
from concourse.bass_utils import run_bass_kernel_spmd

F32 = mybir.dt.float32
BF16 = mybir.dt.bfloat16
I32 = mybir.dt.int32
AF = mybir.ActivationFunctionType
ALU = mybir.AluOpType
AX = mybir.AxisListType

NCORES = 8
L = 2
D = 1024
DIN = 2592
NS = 2
LC = 256
SEQ = 2048
TPS = 18
NT = NS * TPS
T = NT * 128
MB = 256
NBLK = (2 * T) // MB + 64
NSLOT = NBLK * MB
EPS = 1e-6
OFF_AU, OFF_AV, OFF_BQ, OFF_BR, OFF_BK, OFF_BV, OFF_BG = 0, 512, 1024, 1280, 1792, 2048, 2560

SAME_ENGINE_SYNC = True
_DBG = {}


def _freeze(fn):
    if fn.__closure__ is None:
        return fn
    cells = []
    for c in fn.__closure__:
        try:
            cells.append(types.CellType(c.cell_contents))
        except ValueError:
            cells.append(c)
    return types.FunctionType(fn.__code__, fn.__globals__, fn.__name__, fn.__defaults__, tuple(cells))


class Sched:
    ENGS = ['pe', 'act', 'dve', 'pool', 'sp']

    def __init__(self, nc, es, n_dma_sems=(('sp', 16), ('act', 4), ('pool', 16))):
        self.nc = nc
        self.prog = {e: [] for e in self.ENGS}
        self.esem = {e: es.enter_context(nc.semaphore('sem_' + e)) for e in self.ENGS}
        self.ecount = {e: 0 for e in self.ENGS}
        self.seen = {e: {} for e in self.ENGS}
        self.res = {}
        self.dpool, self.dnext, self.dcount, self.semobj = {}, {}, {}, {}
        for e in self.ENGS:
            self.semobj['E' + e] = self.esem[e]
        for e, n in n_dma_sems:
            self.dpool[e] = []
            for i in range(n):
                key = 'D%s%d' % (e, i)
                self.semobj[key] = es.enter_context(nc.semaphore('dsem_%s%d' % (e, i)))
                self.dpool[e].append(key)
                self.dcount[key] = 0
            self.dnext[e] = 0
        self.nops = 0
        self.nwaits = 0

    def _wait(self, eng, tok):
        s, v = tok
        if self.seen[eng].get(s, 0) >= v:
            return
        self.seen[eng][s] = v
        self.prog[eng].append(('wait', s, v))
        self.nwaits += 1

    def op(self, eng, fn, reads=(), writes=(), dma=False, sig=True):
        if _DBG.get('maxops') and self.nops >= _DBG['maxops']:
            return None
        fn = _freeze(fn)
        writes = list(writes) + [r for r in reads if r.startswith('ps') and r not in writes]
        deps = []
        for r in reads:
            st = self.res.get(r)
            if st is not None and st['w'] is not None:
                deps.append(st['w'])
        for w in writes:
            st = self.res.get(w)
            if st is not None:
                if st['w'] is not None:
                    deps.append(st['w'])
                deps.extend(st['r'])
        if dma:
            pool = self.dpool[eng]
            key = pool[self.dnext[eng] % len(pool)]
            self.dnext[eng] += 1
            cnt = self.dcount[key]
            if cnt > 0:
                deps.append((key, cnt))
            self.dcount[key] = cnt + 16
            tok = (key, cnt + 16)
            inc = 16
        elif not sig:
            key = 'E' + eng
            tok = (key, self.ecount[eng] + 1)
            inc = 0
        else:
            key = 'E' + eng
            self.ecount[eng] += 1
            tok = (key, self.ecount[eng])
            inc = 1
        own = 'E' + eng
        for d in deps:
            if d[0] == own and (eng == 'pe' or not SAME_ENGINE_SYNC):
                continue
            self._wait(eng, d)
        self.prog[eng].append(('op', fn, key, inc))
        self.nops += 1
        for r in reads:
            st = self.res.setdefault(r, {'w': None, 'r': []})
            st['r'].append(tok)
        for w in writes:
            self.res[w] = {'w': tok, 'r': []}
        return tok

    def barrier(self):
        for e in self.ENGS:
            for key, cnt in self.dcount.items():
                if cnt > 0:
                    self._wait(e, (key, cnt))
            for e2 in self.ENGS:
                if e2 != e and self.ecount[e2] > 0:
                    self._wait(e, ('E' + e2, self.ecount[e2]))
        self.res = {}

    def flush(self):
        if _DBG.get('verbose'):
            print('flush: nops', self.nops, 'nwaits', self.nwaits, flush=True)
        self.barrier()
        nc = self.nc
        with nc.Block() as block:
            def run(e):
                def f(eng):
                    for it in self.prog[e]:
                        if it[0] == 'wait':
                            eng.wait_ge(self.semobj[it[1]], it[2])
                        else:
                            ins = it[1](eng)
                            if it[3]:
                                ins.then_inc(self.semobj[it[2]], it[3])
                return f
            block.tensor(run('pe'))
            block.scalar(run('act'))
            block.vector(run('dve'))
            block.gpsimd(run('pool'))
            block.sync(run('sp'))
        self.prog = {e: [] for e in self.ENGS}


def build_program(dbg=False, stop_after=None):
    nc = bass.Bass("TRN2", target_bir_lowering=False)

    def din(name, shape, dt=F32):
        return nc.dram_tensor(name, list(shape), dt, kind="ExternalInput").ap()

    def dscr(name, shape, dt):
        kind = "ExternalOutput" if dbg else "Internal"
        return nc.dram_tensor(name, list(shape), dt, kind=kind).ap()

    hin = din("hin", [T, D])
    cmod = din("cmod", [128, 8, 3])
    cst = din("cst", [128, 1024])
    w_mod = din("w_mod", [L, D, 6 * D])
    b_mod3 = din("b_mod3", [L, 3, 6 * D])
    gn1T = din("gn1T", [L, 128, 8])
    w_in = din("w_in", [L, D, DIN])
    ln_g = din("ln_v_g", [L, 512])
    ln_b = din("ln_v_b", [L, 512])
    w_spT = din("w_spT", [L, 4, 128, 128])
    b_sp = din("b_sp", [L, 512])
    w_gu = din("w_gate_up", [L, 2, 16, 256])
    b_gate = din("b_gate", [L, 2, 256])
    g_glaT = din("g_glaT", [L, 128, 4])
    w_out = din("w_out", [L, D, D])
    gn2 = din("g_norm2", [L, D])
    w_r = din("w_r", [L, D, 72])
    b_r = din("b_r", [L, 72])
    EW = 1 if stop_after in ("p0", "p1", "p2", "p3") else 64
    w1 = din("w1", [L, EW, D, 512])
    w3 = din("w3", [L, EW, D, 512])
    w2 = din("w2", [L, EW, 512, D])
    g_final = din("g_final", [1, D])
    out = nc.dram_tensor("out", [NS * SEQ, D], F32, kind="ExternalOutput").ap()

    H = dscr("H", [T, D], F32)
    MS = dscr("MS", [L, 3, 6 * D], F32)
    UT = dscr("UT", [4, 128, T], BF16)
    QT = dscr("QT", [2, 128, T], BF16)
    KT = dscr("KT", [2, 128, T], BF16)
    RT = dscr("RT", [4, 128, T], BF16)
    LA = dscr("LA", [T, 512], F32)
    Vd = dscr("Vd", [T, 512], BF16)
    Kd = dscr("Kd", [T, 256], BF16)
    VA = dscr("VA", [T, 512], BF16)
    XB = dscr("XB", [NSLOT, D], BF16)
    YB = dscr("YB", [NSLOT, D], F32)

    top = ExitStack()
    with top:
        S = Sched(nc, top)
        op = S.op

        def mset(tile_idx):
            s, j = divmod(tile_idx, TPS)
            return 2 if j < 2 else s

        def psb(name, shape, dt):
            return top.enter_context(nc.sbuf_tensor(name, shape, dt))
        cst32 = psb("cst32", [128, 1024], F32)
        cstb = psb("cstb", [128, 768], BF16)
        widx_i = psb("widx_i", [128, NBLK], I32)
        zt = psb("zt", [128, 4, D], BF16)
        dest_i = psb("dest_i", [128, NT * 2], I32)
        gate_f = psb("gate_f", [128, NT * 2], F32)
        IDENT, TRI, TRIT, SU, SLW, ONES = [slice(i * 128, (i + 1) * 128) for i in range(6)]
        op('sp', lambda e: e.dma_start(out=cst32[:], in_=cst[:, :]), writes=['cst32'], dma=True)
        op('dve', lambda e: e.tensor_copy(out=cstb[:], in_=cst32[:, 0:768]), reads=['cst32'], writes=['cstb'])
        op('dve', lambda e: e.memset(zt[:], 0.0), writes=['zt'])
        for zi in range(NSLOT // 512):
            op('sp', lambda e, zi=zi: e.dma_start(out=XB[zi * 512:(zi + 1) * 512, :].rearrange("(s p) d -> p s d", p=128), in_=zt[:]), reads=['zt'], writes=['XBz'], dma=True)
        S.flush()

        def rstd_from_ss(ss, rs, key_ss, key_rs, n):
            op('dve', lambda e: e.tensor_scalar(out=rs, in0=ss, scalar1=1.0 / n, scalar2=EPS, op0=ALU.mult, op1=ALU.add),
               reads=[key_ss], writes=[key_rs])
            op('act', lambda e: e.activation(out=rs, in_=rs, func=AF.Sqrt), reads=[key_rs], writes=[key_rs])
            op('dve', lambda e: e.reciprocal(out=rs, in_=rs), reads=[key_rs], writes=[key_rs])

        for l in range(L):
            Hsrc = hin if l == 0 else H
            with ExitStack() as es:
                def sb(name, shape, dt):
                    return es.enter_context(nc.sbuf_tensor(("L%d_p0_" % l) + name, shape, dt))
                scT = sb("scT", [128, 8, 3], F32)
                wm = [sb("wm%d" % i, [128, 8, 512], F32) for i in range(2)]
                bm = [sb("bm%d" % i, [3, 512], F32) for i in range(2)]
                mo = [sb("mo%d" % i, [3, 512], F32) for i in range(2)]
                psM = [es.enter_context(nc.psum_tensor("L%d_p0_psM%d" % (l, i), [128, 512], F32)) for i in range(2)]
                op('sp', lambda e: e.dma_start(out=scT[:], in_=cmod[:, :, :]), writes=['scT'], dma=True)
                op('act', lambda e: e.activation(out=scT[:], in_=scT[:], func=AF.Silu), reads=['scT'], writes=['scT'])
                for cg in range(12 if not _DBG.get('nop0') else 0):
                    i = cg % 2
                    cs = slice(cg * 512, (cg + 1) * 512)
                    op('sp', lambda e, i=i, cs=cs: e.dma_start(out=wm[i][:], in_=w_mod[l, :, cs].rearrange("(kc p) c -> p kc c", p=128)),
                       writes=['wm%d' % i], dma=True)
                    op('sp', lambda e, i=i, cs=cs: e.dma_start(out=bm[i][:], in_=b_mod3[l, :, cs]), writes=['bm%d' % i], dma=True)
                    for kc in range(8):
                        op('pe', lambda e, i=i, kc=kc: e.matmul(psM[i][0:3, :], lhsT=scT[:, kc, :], rhs=wm[i][:, kc, :],
                                                               start=(kc == 0), stop=(kc == 7)),
                           sig=(kc == 7), reads=['scT', 'wm%d' % i], writes=['psM%d' % i])
                    op('dve', lambda e, i=i: e.tensor_tensor(out=mo[i][:], in0=psM[i][0:3, :], in1=bm[i][:], op=ALU.add),
                       reads=['psM%d' % i, 'bm%d' % i], writes=['mo%d' % i])
                    op('sp', lambda e, i=i, cs=cs: e.dma_start(out=MS[l, :, cs], in_=mo[i][:]), reads=['mo%d' % i], writes=['MS'], dma=True)
                S.flush()

            if stop_after == 'p0':
                break
            with ExitStack() as es:
                def sb(name, shape, dt):
                    return es.enter_context(nc.sbuf_tensor(("L%d_p1_" % l) + name, shape, dt))

                def pst(name, shape, dt):
                    return es.enter_context(nc.psum_tensor(("L%d_p1_" % l) + name, shape, dt))
                winb = sb("winb", [128, 8, DIN], BF16)
                wup = sb("wup", [64, 512], F32)
                A1T = sb("A1T", [128, 3, 8], F32)
                sh1T = sb("sh1T", [128, 3, 8], F32)
                g1t = sb("g1t", [128, 8], F32)
                lng = sb("lng", [128, 512], F32)
                lnb = sb("lnb", [128, 512], F32)
                zgT = sb("zgT", [64, 256], F32)
                ht = [sb("ht%d" % i, [128, D], F32) for i in range(2)]
                junk = sb("junk", [128, D], BF16)
                hs = [sb("hs%d" % i, [128, D], BF16) for i in range(2)]
                hnT = [sb("hnT%d" % i, [128, 8, 256], BF16) for i in range(2)]
                st = [sb("st%d" % i, [128, 24], F32) for i in range(2)]
                fo = [sb("fo%d" % i, [128, 256], BF16) for i in range(4)]
                g32 = [sb("g32_%d" % i, [128, 512], F32) for i in range(2)]
                sq32 = [sb("sq32_%d" % i, [128, 512], F32) for i in range(2)]
                vab = [sb("vab%d" % i, [128, 512], BF16) for i in range(2)]
                vtb = [sb("vtb%d" % i, [128, 512], BF16) for i in range(2)]
                ktb = [sb("ktb%d" % i, [128, 256], BF16) for i in range(2)]
                la32 = [sb("la32_%d" % i, [128, 512], F32) for i in range(2)]
                psT = [pst("psT%d" % i, [128, 1024], BF16) for i in range(2)]
                psF = [pst("psF%d" % i, [128, 512], F32) for i in range(3)]
                psK = [pst("psK%d" % i, [128, 512], F32) for i in range(3)]

                for kc in range(8):
                    op('pool', lambda e, kc=kc: e.dma_start(out=winb[:, kc, :], in_=w_in[l, kc * 128:(kc + 1) * 128, :]),
                       writes=['winb'], dma=True)
                op('dve', lambda e: e.memset(wup[:], 0.0), writes=['wup'])
                op('sp', lambda e: e.dma_start(out=wup[0:16, 0:256], in_=w_gu[l, 0, :, :]), reads=['wup'], writes=['wup0'], dma=True)
                op('sp', lambda e: e.dma_start(out=wup[16:32, 256:512], in_=w_gu[l, 1, :, :]), reads=['wup'], writes=['wup1'], dma=True)
                op('sp', lambda e: e.dma_start(out=wup[32:33, 0:256], in_=b_gate[l, 0:1, :]), reads=['wup'], writes=['wup2'], dma=True)
                op('sp', lambda e: e.dma_start(out=wup[32:33, 256:512], in_=b_gate[l, 1:2, :]), reads=['wup'], writes=['wup3'], dma=True)
                WUPK = ['wup', 'wup0', 'wup1', 'wup2', 'wup3']
                op('dve', lambda e: e.memset(zgT[:], 1.0), writes=['zgT'])
                op('sp', lambda e: e.dma_start(out=g1t[:], in_=gn1T[l, :, :]), writes=['g1t'], dma=True)
                op('sp', lambda e: e.dma_start(out=lng[:], in_=ln_g[l:l + 1, :].to_broadcast([128, 512])), writes=['lng'], dma=True)
                op('sp', lambda e: e.dma_start(out=lnb[:], in_=ln_b[l:l + 1, :].to_broadcast([128, 512])), writes=['lnb'], dma=True)
                for ms in range(3):
                    op('sp', lambda e, ms=ms: e.dma_start(out=sh1T[:, ms, :], in_=MS[l, ms, 0:D].rearrange("(kc p) -> p kc", p=128),
                                                           allow_slow_non_contiguous=True), reads=['MS'], writes=['sh1T%d' % ms], dma=True)
                    op('sp', lambda e, ms=ms: e.dma_start(out=A1T[:, ms, :], in_=MS[l, ms, D:2 * D].rearrange("(kc p) -> p kc", p=128),
                                                           allow_slow_non_contiguous=True), reads=['MS'], writes=['A1T%d' % ms], dma=True)
                    op('dve', lambda e, ms=ms: e.scalar_tensor_tensor(out=A1T[:, ms, :], in0=A1T[:, ms, :], scalar=1.0, in1=g1t[:],
                                                                      op0=ALU.add, op1=ALU.mult),
                       reads=['A1T%d' % ms, 'g1t'], writes=['A1T%d' % ms])

                NG = NT // 2 if not _DBG.get('ng') else _DBG['ng']
                fcnt = [0]
                def p1_front(gi):
                    gb = gi % 2
                    t0 = gi * 256
                    ms = mset(gi * 2)
                    for ti in range(2):
                        tt = gi * 2 + ti
                        b = tt % 2
                        r0 = tt * 128
                        op('sp', lambda e, b=b, r0=r0: e.dma_start(out=ht[b][:], in_=Hsrc[r0:r0 + 128, :]), reads=['H'], writes=['ht%d' % b], dma=True)
                        op('dve', lambda e, b=b: e.memset(st[b][:, 0:1], 0.0), writes=['ss%d' % b])
                        op('act', lambda e, b=b: e.activation(out=junk[:], in_=ht[b][:], func=AF.Square, accum_out=st[b][:, 0:1]),
                           reads=['ht%d' % b, 'ss%d' % b], writes=['junk', 'ss%d' % b])
                        rstd_from_ss(st[b][:, 0:1], st[b][:, 1:2], 'ss%d' % b, 'rs%d' % b, D)
                        op('dve', lambda e, b=b: e.tensor_scalar(out=hs[b][:], in0=ht[b][:], scalar1=st[b][:, 1:2], scalar2=None, op0=ALU.mult),
                           reads=['ht%d' % b, 'rs%d' % b], writes=['hs%d' % b])
                        for kc in range(8):
                            op('pe', lambda e, b=b, kc=kc: e.transpose(out=psT[b][:, kc * 128:(kc + 1) * 128], in_=hs[b][:, kc * 128:(kc + 1) * 128],
                                                                      identity=cstb[:, IDENT]),
                               reads=['hs%d' % b, 'cstb'], writes=['psT%d' % b], sig=(kc == 7))
                        for kc in range(8):
                            eng = 'act' if ti == 0 else 'dve'
                            if eng == 'act':
                                f = lambda e, b=b, kc=kc, ti=ti: e.activation(out=hnT[gb][:, kc, ti * 128:(ti + 1) * 128], in_=psT[b][:, kc * 128:(kc + 1) * 128],
                                                                               func=AF.Identity, scale=A1T[:, ms, kc:kc + 1], bias=sh1T[:, ms, kc:kc + 1])
                            else:
                                f = lambda e, b=b, kc=kc, ti=ti: e.tensor_scalar(out=hnT[gb][:, kc, ti * 128:(ti + 1) * 128], in0=psT[b][:, kc * 128:(kc + 1) * 128],
                                                                                  scalar1=A1T[:, ms, kc:kc + 1], scalar2=sh1T[:, ms, kc:kc + 1],
                                                                                  op0=ALU.mult, op1=ALU.add)
                            op(eng, f, reads=['psT%d' % b, 'A1T%d' % ms, 'sh1T%d' % ms], writes=['hnT%d_%d_%d' % (gb, ti, kc)])

                def p1_back(gi):
                    gb = gi % 2
                    t0 = gi * 256
                    ms = mset(gi * 2)
                    HK = ['hnT%d_%d_%d' % (gb, ti, kc) for ti in range(2) for kc in range(8)]
                    fm = [(OFF_AU + 128 * i, UT, i, AF.Gelu, 1.0) for i in range(4)]
                    fm += [(OFF_BQ + 128 * i, QT, i, AF.Copy, 0.125) for i in range(2)]
                    fm += [(OFF_BR + 128 * i, RT, i, AF.Silu, 1.0) for i in range(4)]
                    fm += [(OFF_BK + 128 * i, KT, i, AF.Copy, 1.0) for i in range(2)]
                    for (c0, dst, ci, func, scl) in fm:
                        n = fcnt[0]
                        fcnt[0] += 1
                        pb, ph = (n // 2) % 3, n % 2
                        pk = 'psF%d' % pb
                        pv = psF[pb][:, ph * 256:(ph + 1) * 256]
                        fb = n % 4
                        for kc in range(8):
                            op('pe', lambda e, pv=pv, kc=kc, c0=c0: e.matmul(pv, lhsT=winb[:, kc, c0:c0 + 128], rhs=hnT[gb][:, kc, :],
                                                                            start=(kc == 0), stop=(kc == 7)),
                               sig=(kc == 7), reads=['winb'] + HK, writes=[pk])
                        op('act', lambda e, pv=pv, fb=fb, func=func, scl=scl: e.activation(out=fo[fb][:], in_=pv, func=func, scale=scl),
                           reads=[pk], writes=['fo%d' % fb])
                        op('sp', lambda e, fb=fb, dst=dst, ci=ci: e.dma_start(out=dst[ci, :, t0:t0 + 256], in_=fo[fb][:]),
                           reads=['fo%d' % fb], writes=['z'], dma=True)
                    n = fcnt[0]
                    fcnt[0] += 1
                    pb, ph = (n // 2) % 3, n % 2
                    pk = 'psF%d' % pb
                    pvg = psF[pb][0:32, ph * 256:(ph + 1) * 256]
                    for kc in range(8):
                        op('pe', lambda e, pvg=pvg, kc=kc: e.matmul(pvg, lhsT=winb[:, kc, OFF_BG:OFF_BG + 32], rhs=hnT[gb][:, kc, :],
                                                                    start=(kc == 0), stop=(kc == 7)),
                           sig=(kc == 7), reads=['winb'] + HK, writes=[pk])
                    op('dve', lambda e, pvg=pvg: e.tensor_copy(out=zgT[0:32, :], in_=pvg), reads=[pk], writes=['zgT'])
                    for ti in range(2):
                        tt = gi * 2 + ti
                        b = tt % 2
                        r0 = tt * 128
                        HKt = ['hnT%d_%d_%d' % (gb, ti, kc) for kc in range(8)]
                        pz, pkk, pvv = psK[0], psK[1], psK[2]
                        for kc in range(8):
                            op('pe', lambda e, kc=kc, ti=ti: e.matmul(pz[:, :], lhsT=hnT[gb][:, kc, ti * 128:(ti + 1) * 128], rhs=winb[:, kc, OFF_AV:OFF_AV + 512],
                                                                      start=(kc == 0), stop=(kc == 7)), sig=(kc == 7), reads=['winb'] + HKt, writes=['psK0'])
                        for kc in range(8):
                            op('pe', lambda e, kc=kc, ti=ti: e.matmul(pvv[:, :], lhsT=hnT[gb][:, kc, ti * 128:(ti + 1) * 128], rhs=winb[:, kc, OFF_BV:OFF_BV + 512],
                                                                      start=(kc == 0), stop=(kc == 7)), sig=(kc == 7), reads=['winb'] + HKt, writes=['psK2'])
                        op('act', lambda e, b=b: e.activation(out=g32[b][:], in_=pz[:, :], func=AF.Gelu), reads=['psK0'], writes=['g32_%d' % b])
                        op('dve', lambda e, b=b: e.tensor_copy(out=vtb[b][:], in_=pvv[:, :]), reads=['psK2'], writes=['vtb%d' % b])
                        op('sp', lambda e, b=b, r0=r0: e.dma_start(out=Vd[r0:r0 + 128, :], in_=vtb[b][:]), reads=['vtb%d' % b], writes=['z'], dma=True)
                        for kc in range(8):
                            op('pe', lambda e, kc=kc, ti=ti: e.matmul(pkk[:, 0:256], lhsT=hnT[gb][:, kc, ti * 128:(ti + 1) * 128], rhs=winb[:, kc, OFF_BK:OFF_BK + 256],
                                                                      start=(kc == 0), stop=(kc == 7)), sig=(kc == 7), reads=['winb'] + HKt, writes=['psK1'])
                        op('act', lambda e, b=b: e.activation(out=ktb[b][:], in_=pkk[:, 0:256], func=AF.Copy), reads=['psK1'], writes=['ktb%d' % b])
                        op('sp', lambda e, b=b, r0=r0: e.dma_start(out=Kd[r0:r0 + 128, :], in_=ktb[b][:]), reads=['ktb%d' % b], writes=['z'], dma=True)
                        g3 = g32[b][:, :].rearrange("p (g c) -> p g c", g=4)
                        s3 = sq32[b][:, :].rearrange("p (g c) -> p g c", g=4)
                        stb = st[b]
                        op('pool', lambda e, b=b: e.tensor_tensor(out=sq32[b][:], in0=g32[b][:], in1=g32[b][:], op=ALU.mult),
                           reads=['g32_%d' % b], writes=['sq32_%d' % b])
                        op('dve', lambda e, g3=g3, stb=stb: e.tensor_reduce(out=stb[:, 4:8], in_=g3, axis=AX.X, op=ALU.add), reads=['g32_%d' % b], writes=['ln1_%d' % b])
                        op('dve', lambda e, s3=s3, stb=stb: e.tensor_reduce(out=stb[:, 8:12], in_=s3, axis=AX.X, op=ALU.add), reads=['sq32_%d' % b], writes=['ln2_%d' % b])
                        op('dve', lambda e, stb=stb: e.tensor_scalar(out=stb[:, 4:8], in0=stb[:, 4:8], scalar1=1.0 / 128, scalar2=None, op0=ALU.mult),
                           reads=['ln1_%d' % b], writes=['ln1_%d' % b])
                        op('dve', lambda e, stb=stb: e.tensor_tensor(out=stb[:, 12:16], in0=stb[:, 4:8], in1=stb[:, 4:8], op=ALU.mult),
                           reads=['ln1_%d' % b], writes=['ln3_%d' % b])
                        op('dve', lambda e, stb=stb: e.scalar_tensor_tensor(out=stb[:, 8:12], in0=stb[:, 8:12], scalar=1.0 / 128, in1=stb[:, 12:16],
                                                                            op0=ALU.mult, op1=ALU.subtract),
                           reads=['ln2_%d' % b, 'ln3_%d' % b], writes=['ln2_%d' % b])
                        op('dve', lambda e, stb=stb: e.tensor_scalar(out=stb[:, 8:12], in0=stb[:, 8:12], scalar1=EPS, scalar2=None, op0=ALU.add),
                           reads=['ln2_%d' % b], writes=['ln2_%d' % b])
                        op('act', lambda e, stb=stb: e.activation(out=stb[:, 8:12], in_=stb[:, 8:12], func=AF.Sqrt), reads=['ln2_%d' % b], writes=['ln2_%d' % b])
                        op('dve', lambda e, stb=stb: e.reciprocal(out=stb[:, 8:12], in_=stb[:, 8:12]), reads=['ln2_%d' % b], writes=['ln2_%d' % b])
                        op('dve', lambda e, g3=g3, stb=stb: e.tensor_tensor(out=g3, in0=g3, in1=stb[:, 4:8].unsqueeze(2).to_broadcast([128, 4, 128]), op=ALU.subtract),
                           reads=['g32_%d' % b, 'ln1_%d' % b, 'sq32_%d' % b], writes=['g32_%d' % b])
                        op('dve', lambda e, g3=g3, stb=stb: e.tensor_tensor(out=g3, in0=g3, in1=stb[:, 8:12].unsqueeze(2).to_broadcast([128, 4, 128]), op=ALU.mult),
                           reads=['g32_%d' % b, 'ln2_%d' % b], writes=['g32_%d' % b])
                        op('pool', lambda e, b=b: e.tensor_tensor(out=g32[b][:], in0=g32[b][:], in1=lng[:], op=ALU.mult),
                           reads=['g32_%d' % b, 'lng'], writes=['g32_%d' % b])
                        op('pool', lambda e, b=b: e.tensor_tensor(out=vab[b][:], in0=g32[b][:], in1=lnb[:], op=ALU.add),
                           reads=['g32_%d' % b, 'lnb'], writes=['vab%d' % b])
                        op('sp', lambda e, b=b, r0=r0: e.dma_start(out=VA[r0:r0 + 128, :], in_=vab[b][:]), reads=['vab%d' % b], writes=['z'], dma=True)
                        op('pe', lambda e, ti=ti: e.matmul(pkk[:, :], lhsT=zgT[0:64, ti * 128:(ti + 1) * 128], rhs=wup[0:64, :], start=True, stop=True),
                           reads=['zgT'] + WUPK + ['ktb%d' % b], writes=['psK1'])
                        op('act', lambda e, b=b: e.activation(out=la32[b][:], in_=pkk[:, :], func=AF.Exp, scale=-1.0), reads=['psK1'], writes=['la32_%d' % b])
                        op('act', lambda e, b=b: e.activation(out=la32[b][:], in_=la32[b][:], func=AF.Ln, bias=1.0), reads=['la32_%d' % b], writes=['la32_%d' % b])
                        op('pool', lambda e, b=b: e.tensor_scalar(out=la32[b][:], in0=la32[b][:], scalar1=-1.0 / 16, scalar2=None, op0=ALU.mult),
                           reads=['la32_%d' % b], writes=['la32_%d' % b])
                        op('sp', lambda e, b=b, r0=r0: e.dma_start(out=LA[r0:r0 + 128, :], in_=la32[b][:]), reads=['la32_%d' % b], writes=['z'], dma=True)

                p1_front(0)
                for gi in range(NG):
                    if gi + 1 < NG:
                        p1_front(gi + 1)
                    p1_back(gi)
                S.flush()
            if stop_after == 'p1':
                break
            with ExitStack() as es:
                def sb(name, shape, dt):
                    return es.enter_context(nc.sbuf_tensor(("L%d_p2_" % l) + name, shape, dt))

                def pst(name, shape, dt):
                    return es.enter_context(nc.psum_tensor(("L%d_p2_" % l) + name, shape, dt))
                woutb = sb("woutb", [128, 8, D], BF16)
                wspb = sb("wspb", [128, 4, 128], BF16)
                bsp = sb("bsp", [128, 512], F32)
                ggla = sb("ggla", [128, 4], F32)
                g1rep = sb("g1rep", [128, 3, D], F32)
                KVs = sb("KVs", [128, TPS * 4, 128], F32)
                dec = sb("dec", [128, TPS * 4], F32)
                Sst = sb("Sst", [128, 4, 128], F32)
                Sprev = sb("Sprev", [128, TPS * 4, 128], BF16)
                la = [sb("la%d" % i, [128, 512], F32) for i in range(2)]
                ktm = [sb("ktm%d" % i, [128, 256], BF16) for i in range(2)]
                vtm = [sb("vtm%d" % i, [128, 512], BF16) for i in range(2)]
                eB = [sb("eB%d" % i, [128, 512], F32) for i in range(2)]
                kd = [sb("kd%d" % i, [128, 512], BF16) for i in range(2)]
                qTt = [sb("qT%d" % i, [128, 2, 128], BF16) for i in range(2)]
                kTt = [sb("kT%d" % i, [128, 2, 128], BF16) for i in range(2)]
                rTt = [sb("rT%d" % i, [128, 4, 128], BF16) for i in range(2)]
                uTt = [sb("uT%d" % i, [128, 4, 128], BF16) for i in range(2)]
                vat = [sb("va%d" % i, [128, 512], BF16) for i in range(2)]
                hh = [sb("hh%d" % i, [128, D], F32) for i in range(2)]
                Ep = [sb("Ep%d" % i, [128, 512], F32) for i in range(2)]
                Em = [sb("Em%d" % i, [128, 512], F32) for i in range(2)]
                qeL = [sb("qeL%d" % i, [128, 512], BF16) for i in range(2)]
                qeH = [sb("qeH%d" % i, [128, 512], BF16) for i in range(2)]
                keL = [sb("keL%d" % i, [128, 512], BF16) for i in range(2)]
                keH = [sb("keH%d" % i, [128, 512], BF16) for i in range(2)]
                atf = [sb("atf%d" % i, [128, 512], BF16) for i in range(2)]
                atb = [sb("atb%d" % i, [128, 512], BF16) for i in range(2)]
                sq = [sb("sq%d" % i, [128, 512], BF16) for i in range(2)]
                rs = [sb("rs%d" % i, [128, 512], F32) for i in range(2)]
                tmp = [sb("tmp%d" % i, [128, 512], F32) for i in range(2)]
                tmp2 = [sb("tmp2%d" % i, [128, 512], F32) for i in range(2)]
                bo = [sb("bo%d" % i, [128, 512], BF16) for i in range(2)]
                ao = [sb("ao%d" % i, [128, 512], BF16) for i in range(2)]
                hm = [sb("hm%d" % i, [128, D], F32) for i in range(2)]
                psB = pst("psB", [128, 512], F32)
                psKV = [pst("psKV%d" % i, [128, 512], F32) for i in range(2)]
                psD = pst("psD", [128, 512], F32)
                psO = pst("psO", [128, 512], F32)
                psP = pst("psP", [128, 512], F32)
                psM = [pst("psM%d" % i, [128, 512], F32) for i in range(2)]

                for i in range(2):
                    for (tl, nm) in ((qeL, 'qeL'), (qeH, 'qeH'), (keL, 'keL'), (keH, 'keH')):
                        op('dve', lambda e, tl=tl, i=i: e.memset(tl[i][:], 0.0), writes=['%s%d' % (nm, i)])
                for kc in range(8):
                    op('pool', lambda e, kc=kc: e.dma_start(out=woutb[:, kc, :], in_=w_out[l, kc * 128:(kc + 1) * 128, :]), writes=['woutb%d' % kc], dma=True)
                op('sp', lambda e: e.dma_start(out=ggla[:], in_=g_glaT[l, :, :]), writes=['ggla'], dma=True)
                for h4 in range(4):
                    op('dve', lambda e, h4=h4: e.tensor_scalar(out=woutb[:, 4 + h4, :], in0=woutb[:, 4 + h4, :], scalar1=ggla[:, h4:h4 + 1], scalar2=None, op0=ALU.mult),
                       reads=['ggla', 'woutb%d' % (4 + h4)], writes=['woutb%d' % (4 + h4)])
                WOK = ['woutb%d' % kc for kc in range(8)]
                op('pool', lambda e: e.dma_start(out=wspb[:], in_=w_spT[l, :, :, :].rearrange("g q p -> q g p")), writes=['wspb'], dma=True)
                op('sp', lambda e: e.dma_start(out=bsp[:], in_=b_sp[l:l + 1, :].to_broadcast([128, 512])), writes=['bsp'], dma=True)
                for ms in range(3):
                    op('sp', lambda e, ms=ms: e.dma_start(out=g1rep[:, ms, :], in_=MS[l, ms:ms + 1, 2 * D:3 * D].to_broadcast([128, D])), writes=['g1rep'], dma=True)

                for s in range(NS):
                    for j in range(TPS):
                        tt = s * TPS + j
                        r0 = tt * 128
                        b = j % 2
                        op('sp', lambda e, b=b, r0=r0: e.dma_start(out=la[b][:], in_=LA[r0:r0 + 128, :]), writes=['la%d' % b], dma=True)
                        op('sp', lambda e, b=b, r0=r0: e.dma_start(out=ktm[b][:], in_=Kd[r0:r0 + 128, :]), writes=['ktm%d' % b], dma=True)
                        op('sp', lambda e, b=b, r0=r0: e.dma_start(out=vtm[b][:], in_=Vd[r0:r0 + 128, :]), writes=['vtm%d' % b], dma=True)
                        op('pe', lambda e, b=b: e.matmul(psB[:, 0:256], lhsT=cst32[:, SU], rhs=la[b][:, 0:256], start=True, stop=True), reads=['la%d' % b, 'cst32'], writes=['psB'])
                        op('pe', lambda e, b=b: e.matmul(psB[:, 256:512], lhsT=cst32[:, SLW], rhs=la[b][:, 256:512], start=True, stop=True), reads=['la%d' % b, 'cst32'], writes=['psB'])
                        op('act', lambda e, b=b: e.activation(out=eB[b][:], in_=psB[:, :], func=AF.Exp), reads=['psB'], writes=['eB%d' % b])
                        op('dve', lambda e, b=b: e.tensor_tensor(out=kd[b][:, :].rearrange("p (a c) -> p a c", a=2), in0=eB[b][:, :].rearrange("p (a c) -> p a c", a=2),
                                                                 in1=ktm[b][:, :].unsqueeze(1).to_broadcast([128, 2, 256]), op=ALU.mult),
                           reads=['eB%d' % b, 'ktm%d' % b], writes=['kd%d' % b])
                        for idx in range(4):
                            dr, hp = divmod(idx, 2)
                            pv = psKV[dr][:, hp * 256:(hp + 1) * 256]
                            op('pe', lambda e, b=b, pv=pv, dr=dr, hp=hp: e.matmul(pv, lhsT=kd[b][:, dr * 256 + hp * 128: dr * 256 + hp * 128 + 128],
                                                                                  rhs=vtm[b][:, hp * 256:(hp + 1) * 256], start=True, stop=True),
                               reads=['kd%d' % b, 'vtm%d' % b], writes=['psKV%d' % dr])
                            if dr == 0:
                                op('act', lambda e, pv=pv, j=j, idx=idx: e.activation(out=KVs[0:64, j * 4 + idx, :], in_=pv[0:64, 0:128], func=AF.Copy),
                                   reads=['psKV%d' % dr], writes=['KVa%d_%d' % (j, idx)])
                                op('act', lambda e, pv=pv, j=j, idx=idx: e.activation(out=KVs[64:128, j * 4 + idx, :], in_=pv[64:128, 128:256], func=AF.Copy),
                                   reads=['psKV%d' % dr], writes=['KVb%d_%d' % (j, idx)])
                            else:
                                op('dve', lambda e, pv=pv, j=j, idx=idx: e.tensor_copy(out=KVs[0:64, j * 4 + idx, :], in_=pv[0:64, 0:128]),
                                   reads=['psKV%d' % dr], writes=['KVa%d_%d' % (j, idx)])
                                op('dve', lambda e, pv=pv, j=j, idx=idx: e.tensor_copy(out=KVs[64:128, j * 4 + idx, :], in_=pv[64:128, 128:256]),
                                   reads=['psKV%d' % dr], writes=['KVb%d_%d' % (j, idx)])
                            op('pe', lambda e, b=b, idx=idx, dr=dr, hp=hp: e.matmul(psD[:, idx:idx + 1], lhsT=la[b][:, dr * 256 + hp * 128: dr * 256 + hp * 128 + 128],
                                                                                    rhs=cst32[:, 5 * 128:5 * 128 + 1], start=True, stop=True),
                               reads=['la%d' % b, 'cst32'], writes=['psD'])
                        op('act', lambda e, j=j: e.activation(out=dec[:, j * 4:(j + 1) * 4], in_=psD[:, 0:4], func=AF.Exp), reads=['psD'], writes=['dec%d' % j])
                    op('dve', lambda e: e.memset(Sst[:], 0.0), writes=['Sst0', 'Sst1', 'Sst2', 'Sst3'])
                    order_f = list(range(TPS))
                    order_b = [1, 0] + list(range(TPS - 1, 1, -1))
                    for dr, order in ((0, order_f), (1, order_b)):
                        for j in order:
                            for hp in range(2):
                                idx = dr * 2 + hp
                                op('pool', lambda e, j=j, idx=idx: e.tensor_copy(out=Sprev[:, j * 4 + idx, :], in_=Sst[:, idx, :]),
                                   reads=['Sst%d' % idx], writes=['Sprev%d_%d' % (j, idx)])
                                op('dve', lambda e, j=j, idx=idx: e.scalar_tensor_tensor(out=Sst[:, idx, :], in0=Sst[:, idx, :], scalar=dec[:, j * 4 + idx: j * 4 + idx + 1],
                                                                                         in1=KVs[:, j * 4 + idx, :], op0=ALU.mult, op1=ALU.add),
                                   reads=['Sst%d' % idx, 'dec%d' % j, 'KVa%d_%d' % (j, idx), 'KVb%d_%d' % (j, idx)], writes=['Sst%d' % idx])
                    def p2_front(j):
                        tt = s * TPS + j
                        r0 = tt * 128
                        b = j % 2
                        ms = mset(tt)
                        op('sp', lambda e, b=b, r0=r0: e.dma_start(out=la[b][:], in_=LA[r0:r0 + 128, :]), writes=['la%d' % b], dma=True)
                        op('sp', lambda e, b=b, r0=r0: e.dma_start(out=vtm[b][:], in_=Vd[r0:r0 + 128, :]), writes=['vtm%d' % b], dma=True)
                        op('sp', lambda e, b=b, r0=r0: e.dma_start(out=qTt[b][:], in_=QT[:, :, r0:r0 + 128].rearrange("h p t -> p h t")), writes=['qT%d' % b], dma=True)
                        op('sp', lambda e, b=b, r0=r0: e.dma_start(out=kTt[b][:], in_=KT[:, :, r0:r0 + 128].rearrange("h p t -> p h t")), writes=['kT%d' % b], dma=True)
                        op('sp', lambda e, b=b, r0=r0: e.dma_start(out=rTt[b][:], in_=RT[:, :, r0:r0 + 128].rearrange("h p t -> p h t")), writes=['rT%d' % b], dma=True)
                        op('sp', lambda e, b=b, r0=r0: e.dma_start(out=uTt[b][:], in_=UT[:, :, r0:r0 + 128].rearrange("h p t -> p h t")), writes=['uT%d' % b], dma=True)
                        op('sp', lambda e, b=b, r0=r0: e.dma_start(out=vat[b][:], in_=VA[r0:r0 + 128, :]), writes=['va%d' % b], dma=True)
                        op('sp', lambda e, b=b, r0=r0: e.dma_start(out=hh[b][:], in_=Hsrc[r0:r0 + 128, :]), writes=['hh%d' % b], dma=True)
                        for idx in range(4):
                            dr, hp = divmod(idx, 2)
                            msk = TRI if dr == 0 else TRIT
                            op('pe', lambda e, b=b, idx=idx, dr=dr, hp=hp, msk=msk: e.matmul(psB[:, idx * 128:(idx + 1) * 128],
                                                                                             lhsT=la[b][:, dr * 256 + hp * 128: dr * 256 + hp * 128 + 128],
                                                                                             rhs=cst32[:, msk], start=True, stop=True),
                               reads=['la%d' % b, 'cst32'], writes=['psB'])
                        op('act', lambda e, b=b: e.activation(out=Ep[b][:], in_=psB[:, :], func=AF.Exp), reads=['psB'], writes=['Ep%d' % b])
                        op('act', lambda e, b=b: e.activation(out=Em[b][:], in_=psB[:, :], func=AF.Exp, scale=-1.0), reads=['psB'], writes=['Em%d' % b])
                        for (rows_, qd, kd_, sfx) in ((slice(0, 64), qeL, keL, 'L'), (slice(64, 128), qeH, keH, 'H')):
                            op('dve', lambda e, b=b, rows_=rows_, qd=qd: e.tensor_tensor(out=qd[b][rows_, :].rearrange("p (a c) -> p a c", a=2),
                                                                                         in0=Ep[b][rows_, :].rearrange("p (a c) -> p a c", a=2),
                                                                                         in1=qTt[b][rows_, :, :].rearrange("p h t -> p (h t)").unsqueeze(1).to_broadcast([64, 2, 256]), op=ALU.mult),
                               reads=['Ep%d' % b, 'qT%d' % b], writes=['qe%s%d' % (sfx, b)])
                            op('pool', lambda e, b=b, rows_=rows_, kd_=kd_: e.tensor_tensor(out=kd_[b][rows_, :].rearrange("p (a c) -> p a c", a=2),
                                                                                            in0=Em[b][rows_, :].rearrange("p (a c) -> p a c", a=2),
                                                                                            in1=kTt[b][rows_, :, :].rearrange("p h t -> p (h t)").unsqueeze(1).to_broadcast([64, 2, 256]), op=ALU.mult),
                               reads=['Em%d' % b, 'kT%d' % b], writes=['ke%s%d' % (sfx, b)])
                        for dr in range(2):
                            for h4 in range(4):
                                hp, par = divmod(h4, 2)
                                rows = slice(par * 64, (par + 1) * 64)
                                cs = slice((dr * 2 + hp) * 128, (dr * 2 + hp + 1) * 128)
                                kx = keL if par == 0 else keH
                                qx = qeL if par == 0 else qeH
                                sfx = 'L' if par == 0 else 'H'
                                op('pe', lambda e, b=b, dr=dr, h4=h4, cs=cs, kx=kx, qx=qx: e.matmul(psKV[dr][:, h4 * 128:(h4 + 1) * 128], lhsT=kx[b][:, cs], rhs=qx[b][:, cs],
                                                                                                    start=True, stop=True),
                                   reads=['ke%s%d' % (sfx, b), 'qe%s%d' % (sfx, b)], writes=['psKV%d' % dr], sig=(h4 == 3))
                        op('dve', lambda e, b=b: e.tensor_tensor(out=atf[b][:, :].rearrange("p (a c) -> p a c", a=4), in0=psKV[0][:, :].rearrange("p (a c) -> p a c", a=4),
                                                                 in1=cst32[:, TRI].unsqueeze(1).to_broadcast([128, 4, 128]), op=ALU.mult),
                           reads=['psKV0', 'cst32'], writes=['atf%d' % b])
                        op('dve', lambda e, b=b: e.tensor_tensor(out=atb[b][:, :].rearrange("p (a c) -> p a c", a=4), in0=psKV[1][:, :].rearrange("p (a c) -> p a c", a=4),
                                                                 in1=cst32[:, TRIT].unsqueeze(1).to_broadcast([128, 4, 128]), op=ALU.mult),
                           reads=['psKV1', 'cst32'], writes=['atb%d' % b])

                    def p2_back(j):
                        tt = s * TPS + j
                        r0 = tt * 128
                        b = j % 2
                        ms = mset(tt)
                        for h4 in range(4):
                            hp, par = divmod(h4, 2)
                            rows = slice(par * 64, (par + 1) * 64)
                            hs_ = slice(h4 * 128, (h4 + 1) * 128)
                            op('pe', lambda e, b=b, hs_=hs_: e.matmul(psO[:, hs_], lhsT=vtm[b][:, hs_], rhs=atf[b][:, hs_], start=True, stop=False),
                               reads=['vtm%d' % b, 'atf%d' % b], writes=['psO'], sig=False)
                            op('pe', lambda e, b=b, hs_=hs_: e.matmul(psO[:, hs_], lhsT=vtm[b][:, hs_], rhs=atb[b][:, hs_], start=False, stop=False),
                               reads=['vtm%d' % b, 'atb%d' % b], writes=['psO'], sig=False)
                            for dr in range(2):
                                cs = slice((dr * 2 + hp) * 128, (dr * 2 + hp + 1) * 128)
                                qx = qeL if par == 0 else qeH
                                sfx = 'L' if par == 0 else 'H'
                                op('pe', lambda e, b=b, hs_=hs_, cs=cs, dr=dr, hp=hp, j=j, qx=qx: e.matmul(psO[:, hs_], lhsT=Sprev[:, j * 4 + dr * 2 + hp, :], rhs=qx[b][:, cs],
                                                                                                           start=False, stop=(dr == 1)),
                                   reads=['Sprev%d_%d' % (j, dr * 2 + hp), 'qe%s%d' % (sfx, b)], writes=['psO'], sig=(h4 == 3 and dr == 1))
                        op('act', lambda e, b=b: e.activation(out=sq[b][:], in_=psO[:, :], func=AF.Square), reads=['psO'], writes=['sq%d' % b])
                        op('pe', lambda e, b=b: e.matmul(psD[:, :], lhsT=cstb[:, ONES], rhs=sq[b][:], start=True, stop=True), reads=['sq%d' % b, 'cstb'], writes=['psD'])
                        op('dve', lambda e, b=b: e.tensor_scalar(out=rs[b][:], in0=psD[:, :], scalar1=1.0 / 128, scalar2=EPS, op0=ALU.mult, op1=ALU.add), reads=['psD'], writes=['rs%d' % b])
                        op('act', lambda e, b=b: e.activation(out=rs[b][:], in_=rs[b][:], func=AF.Sqrt), reads=['rs%d' % b], writes=['rs%d' % b])
                        op('dve', lambda e, b=b: e.reciprocal(out=rs[b][:], in_=rs[b][:]), reads=['rs%d' % b], writes=['rs%d' % b])
                        op('dve', lambda e, b=b: e.tensor_tensor(out=tmp[b][:], in0=psO[:, :], in1=rs[b][:], op=ALU.mult), reads=['psO', 'rs%d' % b], writes=['tmp%d' % b])
                        op('pool', lambda e, b=b: e.tensor_tensor(out=bo[b][:], in0=tmp[b][:], in1=rTt[b][:, :, :].rearrange("p h t -> p (h t)"), op=ALU.mult),
                           reads=['tmp%d' % b, 'rT%d' % b], writes=['bo%d' % b])
                        for g4 in range(4):
                            gs = slice(g4 * 128, (g4 + 1) * 128)
                            op('pe', lambda e, b=b, gs=gs, g4=g4: e.matmul(psP[:, gs], lhsT=vat[b][:, gs], rhs=wspb[:, g4, :], start=True, stop=True),
                               reads=['va%d' % b, 'wspb'], writes=['psP'])
                        op('dve', lambda e, b=b: e.tensor_tensor(out=tmp2[b][:], in0=psP[:, :], in1=bsp[:], op=ALU.add), reads=['psP', 'bsp'], writes=['tmp2%d' % b])
                        op('pool', lambda e, b=b: e.tensor_tensor(out=ao[b][:], in0=tmp2[b][:], in1=uTt[b][:, :, :].rearrange("p h t -> p (h t)"), op=ALU.mult),
                           reads=['tmp2%d' % b, 'uT%d' % b], writes=['ao%d' % b])
                        for half in range(2):
                            for kc in range(8):
                                src = ao[b] if kc < 4 else bo[b]
                                ks = slice((kc % 4) * 128, (kc % 4 + 1) * 128)
                                op('pe', lambda e, half=half, kc=kc, src=src, ks=ks: e.matmul(psM[half][:, :], lhsT=src[:, ks], rhs=woutb[:, kc, half * 512:(half + 1) * 512],
                                                                                              start=(kc == 0), stop=(kc == 7)),
                                   sig=(kc == 7), reads=['ao%d' % b, 'bo%d' % b] + WOK, writes=['psM%d' % half])
                            op('dve', lambda e, b=b, half=half, ms=ms: e.tensor_tensor(out=hm[b][:, half * 512:(half + 1) * 512], in0=psM[half][:, :],
                                                                                       in1=g1rep[:, ms, half * 512:(half + 1) * 512], op=ALU.mult),
                               reads=['psM%d' % half, 'g1rep'], writes=['hm%d_%d' % (b, half)])
                        op('pool', lambda e, b=b: e.tensor_tensor(out=hm[b][:], in0=hm[b][:], in1=hh[b][:], op=ALU.add),
                           reads=['hm%d_0' % b, 'hm%d_1' % b, 'hh%d' % b], writes=['hm%d_0' % b, 'hm%d_1' % b])
                        op('sp', lambda e, b=b, r0=r0: e.dma_start(out=H[r0:r0 + 128, :], in_=hm[b][:]), reads=['hm%d_0' % b, 'hm%d_1' % b], writes=['Hrow%d' % tt], dma=True)

                    p2_front(0)
                    for j in range(TPS):
                        if j + 1 < TPS:
                            p2_front(j + 1)
                        p2_back(j)
                S.flush()
            if stop_after == 'p2':
                break
            with ExitStack() as es:
                def sb(name, shape, dt):
                    return es.enter_context(nc.sbuf_tensor(("L%d_p3_" % l) + name, shape, dt))

                def pst(name, shape, dt):
                    return es.enter_context(nc.psum_tensor(("L%d_p3_" % l) + name, shape, dt))
                wrb = sb("wrb", [128, 8, 72], BF16)
                brep = sb("brep", [128, 72], F32)
                A2rep = sb("A2rep", [128, 3, D], F32)
                sh2rep = sb("sh2rep", [128, 3, D], F32)
                gn2rep = sb("gn2rep", [128, D], F32)
                hn2all = sb("hn2all", [128, NT, D], BF16)
                ek = sb("ek", [128, NT * 2], F32)
                rank = sb("rank", [128, NT * 2], F32)
                base = sb("base", [128, 64], F32)
                big = sb("big", [128, NBLK * 64], F32)
                h2 = [sb("h2_%d" % i, [128, D], F32) for i in range(2)]
                hs2 = [sb("hs2_%d" % i, [128, D], F32) for i in range(2)]
                junk = sb("junk", [128, D], BF16)
                hT2 = [sb("hT2_%d" % i, [128, D], BF16) for i in range(2)]
                st = [sb("st%d" % i, [128, 4], F32) for i in range(2)]
                Lg = [sb("Lg%d" % i, [128, 72], F32) for i in range(2)]
                R = [sb("R%d" % i, [128, 16], F32) for i in range(2)]
                eg = [sb("eg%d" % i, [128, 8], F32) for i in range(2)]
                ohg = [sb("ohg%d" % i, [128, 8], F32) for i in range(2)]
                sel3 = [sb("sel3_%d" % i, [128, 64], F32) for i in range(2)]
                lsel = [sb("lsel%d" % i, [128, 8], F32) for i in range(2)]
                oh1 = [sb("oh1_%d" % i, [128, 8], F32) for i in range(2)]
                oh2 = [sb("oh2_%d" % i, [128, 8], F32) for i in range(2)]
                l2 = [sb("l2_%d" % i, [128, 8], F32) for i in range(2)]
                oh64 = [[sb("oh64_%d_%d" % (k, i), [128, 64], F32) for i in range(2)] for k in range(2)]
                t64 = [sb("t64_%d" % i, [128, 64], F32) for i in range(2)]
                Abf = [sb("Abf%d" % i, [128, 64], BF16) for i in range(2)]
                pos = [sb("pos%d" % i, [128, 64], F32) for i in range(2)]
                cntT = sb("cntT", [64, 128], F32)
                cmpT = sb("cmpT", [64, 128], F32)
                nblk = sb("nblk", [64, 1], F32)
                padT = sb("padT", [64, 128], F32)
                padse = sb("padse", [128, 128], F32)
                blke_f = sb("blke_f", [128, NBLK], F32)
                dest_f = sb("dest_f", [128, NT * 2], F32)
                psT = [pst("psT%d" % i, [128, D], BF16) for i in range(2)]
                psL = pst("psL", [128, 512], F32)
                psC = pst("psC", [128, 512], F32)
                psCT = pst("psCT", [128, 512], F32)
                psE = pst("psE", [128, 512], F32)
                IOTA = slice(768, 832)
                BLKS = slice(832, 832 + NBLK)
                THR = slice(832, 960)

                op('pool', lambda e: e.dma_start(out=wrb[:], in_=w_r[l, :, :].rearrange("(kc p) c -> p kc c", p=128)), writes=['wrb'], dma=True)
                op('sp', lambda e: e.dma_start(out=brep[:], in_=b_r[l:l + 1, :].to_broadcast([128, 72])), writes=['brep'], dma=True)
                op('sp', lambda e: e.dma_start(out=gn2rep[:], in_=gn2[l:l + 1, :].to_broadcast([128, D])), writes=['gn2rep'], dma=True)
                for ms in range(3):
                    op('sp', lambda e, ms=ms: e.dma_start(out=sh2rep[:, ms, :], in_=MS[l, ms:ms + 1, 3 * D:4 * D].to_broadcast([128, D])), writes=['sh2rep%d' % ms], dma=True)
                    op('sp', lambda e, ms=ms: e.dma_start(out=A2rep[:, ms, :], in_=MS[l, ms:ms + 1, 4 * D:5 * D].to_broadcast([128, D])), writes=['A2rep%d' % ms], dma=True)
                    op('dve', lambda e, ms=ms: e.scalar_tensor_tensor(out=A2rep[:, ms, :], in0=A2rep[:, ms, :], scalar=1.0, in1=gn2rep[:], op0=ALU.add, op1=ALU.mult),
                       reads=['A2rep%d' % ms, 'gn2rep'], writes=['A2rep%d' % ms])
                op('dve', lambda e: e.memset(base[:], 0.0), writes=['base'])
                def p3_front(tt):
                    b = tt % 2
                    r0 = tt * 128
                    ms = mset(tt)
                    Rb, Lgb = R[b], Lg[b]
                    rk = 'R%d' % b
                    op('sp', lambda e, b=b, r0=r0: e.dma_start(out=h2[b][:], in_=H[r0:r0 + 128, :]), writes=['h2_%d' % b], dma=True)
                    op('dve', lambda e, b=b: e.memset(st[b][:, 0:1], 0.0), writes=['ss%d' % b])
                    op('act', lambda e, b=b: e.activation(out=junk[:], in_=h2[b][:], func=AF.Square, accum_out=st[b][:, 0:1]),
                       reads=['h2_%d' % b, 'ss%d' % b], writes=['junk', 'ss%d' % b])
                    rstd_from_ss(st[b][:, 0:1], st[b][:, 1:2], 'ss%d' % b, 'rs%d' % b, D)
                    op('dve', lambda e, b=b: e.tensor_scalar(out=hs2[b][:], in0=h2[b][:], scalar1=st[b][:, 1:2], scalar2=None, op0=ALU.mult),
                       reads=['h2_%d' % b, 'rs%d' % b], writes=['hs2_%d' % b])
                    op('pool', lambda e, b=b, ms=ms: e.tensor_tensor(out=hs2[b][:], in0=hs2[b][:], in1=A2rep[:, ms, :], op=ALU.mult),
                       reads=['hs2_%d' % b, 'A2rep%d' % ms], writes=['hs2_%d' % b])
                    op('pool', lambda e, b=b, ms=ms, tt=tt: e.tensor_tensor(out=hn2all[:, tt, :], in0=hs2[b][:], in1=sh2rep[:, ms, :], op=ALU.add),
                       reads=['hs2_%d' % b, 'sh2rep%d' % ms], writes=['hn2_%d' % tt])
                    for kc in range(8):
                        op('pe', lambda e, b=b, kc=kc, tt=tt: e.transpose(out=psT[b][:, kc * 128:(kc + 1) * 128], in_=hn2all[:, tt, kc * 128:(kc + 1) * 128], identity=cstb[:, IDENT]),
                           reads=['hn2_%d' % tt, 'cstb'], writes=['psT%d' % b], sig=(kc == 7))
                    op('act', lambda e, b=b: e.activation(out=hT2[b][:], in_=psT[b][:, :], func=AF.Copy), reads=['psT%d' % b], writes=['hT2_%d' % b])
                    for kc in range(8):
                        op('pe', lambda e, b=b, kc=kc: e.matmul(psL[:, 0:72], lhsT=hT2[b][:, kc * 128:(kc + 1) * 128], rhs=wrb[:, kc, :], start=(kc == 0), stop=(kc == 7)),
                           sig=(kc == 7), reads=['hT2_%d' % b, 'wrb'], writes=['psL'])
                    op('dve', lambda e, Lgb=Lgb: e.tensor_tensor(out=Lgb[:], in0=psL[:, 0:72], in1=brep[:], op=ALU.add), reads=['psL', 'brep'], writes=['Lg%d' % b])

                def p3_route(tt):
                    b = tt % 2
                    r0 = tt * 128
                    ms = mset(tt)
                    Rb, Lgb = R[b], Lg[b]
                    rk = 'R%d' % b
                    LK = 'Lg%d' % b
                    yield
                    op('dve', lambda e, Rb=Rb, Lgb=Lgb: e.tensor_reduce(out=Rb[:, 0:1], in_=Lgb[:, 0:8], axis=AX.X, op=ALU.max), reads=[LK], writes=[rk])
                    yield
                    op('dve', lambda e, Rb=Rb: e.tensor_scalar(out=Rb[:, 1:2], in0=Rb[:, 0:1], scalar1=-1.0, scalar2=None, op0=ALU.mult), reads=[rk], writes=[rk])
                    yield
                    op('dve', lambda e, Rb=Rb: e.memset(Rb[:, 2:3], 0.0), reads=[rk], writes=[rk])
                    yield
                    op('act', lambda e, Rb=Rb, Lgb=Lgb, b=b: e.activation(out=eg[b][:], in_=Lgb[:, 0:8], func=AF.Exp, bias=Rb[:, 1:2], scale=1.0, accum_out=Rb[:, 2:3]),
                       reads=[LK, rk], writes=[rk, 'eg%d' % b])
                    yield
                    op('dve', lambda e, Rb=Rb: e.reciprocal(out=Rb[:, 3:4], in_=Rb[:, 2:3]), reads=[rk], writes=[rk])
                    yield
                    op('dve', lambda e, Rb=Rb, Lgb=Lgb, b=b: e.tensor_scalar(out=ohg[b][:], in0=Lgb[:, 0:8], scalar1=Rb[:, 0:1], scalar2=None, op0=ALU.is_equal),
                       reads=[LK, rk], writes=['ohg%d' % b])
                    yield
                    op('dve', lambda e, Lgb=Lgb, b=b: e.tensor_tensor(out=sel3[b][:, :].rearrange("p (g x) -> p g x", g=8), in0=Lgb[:, 8:72].rearrange("p (g x) -> p g x", g=8),
                                                                      in1=ohg[b][:, :].unsqueeze(2).to_broadcast([128, 8, 8]), op=ALU.mult),
                       reads=[LK, 'ohg%d' % b], writes=['sel3_%d' % b])
                    yield
                    op('dve', lambda e, b=b: e.tensor_reduce(out=lsel[b][:], in_=sel3[b][:, :].rearrange("p (g x) -> p x g", g=8), axis=AX.X, op=ALU.add),
                       reads=['sel3_%d' % b], writes=['lsel%d' % b])
                    yield
                    op('dve', lambda e, Rb=Rb, b=b: e.tensor_reduce(out=Rb[:, 4:5], in_=lsel[b][:], axis=AX.X, op=ALU.max), reads=['lsel%d' % b, rk], writes=[rk])
                    yield
                    op('dve', lambda e, Rb=Rb, b=b: e.tensor_scalar(out=oh1[b][:], in0=lsel[b][:], scalar1=Rb[:, 4:5], scalar2=None, op0=ALU.is_equal),
                       reads=['lsel%d' % b, rk], writes=['oh1_%d' % b])
                    yield
                    op('dve', lambda e, b=b: e.scalar_tensor_tensor(out=l2[b][:], in0=oh1[b][:], scalar=-1e30, in1=lsel[b][:], op0=ALU.mult, op1=ALU.add),
                       reads=['oh1_%d' % b, 'lsel%d' % b], writes=['l2_%d' % b])
                    yield
                    op('dve', lambda e, Rb=Rb, b=b: e.tensor_reduce(out=Rb[:, 5:6], in_=l2[b][:], axis=AX.X, op=ALU.max), reads=['l2_%d' % b, rk], writes=[rk])
                    yield
                    op('dve', lambda e, Rb=Rb, b=b: e.tensor_scalar(out=oh2[b][:], in0=l2[b][:], scalar1=Rb[:, 5:6], scalar2=None, op0=ALU.is_equal),
                       reads=['l2_%d' % b, rk], writes=['oh2_%d' % b])
                    yield
                    op('dve', lambda e, Rb=Rb: e.tensor_tensor(out=Rb[:, 6:7], in0=Rb[:, 5:6], in1=Rb[:, 4:5], op=ALU.subtract), reads=[rk], writes=[rk])
                    yield
                    op('act', lambda e, Rb=Rb: e.activation(out=Rb[:, 7:8], in_=Rb[:, 6:7], func=AF.Exp), reads=[rk], writes=[rk])
                    yield
                    op('dve', lambda e, Rb=Rb: e.tensor_scalar(out=Rb[:, 7:8], in0=Rb[:, 7:8], scalar1=1.0, scalar2=None, op0=ALU.add), reads=[rk], writes=[rk])
                    yield
                    op('dve', lambda e, Rb=Rb: e.reciprocal(out=Rb[:, 7:8], in_=Rb[:, 7:8]), reads=[rk], writes=[rk])
                    yield
                    op('dve', lambda e, Rb=Rb, tt=tt: e.tensor_tensor(out=gate_f[:, tt * 2:tt * 2 + 1], in0=Rb[:, 7:8], in1=Rb[:, 3:4], op=ALU.mult), reads=[rk], writes=['gate%d' % tt])
                    yield
                    op('dve', lambda e, Rb=Rb, tt=tt: e.tensor_tensor(out=gate_f[:, tt * 2 + 1:tt * 2 + 2], in0=Rb[:, 3:4], in1=gate_f[:, tt * 2:tt * 2 + 1], op=ALU.subtract),
                       reads=[rk, 'gate%d' % tt], writes=['gate%d' % tt])
                    for k, ohk, ohkey in ((0, oh1, 'oh1_%d' % b), (1, oh2, 'oh2_%d' % b)):
                        yield
                        op('dve', lambda e, b=b, k=k, ohk=ohk: e.tensor_tensor(out=oh64[k][b][:, :].rearrange("p (g x) -> p g x", g=8),
                                                                                in0=ohg[b][:, :].unsqueeze(2).to_broadcast([128, 8, 8]),
                                                                                in1=ohk[b][:, :].unsqueeze(1).to_broadcast([128, 8, 8]), op=ALU.mult),
                           reads=['ohg%d' % b, ohkey], writes=['oh64_%d_%d' % (k, b)])
                        yield
                        op('dve', lambda e, b=b, k=k: e.tensor_tensor(out=t64[b][:], in0=oh64[k][b][:], in1=cst32[:, IOTA], op=ALU.mult),
                           reads=['oh64_%d_%d' % (k, b), 'cst32'], writes=['t64_%d' % b])
                        yield
                        op('dve', lambda e, b=b, k=k, tt=tt: e.tensor_reduce(out=ek[:, tt * 2 + k:tt * 2 + k + 1], in_=t64[b][:], axis=AX.X, op=ALU.add),
                           reads=['t64_%d' % b], writes=['ek%d_%d' % (tt, k)])
                    yield
                    op('dve', lambda e, b=b: e.tensor_tensor(out=Abf[b][:], in0=oh64[0][b][:], in1=oh64[1][b][:], op=ALU.add),
                       reads=['oh64_0_%d' % b, 'oh64_1_%d' % b], writes=['Abf%d' % b])

                def p3_tail(tt):
                    b = tt % 2
                    r0 = tt * 128
                    ms = mset(tt)
                    Rb, Lgb = R[b], Lg[b]
                    rk = 'R%d' % b
                    op('pe', lambda e, b=b: e.matmul(psC[:, 0:64], lhsT=cstb[:, SLW], rhs=Abf[b][:], start=True, stop=True), reads=['Abf%d' % b, 'cstb'], writes=['psC'])
                    op('pe', lambda e, b=b: e.matmul(psC[:, 64:128], lhsT=cstb[:, ONES], rhs=Abf[b][:], start=True, stop=True), reads=['Abf%d' % b, 'cstb'], writes=['psC'])
                    op('pe', lambda e, b=b, tt=tt: e.matmul(psCT[0:64, 0:128], lhsT=Abf[b][:], rhs=cstb[:, ONES], start=(tt == 0), stop=(tt == NT - 1)),
                       reads=['Abf%d' % b, 'cstb'], writes=['psCT'])
                    op('dve', lambda e, b=b: e.tensor_tensor(out=pos[b][:], in0=psC[:, 0:64], in1=base[:], op=ALU.add), reads=['psC', 'base'], writes=['pos%d' % b])
                    for k in range(2):
                        op('dve', lambda e, b=b, k=k: e.tensor_tensor(out=t64[b][:], in0=oh64[k][b][:], in1=pos[b][:], op=ALU.mult),
                           reads=['oh64_%d_%d' % (k, b), 'pos%d' % b], writes=['t64_%d' % b])
                        op('dve', lambda e, b=b, k=k, tt=tt: e.tensor_reduce(out=rank[:, tt * 2 + k:tt * 2 + k + 1], in_=t64[b][:], axis=AX.X, op=ALU.add),
                           reads=['t64_%d' % b], writes=['rank%d_%d' % (tt, k)])
                    op('dve', lambda e: e.tensor_tensor(out=base[:], in0=psC[:, 64:128], in1=base[:], op=ALU.add), reads=['psC', 'base'], writes=['base'])

                for tp in range(0, NT, 2):
                    p3_front(tp)
                    p3_front(tp + 1)
                    gens = [p3_route(tp), p3_route(tp + 1)]
                    while gens:
                        for g_ in list(gens):
                            try:
                                next(g_)
                            except StopIteration:
                                gens.remove(g_)
                    p3_tail(tp)
                    p3_tail(tp + 1)
                op('dve', lambda e: e.tensor_copy(out=cntT[:], in_=psCT[0:64, 0:128]), reads=['psCT'], writes=['cntT'])
                op('dve', lambda e: e.tensor_tensor(out=cmpT[:], in0=cntT[:], in1=cst32[0:64, THR], op=ALU.is_gt), reads=['cntT', 'cst32'], writes=['cmpT'])
                op('dve', lambda e: e.tensor_reduce(out=nblk[:], in_=cmpT[:], axis=AX.X, op=ALU.add), reads=['cmpT'], writes=['nblk'])
                op('dve', lambda e: e.tensor_scalar(out=padT[:], in0=cst32[0:64, ONES], scalar1=nblk[:, 0:1], scalar2=float(MB), op0=ALU.mult, op1=ALU.mult),
                   reads=['nblk', 'cst32'], writes=['padT'])
                op('pe', lambda e: e.matmul(psE[:, 0:64], lhsT=padT[:], rhs=cst32[0:64, 4 * 128:4 * 128 + 64], start=True, stop=True), reads=['padT', 'cst32'], writes=['psE'])
                op('pe', lambda e: e.matmul(psE[:, 64:128], lhsT=padT[:], rhs=cst32[0:64, 128:192], start=True, stop=True), reads=['padT', 'cst32'], writes=['psE'])
                op('dve', lambda e: e.tensor_copy(out=padse[:], in_=psE[:, 0:128]), reads=['psE'], writes=['padse'])
                op('dve', lambda e: e.tensor_tensor(out=big[:, 0:NBLK * 64].rearrange("p (b x) -> p b x", x=64),
                                                    in0=padse[:, 64:128].unsqueeze(1).to_broadcast([128, NBLK, 64]),
                                                    in1=cst32[:, BLKS].unsqueeze(2).to_broadcast([128, NBLK, 64]), op=ALU.is_le),
                   reads=['padse', 'cst32'], writes=['big'])
                op('dve', lambda e: e.tensor_reduce(out=blke_f[:], in_=big[:, 0:NBLK * 64].rearrange("p (b x) -> p b x", x=64), axis=AX.X, op=ALU.add),
                   reads=['big'], writes=['blke_f'])
                op('dve', lambda e: e.tensor_scalar(out=blke_f[:], in0=blke_f[:], scalar1=63.0, scalar2=None, op0=ALU.min), reads=['blke_f'], writes=['blke_f'])
                op('dve', lambda e: e.tensor_scalar(out=blke_f[:], in0=blke_f[:], scalar1=128.0, scalar2=cst32[:, 960:961], op0=ALU.mult, op1=ALU.add),
                   reads=['blke_f', 'cst32'], writes=['blke_f'])
                op('dve', lambda e: e.tensor_scalar(out=blke_f[:], in0=blke_f[:], scalar1=float(l * 64 * 128), scalar2=None, op0=ALU.add), reads=['blke_f'], writes=['blke_f'])
                op('dve', lambda e: e.tensor_copy(out=widx_i[:], in_=blke_f[:]), reads=['blke_f'], writes=['widx_i'])
                op('dve', lambda e: e.tensor_tensor(out=big[:, 0:NT * 2 * 64].rearrange("p (b x) -> p b x", x=64),
                                                    in0=cst32[:, IOTA].unsqueeze(1).to_broadcast([128, NT * 2, 64]),
                                                    in1=ek[:, :].unsqueeze(2).to_broadcast([128, NT * 2, 64]), op=ALU.is_equal),
                   reads=['big', 'cst32'] + ['ek%d_%d' % (tt, k) for tt in range(NT) for k in range(2)], writes=['big'])
                op('dve', lambda e: e.tensor_tensor(out=big[:, 0:NT * 2 * 64].rearrange("p (b x) -> p b x", x=64),
                                                    in0=big[:, 0:NT * 2 * 64].rearrange("p (b x) -> p b x", x=64),
                                                    in1=padse[:, 0:64].unsqueeze(1).to_broadcast([128, NT * 2, 64]), op=ALU.mult),
                   reads=['big', 'padse'], writes=['big'])
                op('dve', lambda e: e.tensor_reduce(out=dest_f[:], in_=big[:, 0:NT * 2 * 64].rearrange("p (b x) -> p b x", x=64), axis=AX.X, op=ALU.add),
                   reads=['big'], writes=['dest_f'])
                op('dve', lambda e: e.tensor_tensor(out=dest_f[:], in0=dest_f[:], in1=rank[:], op=ALU.add),
                   reads=['dest_f'] + ['rank%d_%d' % (tt, k) for tt in range(NT) for k in range(2)], writes=['dest_f'])
                op('dve', lambda e: e.tensor_copy(out=dest_i[:], in_=dest_f[:]), reads=['dest_f'], writes=['dest_i'])
                for tt in range(NT):
                    for k in range(2):
                        op('pool', lambda e, tt=tt, k=k: e.indirect_dma_start(out=XB[:, :], out_offset=bass.IndirectOffsetOnAxis(ap=dest_i[:, tt * 2 + k:tt * 2 + k + 1], axis=0),
                                                                              in_=hn2all[:, tt, :], in_offset=None),
                           reads=['dest_i', 'hn2_%d' % tt], writes=['XBs%d_%d' % (tt, k)], dma=True)
                S.flush()
            if stop_after == 'p3':
                break

            with ExitStack() as es:
                def sb(name, shape, dt):
                    return es.enter_context(nc.sbuf_tensor(("L%d_p4_" % l) + name, shape, dt))

                def pst(name, shape, dt):
                    return es.enter_context(nc.psum_tensor(("L%d_p4_" % l) + name, shape, dt))
                NWB = 3
                w1b = [sb("w1b%d" % i, [128, 4096], BF16) for i in range(NWB)]
                w3b = [sb("w3b%d" % i, [128, 4096], BF16) for i in range(NWB)]
                w2b = [sb("w2b%d" % i, [128, 4096], BF16) for i in range(NWB)]
                xb = [sb("xb%d" % i, [128, 2, D], BF16) for i in range(2)]
                xT = [sb("xT%d" % i, [128, 8, MB], BF16) for i in range(2)]
                hT = [sb("hT%d" % i, [128, 4, MB], BF16) for i in range(2)]
                s1 = [sb("s1_%d" % i, [128, MB], F32) for i in range(2)]
                yst = [sb("yst%d" % i, [128, D], F32) for i in range(2)]
                psX = [pst("psX%d" % i, [128, D], BF16) for i in range(2)]
                psH = [pst("psH%d" % i, [128, 512], F32) for i in range(3)]
                psY = [pst("psY%d" % i, [128, 512], F32) for i in range(2)]
                w1v = w1.rearrange("l e (p kc) f -> (l e p) (kc f)", kc=8)
                w3v = w3.rearrange("l e (p kc) f -> (l e p) (kc f)", kc=8)
                w2v = w2.rearrange("l e (p fc) d -> (l e p) (fc d)", fc=4)
                ycnt = [0]

                def emit_loads(blk):
                    b = blk % 2
                    wb = blk % NWB
                    for (wv, wt, nm) in (((w1v, w1b[wb], 'w1b%d' % wb), (w3v, w3b[wb], 'w3b%d' % wb), (w2v, w2b[wb], 'w2b%d' % wb)) if not _DBG.get('nogather') else ()):
                        op('pool', lambda e, wv=wv, wt=wt, blk=blk: e.indirect_dma_start(out=wt[:, :], out_offset=None, in_=wv[:, :],
                                                                                         in_offset=bass.IndirectOffsetOnAxis(ap=widx_i[:, blk:blk + 1], axis=0)),
                           reads=['widx_i'], writes=[nm], dma=True)
                    op('sp', lambda e, b=b, blk=blk: e.dma_start(out=xb[b][:], in_=XB[blk * MB:(blk + 1) * MB, :].rearrange("(s p) d -> p s d", p=128)),
                       writes=['xb%d' % b], dma=True)

                def emit_T(blk):
                    b = blk % 2
                    for s_ in range(2):
                        for kc in range(8):
                            op('pe', lambda e, b=b, s_=s_, kc=kc: e.transpose(out=psX[s_][:, kc * 128:(kc + 1) * 128], in_=xb[b][:, s_, kc:D:8], identity=cstb[:, IDENT]),
                               reads=['xb%d' % b, 'cstb'], writes=['psX%d' % s_], sig=(kc == 7))
                        if s_ == 0:
                            f = lambda e, b=b, s_=s_: e.activation(out=xT[b][:, :, s_ * 128:(s_ + 1) * 128], in_=psX[s_][:, :].rearrange("p (kc t) -> p kc t", kc=8), func=AF.Copy)
                        else:
                            f = lambda e, b=b, s_=s_: e.tensor_copy(out=xT[b][:, :, s_ * 128:(s_ + 1) * 128], in_=psX[s_][:, :].rearrange("p (kc t) -> p kc t", kc=8))
                        op('act' if s_ == 0 else 'dve', f, reads=['psX%d' % s_], writes=['xT%d_%d' % (b, s_)])

                def emit_H(blk):
                    b = blk % 2
                    wb = blk % NWB
                    w1b3 = w1b[wb][:, :].rearrange("p (kc f) -> p kc f", kc=8)
                    w3b3 = w3b[wb][:, :].rearrange("p (kc f) -> p kc f", kc=8)
                    XK = ['xT%d_0' % b, 'xT%d_1' % b]
                    for fc in range(4):
                        hb = (blk * 4 + fc) % 3
                        for kc in range(8):
                            op('pe', lambda e, b=b, fc=fc, kc=kc, hb=hb, w1b3=w1b3: e.matmul(psH[hb][:, 0:MB], lhsT=w1b3[:, kc, fc:512:4], rhs=xT[b][:, kc, :], start=(kc == 0), stop=(kc == 7)),
                               sig=(kc == 7), reads=XK + ['w1b%d' % wb], writes=['psH%d' % hb])
                        for kc in range(8):
                            op('pe', lambda e, b=b, fc=fc, kc=kc, hb=hb, w3b3=w3b3: e.matmul(psH[hb][:, MB:2 * MB], lhsT=w3b3[:, kc, fc:512:4], rhs=xT[b][:, kc, :], start=(kc == 0), stop=(kc == 7)),
                               sig=(kc == 7), reads=XK + ['w3b%d' % wb], writes=['psH%d' % hb])
                        sb_ = (blk * 4 + fc) % 2
                        op('act', lambda e, hb=hb, sb_=sb_: e.activation(out=s1[sb_][:], in_=psH[hb][:, 0:MB], func=AF.Silu), reads=['psH%d' % hb], writes=['s1_%d' % sb_])
                        op('dve', lambda e, b=b, hb=hb, fc=fc, sb_=sb_: e.tensor_tensor(out=hT[b][:, fc, :], in0=s1[sb_][:], in1=psH[hb][:, MB:2 * MB], op=ALU.mult),
                           reads=['s1_%d' % sb_, 'psH%d' % hb], writes=['hT%d_%d' % (b, fc)])

                def emit_Y(blk):
                    b = blk % 2
                    wb = blk % NWB
                    w2b3 = w2b[wb][:, :].rearrange("p (fc d) -> p fc d", fc=4)
                    HKs = ['hT%d_%d' % (b, fc) for fc in range(4)]
                    for s_ in range(2):
                        for half in range(2):
                            yb_ = ycnt[0] % 2
                            ycnt[0] += 1
                            for fc in range(4):
                                op('pe', lambda e, b=b, s_=s_, half=half, fc=fc, yb_=yb_, w2b3=w2b3: e.matmul(psY[yb_][:, :], lhsT=hT[b][:, fc, s_ * 128:(s_ + 1) * 128],
                                                                                                               rhs=w2b3[:, fc, half * 512:(half + 1) * 512], start=(fc == 0), stop=(fc == 3)),
                                   sig=(fc == 3), reads=HKs + ['w2b%d' % wb], writes=['psY%d' % yb_])
                            if half == 0:
                                op('act', lambda e, s_=s_, yb_=yb_: e.activation(out=yst[s_][:, 0:512], in_=psY[yb_][:, :], func=AF.Copy), reads=['psY%d' % yb_], writes=['yst%d_0' % s_])
                            else:
                                op('dve', lambda e, s_=s_, yb_=yb_: e.tensor_copy(out=yst[s_][:, 512:1024], in_=psY[yb_][:, :]), reads=['psY%d' % yb_], writes=['yst%d_1' % s_])
                        op('sp', lambda e, s_=s_, blk=blk: e.dma_start(out=YB[blk * MB + s_ * 128: blk * MB + (s_ + 1) * 128, :], in_=yst[s_][:]),
                           reads=['yst%d_0' % s_, 'yst%d_1' % s_], writes=['YBrow'], dma=True)

                emit_loads(0)
                emit_T(0)
                emit_loads(1)
                for blk in range(NBLK):
                    emit_H(blk)
                    if blk + 1 < NBLK:
                        emit_T(blk + 1)
                    if blk + 2 < NBLK:
                        emit_loads(blk + 2)
                    emit_Y(blk)
                S.flush()
            if stop_after == 'p4':
                break

            last = (l == L - 1)
            with ExitStack() as es:
                def sb(name, shape, dt):
                    return es.enter_context(nc.sbuf_tensor(("L%d_p5_" % l) + name, shape, dt))
                g2rep = sb("g2rep", [128, 3, D], F32)
                gfin = sb("gfin", [128, D], F32)
                y0 = [sb("y0_%d" % i, [128, D], F32) for i in range(3)]
                y1 = [sb("y1_%d" % i, [128, D], F32) for i in range(3)]
                h5 = [sb("h5_%d" % i, [128, D], F32) for i in range(3)]
                junk = sb("junk", [128, D], BF16)
                st = [sb("st%d" % i, [128, 4], F32) for i in range(3)]
                for ms in range(3):
                    op('sp', lambda e, ms=ms: e.dma_start(out=g2rep[:, ms, :], in_=MS[l, ms:ms + 1, 5 * D:6 * D].to_broadcast([128, D])), writes=['g2rep'], dma=True)
                op('sp', lambda e: e.dma_start(out=gfin[:], in_=g_final[0:1, :].to_broadcast([128, D])), writes=['gfin'], dma=True)
                for tt in range(NT):
                    s_, j = divmod(tt, TPS)
                    if last and j < 2:
                        continue
                    b = tt % 3
                    r0 = tt * 128
                    ms = mset(tt)
                    op('pool', lambda e, b=b, tt=tt: e.indirect_dma_start(out=y0[b][:, :], out_offset=None, in_=YB[:, :],
                                                                          in_offset=bass.IndirectOffsetOnAxis(ap=dest_i[:, tt * 2:tt * 2 + 1], axis=0)),
                       writes=['y0_%d' % b], dma=True)
                    op('pool', lambda e, b=b, tt=tt: e.indirect_dma_start(out=y1[b][:, :], out_offset=None, in_=YB[:, :],
                                                                          in_offset=bass.IndirectOffsetOnAxis(ap=dest_i[:, tt * 2 + 1:tt * 2 + 2], axis=0)),
                       writes=['y1_%d' % b], dma=True)
                    op('sp', lambda e, b=b, r0=r0: e.dma_start(out=h5[b][:], in_=H[r0:r0 + 128, :]), writes=['h5_%d' % b], dma=True)
                    op('dve', lambda e, b=b, tt=tt: e.tensor_scalar(out=y0[b][:], in0=y0[b][:], scalar1=gate_f[:, tt * 2:tt * 2 + 1], scalar2=None, op0=ALU.mult),
                       reads=['y0_%d' % b], writes=['y0_%d' % b])
                    op('dve', lambda e, b=b, tt=tt: e.scalar_tensor_tensor(out=y0[b][:], in0=y1[b][:], scalar=gate_f[:, tt * 2 + 1:tt * 2 + 2], in1=y0[b][:], op0=ALU.mult, op1=ALU.add),
                       reads=['y0_%d' % b, 'y1_%d' % b], writes=['y0_%d' % b])
                    op('dve', lambda e, b=b, ms=ms: e.tensor_tensor(out=y0[b][:], in0=y0[b][:], in1=g2rep[:, ms, :], op=ALU.mult), reads=['y0_%d' % b, 'g2rep'], writes=['y0_%d' % b])
                    op('dve', lambda e, b=b: e.tensor_tensor(out=h5[b][:], in0=h5[b][:], in1=y0[b][:], op=ALU.add), reads=['y0_%d' % b, 'h5_%d' % b], writes=['h5_%d' % b])
                    if not last:
                        op('sp', lambda e, b=b, r0=r0: e.dma_start(out=H[r0:r0 + 128, :], in_=h5[b][:]), reads=['h5_%d' % b], writes=['Hrow%d' % tt], dma=True)
                    else:
                        op('dve', lambda e, b=b: e.memset(st[b][:, 0:1], 0.0), writes=['ss%d' % b])
                        op('act', lambda e, b=b: e.activation(out=junk[:], in_=h5[b][:], func=AF.Square, accum_out=st[b][:, 0:1]),
                           reads=['h5_%d' % b, 'ss%d' % b], writes=['junk', 'ss%d' % b])
                        rstd_from_ss(st[b][:, 0:1], st[b][:, 1:2], 'ss%d' % b, 'rs%d' % b, D)
                        op('dve', lambda e, b=b: e.tensor_scalar(out=h5[b][:], in0=h5[b][:], scalar1=st[b][:, 1:2], scalar2=None, op0=ALU.mult),
                           reads=['h5_%d' % b, 'rs%d' % b], writes=['h5_%d' % b])
                        op('pool', lambda e, b=b: e.tensor_tensor(out=h5[b][:], in0=h5[b][:], in1=gfin[:], op=ALU.mult), reads=['h5_%d' % b, 'gfin'], writes=['h5_%d' % b])
                        orow = s_ * SEQ + (j - 2) * 128
                        op('sp', lambda e, b=b, orow=orow: e.dma_start(out=out[orow:orow + 128, :], in_=h5[b][:]), reads=['h5_%d' % b], writes=['orow%d' % tt], dma=True)
                S.flush()
            if stop_after == 'p5':
                break
    return nc


def _prep_inputs(inputs):
    f32 = np.float32
    x, c, ctx, c_ctx = inputs['x'], inputs['c'], inputs['ctx'], inputs['c_ctx']
    j = np.arange(128)
    ident = np.eye(128, dtype=f32)
    tri = (j[:, None] <= j[None, :]).astype(f32)
    trit = (j[:, None] >= j[None, :]).astype(f32)
    su = (j[:, None] > j[None, :]).astype(f32)
    slw = (j[:, None] < j[None, :]).astype(f32)
    ones = np.ones((128, 128), f32)
    iota = np.broadcast_to(np.arange(64, dtype=f32)[None, :], (128, 64))
    blks = np.broadcast_to((np.arange(128, dtype=f32) * MB)[None, :], (128, 128))
    pidx = np.arange(128, dtype=f32)[:, None]
    pad = np.zeros((128, 1024 - 961), f32)
    cst = np.ascontiguousarray(np.concatenate([ident, tri, trit, su, slw, ones, iota, blks, pidx, pad], axis=1))
    shared = {
        'cst': cst,
        'w_mod': inputs['w_mod'],
        'b_mod3': np.ascontiguousarray(np.broadcast_to(inputs['b_mod'][:, None, :], (L, 3, 6 * D))),
        'gn1T': np.ascontiguousarray(inputs['g_norm1'].reshape(L, 8, 128).transpose(0, 2, 1)),
        'w_in': inputs['w_in'],
        'ln_v_g': inputs['ln_v_g'], 'ln_v_b': inputs['ln_v_b'],
        'w_spT': np.ascontiguousarray(inputs['w_sp'].transpose(0, 1, 3, 2)),
        'b_sp': np.ascontiguousarray(inputs['b_sp'].reshape(L, 512)),
        'w_gate_up': inputs['w_gate_up'], 'b_gate': inputs['b_gate'],
        'g_glaT': np.ascontiguousarray(inputs['g_gla'].reshape(L, 4, 128).transpose(0, 2, 1)),
        'w_out': inputs['w_out'], 'g_norm2': inputs['g_norm2'],
        'w_r': np.ascontiguousarray(np.concatenate([inputs['w_router_g'], inputs['w_router_e']], axis=2)),
        'b_r': np.ascontiguousarray(np.concatenate([inputs['b_router_g'], inputs['b_router_e']], axis=1)),
        'w1': inputs['w1'], 'w3': inputs['w3'], 'w2': inputs['w2'],
        'g_final': np.ascontiguousarray(inputs['g_final'].reshape(1, D)),
    }
    in_maps = []
    for core in range(NCORES):
        rows, cm = [], []
        for s in range(NS):
            b = core * NS + s
            rows.append(ctx[b])
            rows.append(x[b])
            cm.append(c[b])
        cm.append(c_ctx)
        cmod = np.ascontiguousarray(np.stack(cm, axis=0).reshape(3, 8, 128).transpose(2, 1, 0)).astype(f32)
        m = dict(shared)
        m['hin'] = np.ascontiguousarray(np.concatenate(rows, axis=0)).astype(f32)
        m['cmod'] = cmod
        in_maps.append(m)
    return in_maps


def kernel(**inputs):
    inputs = {k: np.asarray(v) for k, v in inputs.items()}
    in_maps = _prep_inputs(inputs)
    nc = build_program()
    res = run_bass_kernel_spmd(nc, in_maps, core_ids=list(range(NCORES)))
    outs = [r["out"].reshape(NS, SEQ, D) for r in res.results]
    return np.concatenate(outs, axis=0).astype(np.float32)
```

```python
import types
import numpy as np
from contextlib import ExitStack
import concourse.bass as bass
import concourse.mybir as mybir
from concourse.bass_utils import run_bass_kernel_spmd

F32 = mybir.dt.float32
BF16 = mybir.dt.bfloat16
I32 = mybir.dt.int32
AF = mybir.ActivationFunctionType
ALU = mybir.AluOpType
AX = mybir.AxisListType

NCORES = 8
L = 2
D = 1024
DIN = 2592
NS = 2
LC = 256
SEQ = 2048
TPS = 18
NT = NS * TPS
T = NT * 128
MB = 256
NBLK = (2 * T) // MB + 64
NSLOT = NBLK * MB
EPS = 1e-6
OFF_AU, OFF_AV, OFF_BQ, OFF_BR, OFF_BK, OFF_BV, OFF_BG = 0, 512, 1024, 1280, 1792, 2048, 2560

SAME_ENGINE_SYNC = True
_DBG = {}


def _freeze(fn):
    if fn.__closure__ is None:
        return fn
    cells = []
    for c in fn.__closure__:
        try:
            cells.append(types.CellType(c.cell_contents))
        except ValueError:
            cells.append(c)
    return types.FunctionType(fn.__code__, fn.__globals__, fn.__name__, fn.__defaults__, tuple(cells))


class Sched:
    ENGS = ['pe', 'act', 'dve', 'pool', 'sp']

    def __init__(self, nc, es, n_dma_sems=(('sp', 16), ('act', 4), ('pool', 16))):
        self.nc = nc
        self.prog = {e: [] for e in self.ENGS}
        self.esem = {e: es.enter_context(nc.semaphore('sem_' + e)) for e in self.ENGS}
        self.ecount = {e: 0 for e in self.ENGS}
        self.seen = {e: {} for e in self.ENGS}
        self.res = {}
        self.dpool, self.dnext, self.dcount, self.semobj = {}, {}, {}, {}
        for e in self.ENGS:
            self.semobj['E' + e] = self.esem[e]
        for e, n in n_dma_sems:
            self.dpool[e] = []
            for i in range(n):
                key = 'D%s%d' % (e, i)
                self.semobj[key] = es.enter_context(nc.semaphore('dsem_%s%d' % (e, i)))
                self.dpool[e].append(key)
                self.dcount[key] = 0
            self.dnext[e] = 0
        self.nops = 0
        self.nwaits = 0

    def _wait(self, eng, tok):
        s, v = tok
        if self.seen[eng].get(s, 0) >= v:
            return
        self.seen[eng][s] = v
        self.prog[eng].append(('wait', s, v))
        self.nwaits += 1

    def op(self, eng, fn, reads=(), writes=(), dma=False, sig=True):
        if _DBG.get('maxops') and self.nops >= _DBG['maxops']:
            return None
        fn = _freeze(fn)
        writes = list(writes) + [r for r in reads if r.startswith('ps') and r not in writes]
        deps = []
        for r in reads:
            st = self.res.get(r)
            if st is not None and st['w'] is not None:
                deps.append(st['w'])
        for w in writes:
            st = self.res.get(w)
            if st is not None:
                if st['w'] is not None:
                    deps.append(st['w'])
                deps.extend(st['r'])
        if dma:
            pool = self.dpool[eng]
            key = pool[self.dnext[eng] % len(pool)]
            self.dnext[eng] += 1
            cnt = self.dcount[key]
            if cnt > 0:
                deps.append((key, cnt))
            self.dcount[key] = cnt + 16
            tok = (key, cnt + 16)
            inc = 16
        elif not sig:
            key = 'E' + eng
            tok = (key, self.ecount[eng] + 1)
            inc = 0
        else:
            key = 'E' + eng
            self.ecount[eng] += 1
            tok = (key, self.ecount[eng])
            inc = 1
        own = 'E' + eng
        for d in deps:
            if d[0] == own and (eng == 'pe' or not SAME_ENGINE_SYNC):
                continue
            self._wait(eng, d)
        self.prog[eng].append(('op', fn, key, inc))
        self.nops += 1
        for r in reads:
            st = self.res.setdefault(r, {'w': None, 'r': []})
            st['r'].append(tok)
        for w in writes:
            self.res[w] = {'w': tok, 'r': []}
        return tok

    def barrier(self):
        for e in self.ENGS:
            for key, cnt in self.dcount.items():
                if cnt > 0:
                    self._wait(e, (key, cnt))
            for e2 in self.ENGS:
                if e2 != e and self.ecount[e2] > 0:
                    self._wait(e, ('E' + e2, self.ecount[e2]))
        self.res = {}

    def flush(self):
        if _DBG.get('verbose'):
            print('flush: nops', self.nops, 'nwaits', self.nwaits, flush=True)
        self.barrier()
        nc = self.nc
        with nc.Block() as block:
            def run(e):
                def f(eng):
                    for it in self.prog[e]:
                        if it[0] == 'wait':
                            eng.wait_ge(self.semobj[it[1]], it[2])
                        else:
                            ins = it[1](eng)
                            if it[3]:
                                ins.then_inc(self.semobj[it[2]], it[3])
                return f
            block.tensor(run('pe'))
            block.scalar(run('act'))
            block.vector(run('dve'))
            block.gpsimd(run('pool'))
            block.sync(run('sp'))
        self.prog = {e: [] for e in self.ENGS}


def build_program(dbg=False, stop_after=None):
    nc = bass.Bass("TRN2", target_bir_lowering=False)

    def din(name, shape, dt=F32):
        return nc.dram_tensor(name, list(shape), dt, kind="ExternalInput").ap()

    def dscr(name, shape, dt):
        kind = "ExternalOutput" if dbg else "Internal"
        return nc.dram_tensor(name, list(shape), dt, kind=kind).ap()

    hin = din("hin", [T, D])
    cmod = din("cmod", [128, 8, 3])
    cst = din("cst", [128, 1024])
    w_mod = din("w_mod", [L, D, 6 * D])
    b_mod3 = din("b_mod3", [L, 3, 6 * D])
    gn1T = din("gn1T", [L, 128, 8])
    w_in = din("w_in", [L, D, DIN])
    ln_g = din("ln_v_g", [L, 512])
    ln_b = din("ln_v_b", [L, 512])
    w_spT = din("w_spT", [L, 4, 128, 128])
    b_sp = din("b_sp", [L, 512])
    w_gu = din("w_gate_up", [L, 2, 16, 256])
    b_gate = din("b_gate", [L, 2, 256])
    g_glaT = din("g_glaT", [L, 128, 4])
    w_out = din("w_out", [L, D, D])
    gn2 = din("g_norm2", [L, D])
    w_r = din("w_r", [L, D, 72])
    b_r = din("b_r", [L, 72])
    EW = 1 if stop_after in ("p0", "p1", "p2", "p3") else 64
    w1 = din("w1", [L, EW, D, 512])
    w3 = din("w3", [L, EW, D, 512])
    w2 = din("w2", [L, EW, 512, D])
    g_final = din("g_final", [1, D])
    out = nc.dram_tensor("out", [NS * SEQ, D], F32, kind="ExternalOutput").ap()

    H = dscr("H", [T, D], F32)
    MS = dscr("MS", [L, 3, 6 * D], F32)
    UT = dscr("UT", [4, 128, T], BF16)
    QT = dscr("QT", [2, 128, T], BF16)
    KT = dscr("KT", [2, 128, T], BF16)
    RT = dscr("RT", [4, 128, T], BF16)
    LA = dscr("LA", [T, 512], F32)
    Vd = dscr("Vd", [T, 512], BF16)
    Kd = dscr("Kd", [T, 256], BF16)
    VA = dscr("VA", [T, 512], BF16)
    XB = dscr("XB", [NSLOT, D], BF16)
    YB = dscr("YB", [NSLOT, D], F32)

    top = ExitStack()
    with top:
        S = Sched(nc, top)
        op = S.op

        def mset(tile_idx):
            s, j = divmod(tile_idx, TPS)
            return 2 if j < 2 else s

        def psb(name, shape, dt):
            return top.enter_context(nc.sbuf_tensor(name, shape, dt))
        cst32 = psb("cst32", [128, 1024], F32)
        cstb = psb("cstb", [128, 768], BF16)
        widx_i = psb("widx_i", [128, NBLK], I32)
        zt = psb("zt", [128, 4, D], BF16)
        dest_i = psb("dest_i", [128, NT * 2], I32)
        gate_f = psb("gate_f", [128, NT * 2], F32)
        IDENT, TRI, TRIT, SU, SLW, ONES = [slice(i * 128, (i + 1) * 128) for i in range(6)]
        op('sp', lambda e: e.dma_start(out=cst32[:], in_=cst[:, :]), writes=['cst32'], dma=True)
        op('dve', lambda e: e.tensor_copy(out=cstb[:], in_=cst32[:, 0:768]), reads=['cst32'], writes=['cstb'])
        op('dve', lambda e: e.memset(zt[:], 0.0), writes=['zt'])
        for zi in range(NSLOT // 512):
            op('sp', lambda e, zi=zi: e.dma_start(out=XB[zi * 512:(zi + 1) * 512, :].rearrange("(s p) d -> p s d", p=128), in_=zt[:]), reads=['zt'], writes=['XBz'], dma=True)
        S.flush()

        def rstd_from_ss(ss, rs, key_ss, key_rs, n):
            op('dve', lambda e: e.tensor_scalar(out=rs, in0=ss, scalar1=1.0 / n, scalar2=EPS, op0=ALU.mult, op1=ALU.add),
               reads=[key_ss], writes=[key_rs])
            op('act', lambda e: e.activation(out=rs, in_=rs, func=AF.Sqrt), reads=[key_rs], writes=[key_rs])
            op('dve', lambda e: e.reciprocal(out=rs, in_=rs), reads=[key_rs], writes=[key_rs])

        for l in range(L):
            Hsrc = hin if l == 0 else H
            with ExitStack() as es:
                def sb(name, shape, dt):
                    return es.enter_context(nc.sbuf_tensor(("L%d_p0_" % l) + name, shape, dt))
                scT = sb("scT", [128, 8, 3], F32)
                wm = [sb("wm%d" % i, [128, 8, 512], F32) for i in range(2)]
                bm = [sb("bm%d" % i, [3, 512], F32) for i in range(2)]
                mo = [sb("mo%d" % i, [3, 512], F32) for i in range(2)]
                psM = [es.enter_context(nc.psum_tensor("L%d_p0_psM%d" % (l, i), [128, 512], F32)) for i in range(2)]
                op('sp', lambda e: e.dma_start(out=scT[:], in_=cmod[:, :, :]), writes=['scT'], dma=True)
                op('act', lambda e: e.activation(out=scT[:], in_=scT[:], func=AF.Silu), reads=['scT'], writes=['scT'])
                for cg in range(12 if not _DBG.get('nop0') else 0):
                    i = cg % 2
                    cs = slice(cg * 512, (cg + 1) * 512)
                    op('sp', lambda e, i=i, cs=cs: e.dma_start(out=wm[i][:], in_=w_mod[l, :, cs].rearrange("(kc p) c -> p kc c", p=128)),
                       writes=['wm%d' % i], dma=True)
                    op('sp', lambda e, i=i, cs=cs: e.dma_start(out=bm[i][:], in_=b_mod3[l, :, cs]), writes=['bm%d' % i], dma=True)
                    for kc in range(8):
                        op('pe', lambda e, i=i, kc=kc: e.matmul(psM[i][0:3, :], lhsT=scT[:, kc, :], rhs=wm[i][:, kc, :],
                                                               start=(kc == 0), stop=(kc == 7)),
                           sig=(kc == 7), reads=['scT', 'wm%d' % i], writes=['psM%d' % i])
                    op('dve', lambda e, i=i: e.tensor_tensor(out=mo[i][:], in0=psM[i][0:3, :], in1=bm[i][:], op=ALU.add),
                       reads=['psM%d' % i, 'bm%d' % i], writes=['mo%d' % i])
                    op('sp', lambda e, i=i, cs=cs: e.dma_start(out=MS[l, :, cs], in_=mo[i][:]), reads=['mo%d' % i], writes=['MS'], dma=True)
                S.flush()

            if stop_after == 'p0':
                break
            with ExitStack() as es:
                def sb(name, shape, dt):
                    return es.enter_context(nc.sbuf_tensor(("L%d_p1_" % l) + name, shape, dt))

                def pst(name, shape, dt):
                    return es.enter_context(nc.psum_tensor(("L%d_p1_" % l) + name, shape, dt))
                winb = sb("winb", [128, 8, DIN], BF16)
                wup = sb("wup", [64, 512], F32)
                A1T = sb("A1T", [128, 3, 8], F32)
                sh1T = sb("sh1T", [128, 3, 8], F32)
                g1t = sb("g1t", [128, 8], F32)
                lng = sb("lng", [128, 512], F32)
                lnb = sb("lnb", [128, 512], F32)
                zgT = sb("zgT", [64, 256], F32)
                ht = [sb("ht%d" % i, [128, D], F32) for i in range(2)]
                junk = sb("junk", [128, D], BF16)
                hs = [sb("hs%d" % i, [128, D], BF16) for i in range(2)]
                hnT = [sb("hnT%d" % i, [128, 8, 256], BF16) for i in range(2)]
                st = [sb("st%d" % i, [128, 24], F32) for i in range(2)]
                fo = [sb("fo%d" % i, [128, 256], BF16) for i in range(4)]
                g32 = [sb("g32_%d" % i, [128, 512], F32) for i in range(2)]
                sq32 = [sb("sq32_%d" % i, [128, 512], F32) for i in range(2)]
                vab = [sb("vab%d" % i, [128, 512], BF16) for i in range(2)]
                vtb = [sb("vtb%d" % i, [128, 512], BF16) for i in range(2)]
                ktb = [sb("ktb%d" % i, [128, 256], BF16) for i in range(2)]
                la32 = [sb("la32_%d" % i, [128, 512], F32) for i in range(2)]
                psT = [pst("psT%d" % i, [128, 1024], BF16) for i in range(2)]
                psF = [pst("psF%d" % i, [128, 512], F32) for i in range(3)]
                psK = [pst("psK%d" % i, [128, 512], F32) for i in range(3)]

                for kc in range(8):
                    op('pool', lambda e, kc=kc: e.dma_start(out=winb[:, kc, :], in_=w_in[l, kc * 128:(kc + 1) * 128, :]),
                       writes=['winb'], dma=True)
                op('dve', lambda e: e.memset(wup[:], 0.0), writes=['wup'])
                op('sp', lambda e: e.dma_start(out=wup[0:16, 0:256], in_=w_gu[l, 0, :, :]), reads=['wup'], writes=['wup0'], dma=True)
                op('sp', lambda e: e.dma_start(out=wup[16:32, 256:512], in_=w_gu[l, 1, :, :]), reads=['wup'], writes=['wup1'], dma=True)
                op('sp', lambda e: e.dma_start(out=wup[32:33, 0:256], in_=b_gate[l, 0:1, :]), reads=['wup'], writes=['wup2'], dma=True)
                op('sp', lambda e: e.dma_start(out=wup[32:33, 256:512], in_=b_gate[l, 1:2, :]), reads=['wup'], writes=['wup3'], dma=True)
                WUPK = ['wup', 'wup0', 'wup1', 'wup2', 'wup3']
                op('dve', lambda e: e.memset(zgT[:], 1.0), writes=['zgT'])
                op('sp', lambda e: e.dma_start(out=g1t[:], in_=gn1T[l, :, :]), writes=['g1t'], dma=True)
                op('sp', lambda e: e.dma_start(out=lng[:], in_=ln_g[l:l + 1, :].to_broadcast([128, 512])), writes=['lng'], dma=True)
                op('sp', lambda e: e.dma_start(out=lnb[:], in_=ln_b[l:l + 1, :].to_broadcast([128, 512])), writes=['lnb'], dma=True)
                for ms in range(3):
                    op('sp', lambda e, ms=ms: e.dma_start(out=sh1T[:, ms, :], in_=MS[l, ms, 0:D].rearrange("(kc p) -> p kc", p=128),
                                                           allow_slow_non_contiguous=True), reads=['MS'], writes=['sh1T%d' % ms], dma=True)
                    op('sp', lambda e, ms=ms: e.dma_start(out=A1T[:, ms, :], in_=MS[l, ms, D:2 * D].rearrange("(kc p) -> p kc", p=128),
                                                           allow_slow_non_contiguous=True), reads=['MS'], writes=['A1T%d' % ms], dma=True)
                    op('dve', lambda e, ms=ms: e.scalar_tensor_tensor(out=A1T[:, ms, :], in0=A1T[:, ms, :], scalar=1.0, in1=g1t[:],
                                                                      op0=ALU.add, op1=ALU.mult),
                       reads=['A1T%d' % ms, 'g1t'], writes=['A1T%d' % ms])

                NG = NT // 2 if not _DBG.get('ng') else _DBG['ng']
                fcnt = [0]
                def p1_front(gi):
                    gb = gi % 2
                    t0 = gi * 256
                    ms = mset(gi * 2)
                    for ti in range(2):
                        tt = gi * 2 + ti
                        b = tt % 2
                        r0 = tt * 128
                        op('sp', lambda e, b=b, r0=r0: e.dma_start(out=ht[b][:], in_=Hsrc[r0:r0 + 128, :]), reads=['H'], writes=['ht%d' % b], dma=True)
                        op('dve', lambda e, b=b: e.memset(st[b][:, 0:1], 0.0), writes=['ss%d' % b])
                        op('act', lambda e, b=b: e.activation(out=junk[:], in_=ht[b][:], func=AF.Square, accum_out=st[b][:, 0:1]),
                           reads=['ht%d' % b, 'ss%d' % b], writes=['junk', 'ss%d' % b])
                        rstd_from_ss(st[b][:, 0:1], st[b][:, 1:2], 'ss%d' % b, 'rs%d' % b, D)
                        op('dve', lambda e, b=b: e.tensor_scalar(out=hs[b][:], in0=ht[b][:], scalar1=st[b][:, 1:2], scalar2=None, op0=ALU.mult),
                           reads=['ht%d' % b, 'rs%d' % b], writes=['hs%d' % b])
                        for kc in range(8):
                            op('pe', lambda e, b=b, kc=kc: e.transpose(out=psT[b][:, kc * 128:(kc + 1) * 128], in_=hs[b][:, kc * 128:(kc + 1) * 128],
                                                                      identity=cstb[:, IDENT]),
                               reads=['hs%d' % b, 'cstb'], writes=['psT%d' % b], sig=(kc == 7))
                        for kc in range(8):
                            eng = 'act' if ti == 0 else 'dve'
                            if eng == 'act':
                                f = lambda e, b=b, kc=kc, ti=ti: e.activation(out=hnT[gb][:, kc, ti * 128:(ti + 1) * 128], in_=psT[b][:, kc * 128:(kc + 1) * 128],
                                                                               func=AF.Identity, scale=A1T[:, ms, kc:kc + 1], bias=sh1T[:, ms, kc:kc + 1])
                            else:
                                f = lambda e, b=b, kc=kc, ti=ti: e.tensor_scalar(out=hnT[gb][:, kc, ti * 128:(ti + 1) * 128], in0=psT[b][:, kc * 128:(kc + 1) * 128],
                                                                                  scalar1=A1T[:, ms, kc:kc + 1], scalar2=sh1T[:, ms, kc:kc + 1],
                                                                                  op0=ALU.mult, op1=ALU.add)
                            op(eng, f, reads=['psT%d' % b, 'A1T%d' % ms, 'sh1T%d' % ms], writes=['hnT%d_%d_%d' % (gb, ti, kc)])

                def p1_back(gi):
                    gb = gi % 2
                    t0 = gi * 256
                    ms = mset(gi * 2)
                    HK = ['hnT%d_%d_%d' % (gb, ti, kc) for ti in range(2) for kc in range(8)]
                    fm = [(OFF_AU + 128 * i, UT, i, AF.Gelu, 1.0) for i in range(4)]
                    fm += [(OFF_BQ + 128 * i, QT, i, AF.Copy, 0.125) for i in range(2)]
                    fm += [(OFF_BR + 128 * i, RT, i, AF.Silu, 1.0) for i in range(4)]
                    fm += [(OFF_BK + 128 * i, KT, i, AF.Copy, 1.0) for i in range(2)]
                    for (c0, dst, ci, func, scl) in fm:
                        n = fcnt[0]
                        fcnt[0] += 1
                        pb, ph = (n // 2) % 3, n % 2
                        pk = 'psF%d' % pb
                        pv = psF[pb][:, ph * 256:(ph + 1) * 256]
                        fb = n % 4
                        for kc in range(8):
                            op('pe', lambda e, pv=pv, kc=kc, c0=c0: e.matmul(pv, lhsT=winb[:, kc, c0:c0 + 128], rhs=hnT[gb][:, kc, :],
                                                                            start=(kc == 0), stop=(kc == 7)),
                               sig=(kc == 7), reads=['winb'] + HK, writes=[pk])
                        op('act', lambda e, pv=pv, fb=fb, func=func, scl=scl: e.activation(out=fo[fb][:], in_=pv, func=func, scale=scl),
                           reads=[pk], writes=['fo%d' % fb])
                        op('sp', lambda e, fb=fb, dst=dst, ci=ci: e.dma_start(out=dst[ci, :, t0:t0 + 256], in_=fo[fb][:]),
                           reads=['fo%d' % fb], writes=['z'], dma=True)
                    n = fcnt[0]
                    fcnt[0] += 1
                    pb, ph = (n // 2) % 3, n % 2
                    pk = 'psF%d' % pb
                    pvg = psF[pb][0:32, ph * 256:(ph + 1) * 256]
                    for kc in range(8):
                        op('pe', lambda e, pvg=pvg, kc=kc: e.matmul(pvg, lhsT=winb[:, kc, OFF_BG:OFF_BG + 32], rhs=hnT[gb][:, kc, :],
                                                                    start=(kc == 0), stop=(kc == 7)),
                           sig=(kc == 7), reads=['winb'] + HK, writes=[pk])
                    op('dve', lambda e, pvg=pvg: e.tensor_copy(out=zgT[0:32, :], in_=pvg), reads=[pk], writes=['zgT'])
                    for ti in range(2):
                        tt = gi * 2 + ti
                        b = tt % 2
                        r0 = tt * 128
                        HKt = ['hnT%d_%d_%d' % (gb, ti, kc) for kc in range(8)]
                        pz, pkk, pvv = psK[0], psK[1], psK[2]
                        for kc in range(8):
                            op('pe', lambda e, kc=kc, ti=ti: e.matmul(pz[:, :], lhsT=hnT[gb][:, kc, ti * 128:(ti + 1) * 128], rhs=winb[:, kc, OFF_AV:OFF_AV + 512],
                                                                      start=(kc == 0), stop=(kc == 7)), sig=(kc == 7), reads=['winb'] + HKt, writes=['psK0'])
                        for kc in range(8):
                            op('pe', lambda e, kc=kc, ti=ti: e.matmul(pvv[:, :], lhsT=hnT[gb][:, kc, ti * 128:(ti + 1) * 128], rhs=winb[:, kc, OFF_BV:OFF_BV + 512],
                                                                      start=(kc == 0), stop=(kc == 7)), sig=(kc == 7), reads=['winb'] + HKt, writes=['psK2'])
                        op('act', lambda e, b=b: e.activation(out=g32[b][:], in_=pz[:, :], func=AF.Gelu), reads=['psK0'], writes=['g32_%d' % b])
                        op('dve', lambda e, b=b: e.tensor_copy(out=vtb[b][:], in_=pvv[:, :]), reads=['psK2'], writes=['vtb%d' % b])
                        op('sp', lambda e, b=b, r0=r0: e.dma_start(out=Vd[r0:r0 + 128, :], in_=vtb[b][:]), reads=['vtb%d' % b], writes=['z'], dma=True)
                        for kc in range(8):
                            op('pe', lambda e, kc=kc, ti=ti: e.matmul(pkk[:, 0:256], lhsT=hnT[gb][:, kc, ti * 128:(ti + 1) * 128], rhs=winb[:, kc, OFF_BK:OFF_BK + 256],
                                                                      start=(kc == 0), stop=(kc == 7)), sig=(kc == 7), reads=['winb'] + HKt, writes=['psK1'])
                        op('act', lambda e, b=b: e.activation(out=ktb[b][:], in_=pkk[:, 0:256], func=AF.Copy), reads=['psK1'], writes=['ktb%d' % b])
                        op('sp', lambda e, b=b, r0=r0: e.dma_start(out=Kd[r0:r0 + 128, :], in_=ktb[b][:]), reads=['ktb%d' % b], writes=['z'], dma=True)
                        g3 = g32[b][:, :].rearrange("p (g c) -> p g c", g=4)
                        s3 = sq32[b][:, :].rearrange("p (g c) -> p g c", g=4)
                        stb = st[b]
                        op('pool', lambda e, b=b: e.tensor_tensor(out=sq32[b][:], in0=g32[b][:], in1=g32[b][:], op=ALU.mult),
                           reads=['g32_%d' % b], writes=['sq32_%d' % b])
                        op('dve', lambda e, g3=g3, stb=stb: e.tensor_reduce(out=stb[:, 4:8], in_=g3, axis=AX.X, op=ALU.add), reads=['g32_%d' % b], writes=['ln1_%d' % b])
                        op('dve', lambda e, s3=s3, stb=stb: e.tensor_reduce(out=stb[:, 8:12], in_=s3, axis=AX.X, op=ALU.add), reads=['sq32_%d' % b], writes=['ln2_%d' % b])
                        op('dve', lambda e, stb=stb: e.tensor_scalar(out=stb[:, 4:8], in0=stb[:, 4:8], scalar1=1.0 / 128, scalar2=None, op0=ALU.mult),
                           reads=['ln1_%d' % b], writes=['ln1_%d' % b])
                        op('dve', lambda e, stb=stb: e.tensor_tensor(out=stb[:, 12:16], in0=stb[:, 4:8], in1=stb[:, 4:8], op=ALU.mult),
                           reads=['ln1_%d' % b], writes=['ln3_%d' % b])
                        op('dve', lambda e, stb=stb: e.scalar_tensor_tensor(out=stb[:, 8:12], in0=stb[:, 8:12], scalar=1.0 / 128, in1=stb[:, 12:16],
                                                                            op0=ALU.mult, op1=ALU.subtract),
                           reads=['ln2_%d' % b, 'ln3_%d' % b], writes=['ln2_%d' % b])
                        op('dve', lambda e, stb=stb: e.tensor_scalar(out=stb[:, 8:12], in0=stb[:, 8:12], scalar1=EPS, scalar2=None, op0=ALU.add),
                           reads=['ln2_%d' % b], writes=['ln2_%d' % b])
                        op('act', lambda e, stb=stb: e.activation(out=stb[:, 8:12], in_=stb[:, 8:12], func=AF.Sqrt), reads=['ln2_%d' % b], writes=['ln2_%d' % b])
                        op('dve', lambda e, stb=stb: e.reciprocal(out=stb[:, 8:12], in_=stb[:, 8:12]), reads=['ln2_%d' % b], writes=['ln2_%d' % b])
                        op('dve', lambda e, g3=g3, stb=stb: e.tensor_tensor(out=g3, in0=g3, in1=stb[:, 4:8].unsqueeze(2).to_broadcast([128, 4, 128]), op=ALU.subtract),
                           reads=['g32_%d' % b, 'ln1_%d' % b, 'sq32_%d' % b], writes=['g32_%d' % b])
                        op('dve', lambda e, g3=g3, stb=stb: e.tensor_tensor(out=g3, in0=g3, in1=stb[:, 8:12].unsqueeze(2).to_broadcast([128, 4, 128]), op=ALU.mult),
                           reads=['g32_%d' % b, 'ln2_%d' % b], writes=['g32_%d' % b])
                        op('pool', lambda e, b=b: e.tensor_tensor(out=g32[b][:], in0=g32[b][:], in1=lng[:], op=ALU.mult),
                           reads=['g32_%d' % b, 'lng'], writes=['g32_%d' % b])
                        op('pool', lambda e, b=b: e.tensor_tensor(out=vab[b][:], in0=g32[b][:], in1=lnb[:], op=ALU.add),
                           reads=['g32_%d' % b, 'lnb'], writes=['vab%d' % b])
                        op('sp', lambda e, b=b, r0=r0: e.dma_start(out=VA[r0:r0 + 128, :], in_=vab[b][:]), reads=['vab%d' % b], writes=['z'], dma=True)
                        op('pe', lambda e, ti=ti: e.matmul(pkk[:, :], lhsT=zgT[0:64, ti * 128:(ti + 1) * 128], rhs=wup[0:64, :], start=True, stop=True),
                           reads=['zgT'] + WUPK + ['ktb%d' % b], writes=['psK1'])
                        op('act', lambda e, b=b: e.activation(out=la32[b][:], in_=pkk[:, :], func=AF.Exp, scale=-1.0), reads=['psK1'], writes=['la32_%d' % b])
                        op('act', lambda e, b=b: e.activation(out=la32[b][:], in_=la32[b][:], func=AF.Ln, bias=1.0), reads=['la32_%d' % b], writes=['la32_%d' % b])
                        op('pool', lambda e, b=b: e.tensor_scalar(out=la32[b][:], in0=la32[b][:], scalar1=-1.0 / 16, scalar2=None, op0=ALU.mult),
                           reads=['la32_%d' % b], writes=['la32_%d' % b])
                        op('sp', lambda e, b=b, r0=r0: e.dma_start(out=LA[r0:r0 + 128, :], in_=la32[b][:]), reads=['la32_%d' % b], writes=['z'], dma=True)

                p1_front(0)
                for gi in range(NG):
                    if gi + 1 < NG:
                        p1_front(gi + 1)
                    p1_back(gi)
                S.flush()
            if stop_after == 'p1':
                break
            with ExitStack() as es:
                def sb(name, shape, dt):
                    return es.enter_context(nc.sbuf_tensor(("L%d_p2_" % l) + name, shape, dt))

                def pst(name, shape, dt):
                    return es.enter_context(nc.psum_tensor(("L%d_p2_" % l) + name, shape, dt))
                woutb = sb("woutb", [128, 8, D], BF16)
                wspb = sb("wspb", [128, 4, 128], BF16)
                bsp = sb("bsp", [128, 512], F32)
                ggla = sb("ggla", [128, 4], F32)
                g1rep = sb("g1rep", [128, 3, D], F32)
                KVs = sb("KVs", [128, TPS * 4, 128], F32)
                dec = sb("dec", [128, TPS * 4], F32)
                Sst = sb("Sst", [128, 4, 128], F32)
                Sprev = sb("Sprev", [128, TPS * 4, 128], BF16)
                la = [sb("la%d" % i, [128, 512], F32) for i in range(2)]
                ktm = [sb("ktm%d" % i, [128, 256], BF16) for i in range(2)]
                vtm = [sb("vtm%d" % i, [128, 512], BF16) for i in range(2)]
                eB = [sb("eB%d" % i, [128, 512], F32) for i in range(2)]
                kd = [sb("kd%d" % i, [128, 512], BF16) for i in range(2)]
                qTt = [sb("qT%d" % i, [128, 2, 128], BF16) for i in range(2)]
                kTt = [sb("kT%d" % i, [128, 2, 128], BF16) for i in range(2)]
                rTt = [sb("rT%d" % i, [128, 4, 128], BF16) for i in range(2)]
                uTt = [sb("uT%d" % i, [128, 4, 128], BF16) for i in range(2)]
                vat = [sb("va%d" % i, [128, 512], BF16) for i in range(2)]
                hh = [sb("hh%d" % i, [128, D], F32) for i in range(2)]
                Ep = [sb("Ep%d" % i, [128, 512], F32) for i in range(2)]
                Em = [sb("Em%d" % i, [128, 512], F32) for i in range(2)]
                qeL = [sb("qeL%d" % i, [128, 512], BF16) for i in range(2)]
                qeH = [sb("qeH%d" % i, [128, 512], BF16) for i in range(2)]
                keL = [sb("keL%d" % i, [128, 512], BF16) for i in range(2)]
                keH = [sb("keH%d" % i, [128, 512], BF16) for i in range(2)]
                atf = [sb("atf%d" % i, [128, 512], BF16) for i in range(2)]
                atb = [sb("atb%d" % i, [128, 512], BF16) for i in range(2)]
                sq = [sb("sq%d" % i, [128, 512], BF16) for i in range(2)]
                rs = [sb("rs%d" % i, [128, 512], F32) for i in range(2)]
                tmp = [sb("tmp%d" % i, [128, 512], F32) for i in range(2)]
                tmp2 = [sb("tmp2%d" % i, [128, 512], F32) for i in range(2)]
                bo = [sb("bo%d" % i, [128, 512], BF16) for i in range(2)]
                ao = [sb("ao%d" % i, [128, 512], BF16) for i in range(2)]
                hm = [sb("hm%d" % i, [128, D], F32) for i in range(2)]
                psB = pst("psB", [128, 512], F32)
                psKV = [pst("psKV%d" % i, [128, 512], F32) for i in range(2)]
                psD = pst("psD", [128, 512], F32)
                psO = pst("psO", [128, 512], F32)
                psP = pst("psP", [128, 512], F32)
                psM = [pst("psM%d" % i, [128, 512], F32) for i in range(2)]

                for i in range(2):
                    for (tl, nm) in ((qeL, 'qeL'), (qeH, 'qeH'), (keL, 'keL'), (keH, 'keH')):
                        op('dve', lambda e, tl=tl, i=i: e.memset(tl[i][:], 0.0), writes=['%s%d' % (nm, i)])
                for kc in range(8):
                    op('pool', lambda e, kc=kc: e.dma_start(out=woutb[:, kc, :], in_=w_out[l, kc * 128:(kc + 1) * 128, :]), writes=['woutb%d' % kc], dma=True)
                op('sp', lambda e: e.dma_start(out=ggla[:], in_=g_glaT[l, :, :]), writes=['ggla'], dma=True)
                for h4 in range(4):
                    op('dve', lambda e, h4=h4: e.tensor_scalar(out=woutb[:, 4 + h4, :], in0=woutb[:, 4 + h4, :], scalar1=ggla[:, h4:h4 + 1], scalar2=None, op0=ALU.mult),
                       reads=['ggla', 'woutb%d' % (4 + h4)], writes=['woutb%d' % (4 + h4)])
                WOK = ['woutb%d' % kc for kc in range(8)]
                op('pool', lambda e: e.dma_start(out=wspb[:], in_=w_spT[l, :, :, :].rearrange("g q p -> q g p")), writes=['wspb'], dma=True)
                op('sp', lambda e: e.dma_start(out=bsp[:], in_=b_sp[l:l + 1, :].to_broadcast([128, 512])), writes=['bsp'], dma=True)
                for ms in range(3):
                    op('sp', lambda e, ms=ms: e.dma_start(out=g1rep[:, ms, :], in_=MS[l, ms:ms + 1, 2 * D:3 * D].to_broadcast([128, D])), writes=['g1rep'], dma=True)

                for s in range(NS):
                    for j in range(TPS):
                        tt = s * TPS + j
                        r0 = tt * 128
                        b = j % 2
                        op('sp', lambda e, b=b, r0=r0: e.dma_start(out=la[b][:], in_=LA[r0:r0 + 128, :]), writes=['la%d' % b], dma=True)
                        op('sp', lambda e, b=b, r0=r0: e.dma_start(out=ktm[b][:], in_=Kd[r0:r0 + 128, :]), writes=['ktm%d' % b], dma=True)
                        op('sp', lambda e, b=b, r0=r0: e.dma_start(out=vtm[b][:], in_=Vd[r0:r0 + 128, :]), writes=['vtm%d' % b], dma=True)
                        op('pe', lambda e, b=b: e.matmul(psB[:, 0:256], lhsT=cst32[:, SU], rhs=la[b][:, 0:256], start=True, stop=True), reads=['la%d' % b, 'cst32'], writes=['psB'])
                        op('pe', lambda e, b=b: e.matmul(psB[:, 256:512], lhsT=cst32[:, SLW], rhs=la[b][:, 256:512], start=True, stop=True), reads=['la%d' % b, 'cst32'], writes=['psB'])
                        op('act', lambda e, b=b: e.activation(out=eB[b][:], in_=psB[:, :], func=AF.Exp), reads=['psB'], writes=['eB%d' % b])
                        op('dve', lambda e, b=b: e.tensor_tensor(out=kd[b][:, :].rearrange("p (a c) -> p a c", a=2), in0=eB[b][:, :].rearrange("p (a c) -> p a c", a=2),
                                                                 in1=ktm[b][:, :].unsqueeze(1).to_broadcast([128, 2, 256]), op=ALU.mult),
                           reads=['eB%d' % b, 'ktm%d' % b], writes=['kd%d' % b])
                        for idx in range(4):
                            dr, hp = divmod(idx, 2)
                            pv = psKV[dr][:, hp * 256:(hp + 1) * 256]
                            op('pe', lambda e, b=b, pv=pv, dr=dr, hp=hp: e.matmul(pv, lhsT=kd[b][:, dr * 256 + hp * 128: dr * 256 + hp * 128 + 128],
                                                                                  rhs=vtm[b][:, hp * 256:(hp + 1) * 256], start=True, stop=True),
                               reads=['kd%d' % b, 'vtm%d' % b], writes=['psKV%d' % dr])
                            if dr == 0:
                                op('act', lambda e, pv=pv, j=j, idx=idx: e.activation(out=KVs[0:64, j * 4 + idx, :], in_=pv[0:64, 0:128], func=AF.Copy),
                                   reads=['psKV%d' % dr], writes=['KVa%d_%d' % (j, idx)])
                                op('act', lambda e, pv=pv, j=j, idx=idx: e.activation(out=KVs[64:128, j * 4 + idx, :], in_=pv[64:128, 128:256], func=AF.Copy),
                                   reads=['psKV%d' % dr], writes=['KVb%d_%d' % (j, idx)])
                            else:
                                op('dve', lambda e, pv=pv, j=j, idx=idx: e.tensor_copy(out=KVs[0:64, j * 4 + idx, :], in_=pv[0:64, 0:128]),
                                   reads=['psKV%d' % dr], writes=['KVa%d_%d' % (j, idx)])
                                op('dve', lambda e, pv=pv, j=j, idx=idx: e.tensor_copy(out=KVs[64:128, j * 4 + idx, :], in_=pv[64:128, 128:256]),
                                   reads=['psKV%d' % dr], writes=['KVb%d_%d' % (j, idx)])
                            op('pe', lambda e, b=b, idx=idx, dr=dr, hp=hp: e.matmul(psD[:, idx:idx + 1], lhsT=la[b][:, dr * 256 + hp * 128: dr * 256 + hp * 128 + 128],
                                                                                    rhs=cst32[:, 5 * 128:5 * 128 + 1], start=True, stop=True),
                               reads=['la%d' % b, 'cst32'], writes=['psD'])
                        op('act', lambda e, j=j: e.activation(out=dec[:, j * 4:(j + 1) * 4], in_=psD[:, 0:4], func=AF.Exp), reads=['psD'], writes=['dec%d' % j])
                    op('dve', lambda e: e.memset(Sst[:], 0.0), writes=['Sst0', 'Sst1', 'Sst2', 'Sst3'])
                    order_f = list(range(TPS))
                    order_b = [1, 0] + list(range(TPS - 1, 1, -1))
                    for dr, order in ((0, order_f), (1, order_b)):
                        for j in order:
                            for hp in range(2):
                                idx = dr * 2 + hp
                                op('pool', lambda e, j=j, idx=idx: e.tensor_copy(out=Sprev[:, j * 4 + idx, :], in_=Sst[:, idx, :]),
                                   reads=['Sst%d' % idx], writes=['Sprev%d_%d' % (j, idx)])
                                op('dve', lambda e, j=j, idx=idx: e.scalar_tensor_tensor(out=Sst[:, idx, :], in0=Sst[:, idx, :], scalar=dec[:, j * 4 + idx: j * 4 + idx + 1],
                                                                                         in1=KVs[:, j * 4 + idx, :], op0=ALU.mult, op1=ALU.add),
                                   reads=['Sst%d' % idx, 'dec%d' % j, 'KVa%d_%d' % (j, idx), 'KVb%d_%d' % (j, idx)], writes=['Sst%d' % idx])
                    def p2_front(j):
                        tt = s * TPS + j
                        r0 = tt * 128
                        b = j % 2
                        ms = mset(tt)
                        op('sp', lambda e, b=b, r0=r0: e.dma_start(out=la[b][:], in_=LA[r0:r0 + 128, :]), writes=['la%d' % b], dma=True)
                        op('sp', lambda e, b=b, r0=r0: e.dma_start(out=vtm[b][:], in_=Vd[r0:r0 + 128, :]), writes=['vtm%d' % b], dma=True)
                        op('sp', lambda e, b=b, r0=r0: e.dma_start(out=qTt[b][:], in_=QT[:, :, r0:r0 + 128].rearrange("h p t -> p h t")), writes=['qT%d' % b], dma=True)
                        op('sp', lambda e, b=b, r0=r0: e.dma_start(out=kTt[b][:], in_=KT[:, :, r0:r0 + 128].rearrange("h p t -> p h t")), writes=['kT%d' % b], dma=True)
                        op('sp', lambda e, b=b, r0=r0: e.dma_start(out=rTt[b][:], in_=RT[:, :, r0:r0 + 128].rearrange("h p t -> p h t")), writes=['rT%d' % b], dma=True)
                        op('sp', lambda e, b=b, r0=r0: e.dma_start(out=uTt[b][:], in_=UT[:, :, r0:r0 + 128].rearrange("h p t -> p h t")), writes=['uT%d' % b], dma=True)
                        op('sp', lambda e, b=b, r0=r0: e.dma_start(out=vat[b][:], in_=VA[r0:r0 + 128, :]), writes=['va%d' % b], dma=True)
                        op('sp', lambda e, b=b, r0=r0: e.dma_start(out=hh[b][:], in_=Hsrc[r0:r0 + 128, :]), writes=['hh%d' % b], dma=True)
                        for idx in range(4):
                            dr, hp = divmod(idx, 2)
                            msk = TRI if dr == 0 else TRIT
                            op('pe', lambda e, b=b, idx=idx, dr=dr, hp=hp, msk=msk: e.matmul(psB[:, idx * 128:(idx + 1) * 128],
                                                                                             lhsT=la[b][:, dr * 256 + hp * 128: dr * 256 + hp * 128 + 128],
                                                                                             rhs=cst32[:, msk], start=True, stop=True),
                               reads=['la%d' % b, 'cst32'], writes=['psB'])
                        op('act', lambda e, b=b: e.activation(out=Ep[b][:], in_=psB[:, :], func=AF.Exp), reads=['psB'], writes=['Ep%d' % b])
                        op('act', lambda e, b=b: e.activation(out=Em[b][:], in_=psB[:, :], func=AF.Exp, scale=-1.0), reads=['psB'], writes=['Em%d' % b])
                        for (rows_, qd, kd_, sfx) in ((slice(0, 64), qeL, keL, 'L'), (slice(64, 128), qeH, keH, 'H')):
                            op('dve', lambda e, b=b, rows_=rows_, qd=qd: e.tensor_tensor(out=qd[b][rows_, :].rearrange("p (a c) -> p a c", a=2),
                                                                                         in0=Ep[b][rows_, :].rearrange("p (a c) -> p a c", a=2),
                                                                                         in1=qTt[b][rows_, :, :].rearrange("p h t -> p (h t)").unsqueeze(1).to_broadcast([64, 2, 256]), op=ALU.mult),
                               reads=['Ep%d' % b, 'qT%d' % b], writes=['qe%s%d' % (sfx, b)])
                            op('pool', lambda e, b=b, rows_=rows_, kd_=kd_: e.tensor_tensor(out=kd_[b][rows_, :].rearrange("p (a c) -> p a c", a=2),
                                                                                            in0=Em[b][rows_, :].rearrange("p (a c) -> p a c", a=2),
                                                                                            in1=kTt[b][rows_, :, :].rearrange("p h t -> p (h t)").unsqueeze(1).to_broadcast([64, 2, 256]), op=ALU.mult),
                               reads=['Em%d' % b, 'kT%d' % b], writes=['ke%s%d' % (sfx, b)])
                        for dr in range(2):
                            for h4 in range(4):
                                hp, par = divmod(h4, 2)
                                rows = slice(par * 64, (par + 1) * 64)
                                cs = slice((dr * 2 + hp) * 128, (dr * 2 + hp + 1) * 128)
                                kx = keL if par == 0 else keH
                                qx = qeL if par == 0 else qeH
                                sfx = 'L' if par == 0 else 'H'
                                op('pe', lambda e, b=b, dr=dr, h4=h4, cs=cs, kx=kx, qx=qx: e.matmul(psKV[dr][:, h4 * 128:(h4 + 1) * 128], lhsT=kx[b][:, cs], rhs=qx[b][:, cs],
                                                                                                    start=True, stop=True),
                                   reads=['ke%s%d' % (sfx, b), 'qe%s%d' % (sfx, b)], writes=['psKV%d' % dr], sig=(h4 == 3))
                        op('dve', lambda e, b=b: e.tensor_tensor(out=atf[b][:, :].rearrange("p (a c) -> p a c", a=4), in0=psKV[0][:, :].rearrange("p (a c) -> p a c", a=4),
                                                                 in1=cst32[:, TRI].unsqueeze(1).to_broadcast([128, 4, 128]), op=ALU.mult),
                           reads=['psKV0', 'cst32'], writes=['atf%d' % b])
                        op('dve', lambda e, b=b: e.tensor_tensor(out=atb[b][:, :].rearrange("p (a c) -> p a c", a=4), in0=psKV[1][:, :].rearrange("p (a c) -> p a c", a=4),
                                                                 in1=cst32[:, TRIT].unsqueeze(1).to_broadcast([128, 4, 128]), op=ALU.mult),
                           reads=['psKV1', 'cst32'], writes=['atb%d' % b])

                    def p2_back(j):
                        tt = s * TPS + j
                        r0 = tt * 128
                        b = j % 2
                        ms = mset(tt)
                        for h4 in range(4):
                            hp, par = divmod(h4, 2)
                            rows = slice(par * 64, (par + 1) * 64)
                            hs_ = slice(h4 * 128, (h4 + 1) * 128)
                            op('pe', lambda e, b=b, hs_=hs_: e.matmul(psO[:, hs_], lhsT=vtm[b][:, hs_], rhs=atf[b][:, hs_], start=True, stop=False),
                               reads=['vtm%d' % b, 'atf%d' % b], writes=['psO'], sig=False)
                            op('pe', lambda e, b=b, hs_=hs_: e.matmul(psO[:, hs_], lhsT=vtm[b][:, hs_], rhs=atb[b][:, hs_], start=False, stop=False),
                               reads=['vtm%d' % b, 'atb%d' % b], writes=['psO'], sig=False)
                            for dr in range(2):
                                cs = slice((dr * 2 + hp) * 128, (dr * 2 + hp + 1) * 128)
                                qx = qeL if par == 0 else qeH
                                sfx = 'L' if par == 0 else 'H'
                                op('pe', lambda e, b=b, hs_=hs_, cs=cs, dr=dr, hp=hp, j=j, qx=qx: e.matmul(psO[:, hs_], lhsT=Sprev[:, j * 4 + dr * 2 + hp, :], rhs=qx[b][:, cs],
                                                                                                           start=False, stop=(dr == 1)),
                                   reads=['Sprev%d_%d' % (j, dr * 2 + hp), 'qe%s%d' % (sfx, b)], writes=['psO'], sig=(h4 == 3 and dr == 1))
                        op('act', lambda e, b=b: e.activation(out=sq[b][:], in_=psO[:, :], func=AF.Square), reads=['psO'], writes=['sq%d' % b])
                        op('pe', lambda e, b=b: e.matmul(psD[:, :], lhsT=cstb[:, ONES], rhs=sq[b][:], start=True, stop=True), reads=['sq%d' % b, 'cstb'], writes=['psD'])
                        op('dve', lambda e, b=b: e.tensor_scalar(out=rs[b][:], in0=psD[:, :], scalar1=1.0 / 128, scalar2=EPS, op0=ALU.mult, op1=ALU.add), reads=['psD'], writes=['rs%d' % b])
                        op('act', lambda e, b=b: e.activation(out=rs[b][:], in_=rs[b][:], func=AF.Sqrt), reads=['rs%d' % b], writes=['rs%d' % b])
                        op('dve', lambda e, b=b: e.reciprocal(out=rs[b][:], in_=rs[b][:]), reads=['rs%d' % b], writes=['rs%d' % b])
                        op('dve', lambda e, b=b: e.tensor_tensor(out=tmp[b][:], in0=psO[:, :], in1=rs[b][:], op=ALU.mult), reads=['psO', 'rs%d' % b], writes=['tmp%d' % b])
                        op('pool', lambda e, b=b: e.tensor_tensor(out=bo[b][:], in0=tmp[b][:], in1=rTt[b][:, :, :].rearrange("p h t -> p (h t)"), op=ALU.mult),
                           reads=['tmp%d' % b, 'rT%d' % b], writes=['bo%d' % b])
                        for g4 in range(4):
                            gs = slice(g4 * 128, (g4 + 1) * 128)
                            op('pe', lambda e, b=b, gs=gs, g4=g4: e.matmul(psP[:, gs], lhsT=vat[b][:, gs], rhs=wspb[:, g4, :], start=True, stop=True),
                               reads=['va%d' % b, 'wspb'], writes=['psP'])
                        op('dve', lambda e, b=b: e.tensor_tensor(out=tmp2[b][:], in0=psP[:, :], in1=bsp[:], op=ALU.add), reads=['psP', 'bsp'], writes=['tmp2%d' % b])
                        op('pool', lambda e, b=b: e.tensor_tensor(out=ao[b][:], in0=tmp2[b][:], in1=uTt[b][:, :, :].rearrange("p h t -> p (h t)"), op=ALU.mult),
                           reads=['tmp2%d' % b, 'uT%d' % b], writes=['ao%d' % b])
                        for half in range(2):
                            for kc in range(8):
                                src = ao[b] if kc < 4 else bo[b]
                                ks = slice((kc % 4) * 128, (kc % 4 + 1) * 128)
                                op('pe', lambda e, half=half, kc=kc, src=src, ks=ks: e.matmul(psM[half][:, :], lhsT=src[:, ks], rhs=woutb[:, kc, half * 512:(half + 1) * 512],
                                                                                              start=(kc == 0), stop=(kc == 7)),
                                   sig=(kc == 7), reads=['ao%d' % b, 'bo%d' % b] + WOK, writes=['psM%d' % half])
                            op('dve', lambda e, b=b, half=half, ms=ms: e.tensor_tensor(out=hm[b][:, half * 512:(half + 1) * 512], in0=psM[half][:, :],
                                                                                       in1=g1rep[:, ms, half * 512:(half + 1) * 512], op=ALU.mult),
                               reads=['psM%d' % half, 'g1rep'], writes=['hm%d_%d' % (b, half)])
                        op('pool', lambda e, b=b: e.tensor_tensor(out=hm[b][:], in0=hm[b][:], in1=hh[b][:], op=ALU.add),
                           reads=['hm%d_0' % b, 'hm%d_1' % b, 'hh%d' % b], writes=['hm%d_0' % b, 'hm%d_1' % b])
                        op('sp', lambda e, b=b, r0=r0: e.dma_start(out=H[r0:r0 + 128, :], in_=hm[b][:]), reads=['hm%d_0' % b, 'hm%d_1' % b], writes=['Hrow%d' % tt], dma=True)

                    js = [j for j in range(TPS) if not (l == L - 1 and j < 2)]
                    p2_front(js[0])
                    for ji, j in enumerate(js):
                        if ji + 1 < len(js):
                            p2_front(js[ji + 1])
                        p2_back(j)
                S.flush()
            if stop_after == 'p2':
                break
            with ExitStack() as es:
                def sb(name, shape, dt):
                    return es.enter_context(nc.sbuf_tensor(("L%d_p3_" % l) + name, shape, dt))

                def pst(name, shape, dt):
                    return es.enter_context(nc.psum_tensor(("L%d_p3_" % l) + name, shape, dt))
                wrb = sb("wrb", [128, 8, 72], BF16)
                brep = sb("brep", [128, 72], F32)
                A2rep = sb("A2rep", [128, 3, D], F32)
                sh2rep = sb("sh2rep", [128, 3, D], F32)
                gn2rep = sb("gn2rep", [128, D], F32)
                hn2all = sb("hn2all", [128, NT, D], BF16)
                ek = sb("ek", [128, NT * 2], F32)
                rank = sb("rank", [128, NT * 2], F32)
                base = sb("base", [128, 64], F32)
                big = sb("big", [128, NBLK * 64], F32)
                h2 = [sb("h2_%d" % i, [128, D], F32) for i in range(2)]
                hs2 = [sb("hs2_%d" % i, [128, D], F32) for i in range(2)]
                junk = sb("junk", [128, D], BF16)
                hT2 = [sb("hT2_%d" % i, [128, D], BF16) for i in range(2)]
                st = [sb("st%d" % i, [128, 4], F32) for i in range(2)]
                Lg = [sb("Lg%d" % i, [128, 72], F32) for i in range(2)]
                R = [sb("R%d" % i, [128, 16], F32) for i in range(2)]
                eg = [sb("eg%d" % i, [128, 8], F32) for i in range(2)]
                ohg = [sb("ohg%d" % i, [128, 8], F32) for i in range(2)]
                sel3 = [sb("sel3_%d" % i, [128, 64], F32) for i in range(2)]
                lsel = [sb("lsel%d" % i, [128, 8], F32) for i in range(2)]
                oh1 = [sb("oh1_%d" % i, [128, 8], F32) for i in range(2)]
                oh2 = [sb("oh2_%d" % i, [128, 8], F32) for i in range(2)]
                l2 = [sb("l2_%d" % i, [128, 8], F32) for i in range(2)]
                oh64 = [[sb("oh64_%d_%d" % (k, i), [128, 64], F32) for i in range(2)] for k in range(2)]
                t64 = [sb("t64_%d" % i, [128, 64], F32) for i in range(2)]
                Abf = [sb("Abf%d" % i, [128, 64], BF16) for i in range(2)]
                pos = [sb("pos%d" % i, [128, 64], F32) for i in range(2)]
                cntT = sb("cntT", [64, 128], F32)
                cmpT = sb("cmpT", [64, 128], F32)
                nblk = sb("nblk", [64, 1], F32)
                padT = sb("padT", [64, 128], F32)
                padse = sb("padse", [128, 128], F32)
                blke_f = sb("blke_f", [128, NBLK], F32)
                dest_f = sb("dest_f", [128, NT * 2], F32)
                psT = [pst("psT%d" % i, [128, D], BF16) for i in range(2)]
                psL = pst("psL", [128, 512], F32)
                psC = pst("psC", [128, 512], F32)
                psCT = pst("psCT", [128, 512], F32)
                psE = pst("psE", [128, 512], F32)
                IOTA = slice(768, 832)
                BLKS = slice(832, 832 + NBLK)
                THR = slice(832, 960)

                op('pool', lambda e: e.dma_start(out=wrb[:], in_=w_r[l, :, :].rearrange("(kc p) c -> p kc c", p=128)), writes=['wrb'], dma=True)
                op('sp', lambda e: e.dma_start(out=brep[:], in_=b_r[l:l + 1, :].to_broadcast([128, 72])), writes=['brep'], dma=True)
                op('sp', lambda e: e.dma_start(out=gn2rep[:], in_=gn2[l:l + 1, :].to_broadcast([128, D])), writes=['gn2rep'], dma=True)
                for ms in range(3):
                    op('sp', lambda e, ms=ms: e.dma_start(out=sh2rep[:, ms, :], in_=MS[l, ms:ms + 1, 3 * D:4 * D].to_broadcast([128, D])), writes=['sh2rep%d' % ms], dma=True)
                    op('sp', lambda e, ms=ms: e.dma_start(out=A2rep[:, ms, :], in_=MS[l, ms:ms + 1, 4 * D:5 * D].to_broadcast([128, D])), writes=['A2rep%d' % ms], dma=True)
                    op('dve', lambda e, ms=ms: e.scalar_tensor_tensor(out=A2rep[:, ms, :], in0=A2rep[:, ms, :], scalar=1.0, in1=gn2rep[:], op0=ALU.add, op1=ALU.mult),
                       reads=['A2rep%d' % ms, 'gn2rep'], writes=['A2rep%d' % ms])
                op('dve', lambda e: e.memset(base[:], 0.0), writes=['base'])
                def p3_front(tt):
                    b = tt % 2
                    r0 = tt * 128
                    ms = mset(tt)
                    Rb, Lgb = R[b], Lg[b]
                    rk = 'R%d' % b
                    op('sp', lambda e, b=b, r0=r0: e.dma_start(out=h2[b][:], in_=H[r0:r0 + 128, :]), writes=['h2_%d' % b], dma=True)
                    op('dve', lambda e, b=b: e.memset(st[b][:, 0:1], 0.0), writes=['ss%d' % b])
                    op('act', lambda e, b=b: e.activation(out=junk[:], in_=h2[b][:], func=AF.Square, accum_out=st[b][:, 0:1]),
                       reads=['h2_%d' % b, 'ss%d' % b], writes=['junk', 'ss%d' % b])
                    rstd_from_ss(st[b][:, 0:1], st[b][:, 1:2], 'ss%d' % b, 'rs%d' % b, D)
                    op('dve', lambda e, b=b: e.tensor_scalar(out=hs2[b][:], in0=h2[b][:], scalar1=st[b][:, 1:2], scalar2=None, op0=ALU.mult),
                       reads=['h2_%d' % b, 'rs%d' % b], writes=['hs2_%d' % b])
                    op('pool', lambda e, b=b, ms=ms: e.tensor_tensor(out=hs2[b][:], in0=hs2[b][:], in1=A2rep[:, ms, :], op=ALU.mult),
                       reads=['hs2_%d' % b, 'A2rep%d' % ms], writes=['hs2_%d' % b])
                    op('pool', lambda e, b=b, ms=ms, tt=tt: e.tensor_tensor(out=hn2all[:, tt, :], in0=hs2[b][:], in1=sh2rep[:, ms, :], op=ALU.add),
                       reads=['hs2_%d' % b, 'sh2rep%d' % ms], writes=['hn2_%d' % tt])
                    for kc in range(8):
                        op('pe', lambda e, b=b, kc=kc, tt=tt: e.transpose(out=psT[b][:, kc * 128:(kc + 1) * 128], in_=hn2all[:, tt, kc * 128:(kc + 1) * 128], identity=cstb[:, IDENT]),
                           reads=['hn2_%d' % tt, 'cstb'], writes=['psT%d' % b], sig=(kc == 7))
                    op('act', lambda e, b=b: e.activation(out=hT2[b][:], in_=psT[b][:, :], func=AF.Copy), reads=['psT%d' % b], writes=['hT2_%d' % b])
                    for kc in range(8):
                        op('pe', lambda e, b=b, kc=kc: e.matmul(psL[:, 0:72], lhsT=hT2[b][:, kc * 128:(kc + 1) * 128], rhs=wrb[:, kc, :], start=(kc == 0), stop=(kc == 7)),
                           sig=(kc == 7), reads=['hT2_%d' % b, 'wrb'], writes=['psL'])
                    op('dve', lambda e, Lgb=Lgb: e.tensor_tensor(out=Lgb[:], in0=psL[:, 0:72], in1=brep[:], op=ALU.add), reads=['psL', 'brep'], writes=['Lg%d' % b])

                def p3_route(tt):
                    b = tt % 2
                    r0 = tt * 128
                    ms = mset(tt)
                    Rb, Lgb = R[b], Lg[b]
                    rk = 'R%d' % b
                    LK = 'Lg%d' % b
                    yield
                    op('dve', lambda e, Rb=Rb, Lgb=Lgb: e.tensor_reduce(out=Rb[:, 0:1], in_=Lgb[:, 0:8], axis=AX.X, op=ALU.max), reads=[LK], writes=[rk])
                    yield
                    op('dve', lambda e, Rb=Rb: e.tensor_scalar(out=Rb[:, 1:2], in0=Rb[:, 0:1], scalar1=-1.0, scalar2=None, op0=ALU.mult), reads=[rk], writes=[rk])
                    yield
                    op('dve', lambda e, Rb=Rb: e.memset(Rb[:, 2:3], 0.0), reads=[rk], writes=[rk])
                    yield
                    op('act', lambda e, Rb=Rb, Lgb=Lgb, b=b: e.activation(out=eg[b][:], in_=Lgb[:, 0:8], func=AF.Exp, bias=Rb[:, 1:2], scale=1.0, accum_out=Rb[:, 2:3]),
                       reads=[LK, rk], writes=[rk, 'eg%d' % b])
                    yield
                    op('dve', lambda e, Rb=Rb: e.reciprocal(out=Rb[:, 3:4], in_=Rb[:, 2:3]), reads=[rk], writes=[rk])
                    yield
                    op('dve', lambda e, Rb=Rb, Lgb=Lgb, b=b: e.tensor_scalar(out=ohg[b][:], in0=Lgb[:, 0:8], scalar1=Rb[:, 0:1], scalar2=None, op0=ALU.is_equal),
                       reads=[LK, rk], writes=['ohg%d' % b])
                    yield
                    op('dve', lambda e, Lgb=Lgb, b=b: e.tensor_tensor(out=sel3[b][:, :].rearrange("p (g x) -> p g x", g=8), in0=Lgb[:, 8:72].rearrange("p (g x) -> p g x", g=8),
                                                                      in1=ohg[b][:, :].unsqueeze(2).to_broadcast([128, 8, 8]), op=ALU.mult),
                       reads=[LK, 'ohg%d' % b], writes=['sel3_%d' % b])
                    yield
                    op('dve', lambda e, b=b: e.tensor_reduce(out=lsel[b][:], in_=sel3[b][:, :].rearrange("p (g x) -> p x g", g=8), axis=AX.X, op=ALU.add),
                       reads=['sel3_%d' % b], writes=['lsel%d' % b])
                    yield
                    op('dve', lambda e, Rb=Rb, b=b: e.tensor_reduce(out=Rb[:, 4:5], in_=lsel[b][:], axis=AX.X, op=ALU.max), reads=['lsel%d' % b, rk], writes=[rk])
                    yield
                    op('dve', lambda e, Rb=Rb, b=b: e.tensor_scalar(out=oh1[b][:], in0=lsel[b][:], scalar1=Rb[:, 4:5], scalar2=None, op0=ALU.is_equal),
                       reads=['lsel%d' % b, rk], writes=['oh1_%d' % b])
                    yield
                    op('dve', lambda e, b=b: e.scalar_tensor_tensor(out=l2[b][:], in0=oh1[b][:], scalar=-1e30, in1=lsel[b][:], op0=ALU.mult, op1=ALU.add),
                       reads=['oh1_%d' % b, 'lsel%d' % b], writes=['l2_%d' % b])
                    yield
                    op('dve', lambda e, Rb=Rb, b=b: e.tensor_reduce(out=Rb[:, 5:6], in_=l2[b][:], axis=AX.X, op=ALU.max), reads=['l2_%d' % b, rk], writes=[rk])
                    yield
                    op('dve', lambda e, Rb=Rb, b=b: e.tensor_scalar(out=oh2[b][:], in0=l2[b][:], scalar1=Rb[:, 5:6], scalar2=None, op0=ALU.is_equal),
                       reads=['l2_%d' % b, rk], writes=['oh2_%d' % b])
                    yield
                    op('dve', lambda e, Rb=Rb: e.tensor_tensor(out=Rb[:, 6:7], in0=Rb[:, 5:6], in1=Rb[:, 4:5], op=ALU.subtract), reads=[rk], writes=[rk])
                    yield
                    op('act', lambda e, Rb=Rb: e.activation(out=Rb[:, 7:8], in_=Rb[:, 6:7], func=AF.Exp), reads=[rk], writes=[rk])
                    yield
                    op('dve', lambda e, Rb=Rb: e.tensor_scalar(out=Rb[:, 7:8], in0=Rb[:, 7:8], scalar1=1.0, scalar2=None, op0=ALU.add), reads=[rk], writes=[rk])
                    yield
                    op('dve', lambda e, Rb=Rb: e.reciprocal(out=Rb[:, 7:8], in_=Rb[:, 7:8]), reads=[rk], writes=[rk])
                    yield
                    op('dve', lambda e, Rb=Rb, tt=tt: e.tensor_tensor(out=gate_f[:, tt * 2:tt * 2 + 1], in0=Rb[:, 7:8], in1=Rb[:, 3:4], op=ALU.mult), reads=[rk], writes=['gate%d' % tt])
                    yield
                    op('dve', lambda e, Rb=Rb, tt=tt: e.tensor_tensor(out=gate_f[:, tt * 2 + 1:tt * 2 + 2], in0=Rb[:, 3:4], in1=gate_f[:, tt * 2:tt * 2 + 1], op=ALU.subtract),
                       reads=[rk, 'gate%d' % tt], writes=['gate%d' % tt])
                    for k, ohk, ohkey in ((0, oh1, 'oh1_%d' % b), (1, oh2, 'oh2_%d' % b)):
                        yield
                        op('dve', lambda e, b=b, k=k, ohk=ohk: e.tensor_tensor(out=oh64[k][b][:, :].rearrange("p (g x) -> p g x", g=8),
                                                                                in0=ohg[b][:, :].unsqueeze(2).to_broadcast([128, 8, 8]),
                                                                                in1=ohk[b][:, :].unsqueeze(1).to_broadcast([128, 8, 8]), op=ALU.mult),
                           reads=['ohg%d' % b, ohkey], writes=['oh64_%d_%d' % (k, b)])
                        yield
                        op('dve', lambda e, b=b, k=k: e.tensor_tensor(out=t64[b][:], in0=oh64[k][b][:], in1=cst32[:, IOTA], op=ALU.mult),
                           reads=['oh64_%d_%d' % (k, b), 'cst32'], writes=['t64_%d' % b])
                        yield
                        op('dve', lambda e, b=b, k=k, tt=tt: e.tensor_reduce(out=ek[:, tt * 2 + k:tt * 2 + k + 1], in_=t64[b][:], axis=AX.X, op=ALU.add),
                           reads=['t64_%d' % b], writes=['ek%d_%d' % (tt, k)])
                    yield
                    op('dve', lambda e, b=b: e.tensor_tensor(out=Abf[b][:], in0=oh64[0][b][:], in1=oh64[1][b][:], op=ALU.add),
                       reads=['oh64_0_%d' % b, 'oh64_1_%d' % b], writes=['Abf%d' % b])

                def p3_tail(tt):
                    b = tt % 2
                    r0 = tt * 128
                    ms = mset(tt)
                    Rb, Lgb = R[b], Lg[b]
                    rk = 'R%d' % b
                    op('pe', lambda e, b=b: e.matmul(psC[:, 0:64], lhsT=cstb[:, SLW], rhs=Abf[b][:], start=True, stop=True), reads=['Abf%d' % b, 'cstb'], writes=['psC'])
                    op('pe', lambda e, b=b: e.matmul(psC[:, 64:128], lhsT=cstb[:, ONES], rhs=Abf[b][:], start=True, stop=True), reads=['Abf%d' % b, 'cstb'], writes=['psC'])
                    op('pe', lambda e, b=b, tt=tt: e.matmul(psCT[0:64, 0:128], lhsT=Abf[b][:], rhs=cstb[:, ONES], start=(tt == 0), stop=(tt == NT - 1)),
                       reads=['Abf%d' % b, 'cstb'], writes=['psCT'])
                    op('dve', lambda e, b=b: e.tensor_tensor(out=pos[b][:], in0=psC[:, 0:64], in1=base[:], op=ALU.add), reads=['psC', 'base'], writes=['pos%d' % b])
                    for k in range(2):
                        op('dve', lambda e, b=b, k=k: e.tensor_tensor(out=t64[b][:], in0=oh64[k][b][:], in1=pos[b][:], op=ALU.mult),
                           reads=['oh64_%d_%d' % (k, b), 'pos%d' % b], writes=['t64_%d' % b])
                        op('dve', lambda e, b=b, k=k, tt=tt: e.tensor_reduce(out=rank[:, tt * 2 + k:tt * 2 + k + 1], in_=t64[b][:], axis=AX.X, op=ALU.add),
                           reads=['t64_%d' % b], writes=['rank%d_%d' % (tt, k)])
                    op('dve', lambda e: e.tensor_tensor(out=base[:], in0=psC[:, 64:128], in1=base[:], op=ALU.add), reads=['psC', 'base'], writes=['base'])

                for tp in range(0, NT, 2):
                    p3_front(tp)
                    p3_front(tp + 1)
                    gens = [p3_route(tp), p3_route(tp + 1)]
                    while gens:
                        for g_ in list(gens):
                            try:
                                next(g_)
                            except StopIteration:
                                gens.remove(g_)
                    p3_tail(tp)
                    p3_tail(tp + 1)
                op('dve', lambda e: e.tensor_copy(out=cntT[:], in_=psCT[0:64, 0:128]), reads=['psCT'], writes=['cntT'])
                op('dve', lambda e: e.tensor_tensor(out=cmpT[:], in0=cntT[:], in1=cst32[0:64, THR], op=ALU.is_gt), reads=['cntT', 'cst32'], writes=['cmpT'])
                op('dve', lambda e: e.tensor_reduce(out=nblk[:], in_=cmpT[:], axis=AX.X, op=ALU.add), reads=['cmpT'], writes=['nblk'])
                op('dve', lambda e: e.tensor_scalar(out=padT[:], in0=cst32[0:64, ONES], scalar1=nblk[:, 0:1], scalar2=float(MB), op0=ALU.mult, op1=ALU.mult),
                   reads=['nblk', 'cst32'], writes=['padT'])
                op('pe', lambda e: e.matmul(psE[:, 0:64], lhsT=padT[:], rhs=cst32[0:64, 4 * 128:4 * 128 + 64], start=True, stop=True), reads=['padT', 'cst32'], writes=['psE'])
                op('pe', lambda e: e.matmul(psE[:, 64:128], lhsT=padT[:], rhs=cst32[0:64, 128:192], start=True, stop=True), reads=['padT', 'cst32'], writes=['psE'])
                op('dve', lambda e: e.tensor_copy(out=padse[:], in_=psE[:, 0:128]), reads=['psE'], writes=['padse'])
                op('dve', lambda e: e.tensor_tensor(out=big[:, 0:NBLK * 64].rearrange("p (b x) -> p b x", x=64),
                                                    in0=padse[:, 64:128].unsqueeze(1).to_broadcast([128, NBLK, 64]),
                                                    in1=cst32[:, BLKS].unsqueeze(2).to_broadcast([128, NBLK, 64]), op=ALU.is_le),
                   reads=['padse', 'cst32'], writes=['big'])
                op('dve', lambda e: e.tensor_reduce(out=blke_f[:], in_=big[:, 0:NBLK * 64].rearrange("p (b x) -> p b x", x=64), axis=AX.X, op=ALU.add),
                   reads=['big'], writes=['blke_f'])
                op('dve', lambda e: e.tensor_scalar(out=blke_f[:], in0=blke_f[:], scalar1=63.0, scalar2=None, op0=ALU.min), reads=['blke_f'], writes=['blke_f'])
                op('dve', lambda e: e.tensor_scalar(out=blke_f[:], in0=blke_f[:], scalar1=128.0, scalar2=cst32[:, 960:961], op0=ALU.mult, op1=ALU.add),
                   reads=['blke_f', 'cst32'], writes=['blke_f'])
                op('dve', lambda e: e.tensor_scalar(out=blke_f[:], in0=blke_f[:], scalar1=float(l * 64 * 128), scalar2=None, op0=ALU.add), reads=['blke_f'], writes=['blke_f'])
                op('dve', lambda e: e.tensor_copy(out=widx_i[:], in_=blke_f[:]), reads=['blke_f'], writes=['widx_i'])
                op('dve', lambda e: e.tensor_tensor(out=big[:, 0:NT * 2 * 64].rearrange("p (b x) -> p b x", x=64),
                                                    in0=cst32[:, IOTA].unsqueeze(1).to_broadcast([128, NT * 2, 64]),
                                                    in1=ek[:, :].unsqueeze(2).to_broadcast([128, NT * 2, 64]), op=ALU.is_equal),
                   reads=['big', 'cst32'] + ['ek%d_%d' % (tt, k) for tt in range(NT) for k in range(2)], writes=['big'])
                op('dve', lambda e: e.tensor_tensor(out=big[:, 0:NT * 2 * 64].rearrange("p (b x) -> p b x", x=64),
                                                    in0=big[:, 0:NT * 2 * 64].rearrange("p (b x) -> p b x", x=64),
                                                    in1=padse[:, 0:64].unsqueeze(1).to_broadcast([128, NT * 2, 64]), op=ALU.mult),
                   reads=['big', 'padse'], writes=['big'])
                op('dve', lambda e: e.tensor_reduce(out=dest_f[:], in_=big[:, 0:NT * 2 * 64].rearrange("p (b x) -> p b x", x=64), axis=AX.X, op=ALU.add),
                   reads=['big'], writes=['dest_f'])
                op('dve', lambda e: e.tensor_tensor(out=dest_f[:], in0=dest_f[:], in1=rank[:], op=ALU.add),
                   reads=['dest_f'] + ['rank%d_%d' % (tt, k) for tt in range(NT) for k in range(2)], writes=['dest_f'])
                op('dve', lambda e: e.tensor_copy(out=dest_i[:], in_=dest_f[:]), reads=['dest_f'], writes=['dest_i'])
                for tt in range(NT):
                    for k in range(2):
                        op('pool', lambda e, tt=tt, k=k: e.indirect_dma_start(out=XB[:, :], out_offset=bass.IndirectOffsetOnAxis(ap=dest_i[:, tt * 2 + k:tt * 2 + k + 1], axis=0),
                                                                              in_=hn2all[:, tt, :], in_offset=None),
                           reads=['dest_i', 'hn2_%d' % tt], writes=['XBs%d_%d' % (tt, k)], dma=True)
                S.flush()
            if stop_after == 'p3':
                break

            with ExitStack() as es:
                def sb(name, shape, dt):
                    return es.enter_context(nc.sbuf_tensor(("L%d_p4_" % l) + name, shape, dt))

                def pst(name, shape, dt):
                    return es.enter_context(nc.psum_tensor(("L%d_p4_" % l) + name, shape, dt))
                NWB = 3
                w1b = [sb("w1b%d" % i, [128, 4096], BF16) for i in range(NWB)]
                w3b = [sb("w3b%d" % i, [128, 4096], BF16) for i in range(NWB)]
                w2b = [sb("w2b%d" % i, [128, 4096], BF16) for i in range(NWB)]
                xb = [sb("xb%d" % i, [128, 2, D], BF16) for i in range(2)]
                xT = [sb("xT%d" % i, [128, 8, MB], BF16) for i in range(2)]
                hT = [sb("hT%d" % i, [128, 4, MB], BF16) for i in range(2)]
                s1 = [sb("s1_%d" % i, [128, MB], F32) for i in range(2)]
                yst = [sb("yst%d" % i, [128, D], F32) for i in range(2)]
                psX = [pst("psX%d" % i, [128, D], BF16) for i in range(2)]
                psH = [pst("psH%d" % i, [128, 512], F32) for i in range(3)]
                psY = [pst("psY%d" % i, [128, 512], F32) for i in range(2)]
                w1v = w1.rearrange("l e (p kc) f -> (l e p) (kc f)", kc=8)
                w3v = w3.rearrange("l e (p kc) f -> (l e p) (kc f)", kc=8)
                w2v = w2.rearrange("l e (p fc) d -> (l e p) (fc d)", fc=4)
                ycnt = [0]

                def emit_loads(blk):
                    b = blk % 2
                    wb = blk % NWB
                    for (wv, wt, nm) in (((w1v, w1b[wb], 'w1b%d' % wb), (w3v, w3b[wb], 'w3b%d' % wb), (w2v, w2b[wb], 'w2b%d' % wb)) if not _DBG.get('nogather') else ()):
                        op('pool', lambda e, wv=wv, wt=wt, blk=blk: e.indirect_dma_start(out=wt[:, :], out_offset=None, in_=wv[:, :],
                                                                                         in_offset=bass.IndirectOffsetOnAxis(ap=widx_i[:, blk:blk + 1], axis=0)),
                           reads=['widx_i'], writes=[nm], dma=True)
                    op('sp', lambda e, b=b, blk=blk: e.dma_start(out=xb[b][:], in_=XB[blk * MB:(blk + 1) * MB, :].rearrange("(s p) d -> p s d", p=128)),
                       writes=['xb%d' % b], dma=True)

                def emit_T(blk):
                    b = blk % 2
                    for s_ in range(2):
                        for kc in range(8):
                            op('pe', lambda e, b=b, s_=s_, kc=kc: e.transpose(out=psX[s_][:, kc * 128:(kc + 1) * 128], in_=xb[b][:, s_, kc:D:8], identity=cstb[:, IDENT]),
                               reads=['xb%d' % b, 'cstb'], writes=['psX%d' % s_], sig=(kc == 7))
                        if s_ == 0:
                            f = lambda e, b=b, s_=s_: e.activation(out=xT[b][:, :, s_ * 128:(s_ + 1) * 128], in_=psX[s_][:, :].rearrange("p (kc t) -> p kc t", kc=8), func=AF.Copy)
                        else:
                            f = lambda e, b=b, s_=s_: e.tensor_copy(out=xT[b][:, :, s_ * 128:(s_ + 1) * 128], in_=psX[s_][:, :].rearrange("p (kc t) -> p kc t", kc=8))
                        op('act' if s_ == 0 else 'dve', f, reads=['psX%d' % s_], writes=['xT%d_%d' % (b, s_)])

                def emit_H(blk):
                    b = blk % 2
                    wb = blk % NWB
                    w1b3 = w1b[wb][:, :].rearrange("p (kc f) -> p kc f", kc=8)
                    w3b3 = w3b[wb][:, :].rearrange("p (kc f) -> p kc f", kc=8)
                    XK = ['xT%d_0' % b, 'xT%d_1' % b]
                    for fc in range(4):
                        hb = (blk * 4 + fc) % 3
                        for kc in range(8):
                            op('pe', lambda e, b=b, fc=fc, kc=kc, hb=hb, w1b3=w1b3: e.matmul(psH[hb][:, 0:MB], lhsT=w1b3[:, kc, fc:512:4], rhs=xT[b][:, kc, :], start=(kc == 0), stop=(kc == 7)),
                               sig=(kc == 7), reads=XK + ['w1b%d' % wb], writes=['psH%d' % hb])
                        for kc in range(8):
                            op('pe', lambda e, b=b, fc=fc, kc=kc, hb=hb, w3b3=w3b3: e.matmul(psH[hb][:, MB:2 * MB], lhsT=w3b3[:, kc, fc:512:4], rhs=xT[b][:, kc, :], start=(kc == 0), stop=(kc == 7)),
                               sig=(kc == 7), reads=XK + ['w3b%d' % wb], writes=['psH%d' % hb])
                        sb_ = (blk * 4 + fc) % 2
                        op('act', lambda e, hb=hb, sb_=sb_: e.activation(out=s1[sb_][:], in_=psH[hb][:, 0:MB], func=AF.Silu), reads=['psH%d' % hb], writes=['s1_%d' % sb_])
                        op('dve', lambda e, b=b, hb=hb, fc=fc, sb_=sb_: e.tensor_tensor(out=hT[b][:, fc, :], in0=s1[sb_][:], in1=psH[hb][:, MB:2 * MB], op=ALU.mult),
                           reads=['s1_%d' % sb_, 'psH%d' % hb], writes=['hT%d_%d' % (b, fc)])

                def emit_Y(blk):
                    b = blk % 2
                    wb = blk % NWB
                    w2b3 = w2b[wb][:, :].rearrange("p (fc d) -> p fc d", fc=4)
                    HKs = ['hT%d_%d' % (b, fc) for fc in range(4)]
                    for s_ in range(2):
                        for half in range(2):
                            yb_ = ycnt[0] % 2
                            ycnt[0] += 1
                            for fc in range(4):
                                op('pe', lambda e, b=b, s_=s_, half=half, fc=fc, yb_=yb_, w2b3=w2b3: e.matmul(psY[yb_][:, :], lhsT=hT[b][:, fc, s_ * 128:(s_ + 1) * 128],
                                                                                                               rhs=w2b3[:, fc, half * 512:(half + 1) * 512], start=(fc == 0), stop=(fc == 3)),
                                   sig=(fc == 3), reads=HKs + ['w2b%d' % wb], writes=['psY%d' % yb_])
                            if half == 0:
                                op('act', lambda e, s_=s_, yb_=yb_: e.activation(out=yst[s_][:, 0:512], in_=psY[yb_][:, :], func=AF.Copy), reads=['psY%d' % yb_], writes=['yst%d_0' % s_])
                            else:
                                op('dve', lambda e, s_=s_, yb_=yb_: e.tensor_copy(out=yst[s_][:, 512:1024], in_=psY[yb_][:, :]), reads=['psY%d' % yb_], writes=['yst%d_1' % s_])
                        op('sp', lambda e, s_=s_, blk=blk: e.dma_start(out=YB[blk * MB + s_ * 128: blk * MB + (s_ + 1) * 128, :], in_=yst[s_][:]),
                           reads=['yst%d_0' % s_, 'yst%d_1' % s_], writes=['YBrow'], dma=True)

                emit_loads(0)
                emit_T(0)
                emit_loads(1)
                for blk in range(NBLK):
                    emit_H(blk)
                    if blk + 1 < NBLK:
                        emit_T(blk + 1)
                    if blk + 2 < NBLK:
                        emit_loads(blk + 2)
                    emit_Y(blk)
                S.flush()
            if stop_after == 'p4':
                break

            last = (l == L - 1)
            with ExitStack() as es:
                def sb(name, shape, dt):
                    return es.enter_context(nc.sbuf_tensor(("L%d_p5_" % l) + name, shape, dt))
                g2rep = sb("g2rep", [128, 3, D], F32)
                gfin = sb("gfin", [128, D], F32)
                y0 = [sb("y0_%d" % i, [128, D], F32) for i in range(3)]
                y1 = [sb("y1_%d" % i, [128, D], F32) for i in range(3)]
                h5 = [sb("h5_%d" % i, [128, D], F32) for i in range(3)]
                junk = sb("junk", [128, D], BF16)
                st = [sb("st%d" % i, [128, 4], F32) for i in range(3)]
                for ms in range(3):
                    op('sp', lambda e, ms=ms: e.dma_start(out=g2rep[:, ms, :], in_=MS[l, ms:ms + 1, 5 * D:6 * D].to_broadcast([128, D])), writes=['g2rep'], dma=True)
                op('sp', lambda e: e.dma_start(out=gfin[:], in_=g_final[0:1, :].to_broadcast([128, D])), writes=['gfin'], dma=True)
                for tt in range(NT):
                    s_, j = divmod(tt, TPS)
                    if last and j < 2:
                        continue
                    b = tt % 3
                    r0 = tt * 128
                    ms = mset(tt)
                    op('pool', lambda e, b=b, tt=tt: e.indirect_dma_start(out=y0[b][:, :], out_offset=None, in_=YB[:, :],
                                                                          in_offset=bass.IndirectOffsetOnAxis(ap=dest_i[:, tt * 2:tt * 2 + 1], axis=0)),
                       writes=['y0_%d' % b], dma=True)
                    op('pool', lambda e, b=b, tt=tt: e.indirect_dma_start(out=y1[b][:, :], out_offset=None, in_=YB[:, :],
                                                                          in_offset=bass.IndirectOffsetOnAxis(ap=dest_i[:, tt * 2 + 1:tt * 2 + 2], axis=0)),
                       writes=['y1_%d' % b], dma=True)
                    op('sp', lambda e, b=b, r0=r0: e.dma_start(out=h5[b][:], in_=H[r0:r0 + 128, :]), writes=['h5_%d' % b], dma=True)
                    op('dve', lambda e, b=b, tt=tt: e.tensor_scalar(out=y0[b][:], in0=y0[b][:], scalar1=gate_f[:, tt * 2:tt * 2 + 1], scalar2=None, op0=ALU.mult),
                       reads=['y0_%d' % b], writes=['y0_%d' % b])
                    op('dve', lambda e, b=b, tt=tt: e.scalar_tensor_tensor(out=y0[b][:], in0=y1[b][:], scalar=gate_f[:, tt * 2 + 1:tt * 2 + 2], in1=y0[b][:], op0=ALU.mult, op1=ALU.add),
                       reads=['y0_%d' % b, 'y1_%d' % b], writes=['y0_%d' % b])
                    op('dve', lambda e, b=b, ms=ms: e.tensor_tensor(out=y0[b][:], in0=y0[b][:], in1=g2rep[:, ms, :], op=ALU.mult), reads=['y0_%d' % b, 'g2rep'], writes=['y0_%d' % b])
                    op('dve', lambda e, b=b: e.tensor_tensor(out=h5[b][:], in0=h5[b][:], in1=y0[b][:], op=ALU.add), reads=['y0_%d' % b, 'h5_%d' % b], writes=['h5_%d' % b])
                    if not last:
                        op('sp', lambda e, b=b, r0=r0: e.dma_start(out=H[r0:r0 + 128, :], in_=h5[b][:]), reads=['h5_%d' % b], writes=['Hrow%d' % tt], dma=True)
                    else:
                        op('dve', lambda e, b=b: e.memset(st[b][:, 0:1], 0.0), writes=['ss%d' % b])
                        op('act', lambda e, b=b: e.activation(out=junk[:], in_=h5[b][:], func=AF.Square, accum_out=st[b][:, 0:1]),
                           reads=['h5_%d' % b, 'ss%d' % b], writes=['junk', 'ss%d' % b])
                        rstd_from_ss(st[b][:, 0:1], st[b][:, 1:2], 'ss%d' % b, 'rs%d' % b, D)
                        op('dve', lambda e, b=b: e.tensor_scalar(out=h5[b][:], in0=h5[b][:], scalar1=st[b][:, 1:2], scalar2=None, op0=ALU.mult),
                           reads=['h5_%d' % b, 'rs%d' % b], writes=['h5_%d' % b])
                        op('pool', lambda e, b=b: e.tensor_tensor(out=h5[b][:], in0=h5[b][:], in1=gfin[:], op=ALU.mult), reads=['h5_%d' % b, 'gfin'], writes=['h5_%d' % b])
                        orow = s_ * SEQ + (j - 2) * 128
                        op('sp', lambda e, b=b, orow=orow: e.dma_start(out=out[orow:orow + 128, :], in_=h5[b][:]), reads=['h5_%d' % b], writes=['orow%d' % tt], dma=True)
                S.flush()
            if stop_after == 'p5':
                break
    return nc


def _prep_inputs(inputs):
    f32 = np.float32
    x, c, ctx, c_ctx = inputs['x'], inputs['c'], inputs['ctx'], inputs['c_ctx']
    j = np.arange(128)
    ident = np.eye(128, dtype=f32)
    tri = (j[:, None] <= j[None, :]).astype(f32)
    trit = (j[:, None] >= j[None, :]).astype(f32)
    su = (j[:, None] > j[None, :]).astype(f32)
    slw = (j[:, None] < j[None, :]).astype(f32)
    ones = np.ones((128, 128), f32)
    iota = np.broadcast_to(np.arange(64, dtype=f32)[None, :], (128, 64))
    blks = np.broadcast_to((np.arange(128, dtype=f32) * MB)[None, :], (128, 128))
    pidx = np.arange(128, dtype=f32)[:, None]
    pad = np.zeros((128, 1024 - 961), f32)
    cst = np.ascontiguousarray(np.concatenate([ident, tri, trit, su, slw, ones, iota, blks, pidx, pad], axis=1))
    shared = {
        'cst': cst,
        'w_mod': inputs['w_mod'],
        'b_mod3': np.ascontiguousarray(np.broadcast_to(inputs['b_mod'][:, None, :], (L, 3, 6 * D))),
        'gn1T': np.ascontiguousarray(inputs['g_norm1'].reshape(L, 8, 128).transpose(0, 2, 1)),
        'w_in': inputs['w_in'],
        'ln_v_g': inputs['ln_v_g'], 'ln_v_b': inputs['ln_v_b'],
        'w_spT': np.ascontiguousarray(inputs['w_sp'].transpose(0, 1, 3, 2)),
        'b_sp': np.ascontiguousarray(inputs['b_sp'].reshape(L, 512)),
        'w_gate_up': inputs['w_gate_up'], 'b_gate': inputs['b_gate'],
        'g_glaT': np.ascontiguousarray(inputs['g_gla'].reshape(L, 4, 128).transpose(0, 2, 1)),
        'w_out': inputs['w_out'], 'g_norm2': inputs['g_norm2'],
        'w_r': np.ascontiguousarray(np.concatenate([inputs['w_router_g'], inputs['w_router_e']], axis=2)),
        'b_r': np.ascontiguousarray(np.concatenate([inputs['b_router_g'], inputs['b_router_e']], axis=1)),
        'w1': inputs['w1'], 'w3': inputs['w3'], 'w2': inputs['w2'],
        'g_final': np.ascontiguousarray(inputs['g_final'].reshape(1, D)),
    }
    in_maps = []
    for core in range(NCORES):
        rows, cm = [], []
        for s in range(NS):
            b = core * NS + s
            rows.append(ctx[b])
            rows.append(x[b])
            cm.append(c[b])
        cm.append(c_ctx)
        cmod = np.ascontiguousarray(np.stack(cm, axis=0).reshape(3, 8, 128).transpose(2, 1, 0)).astype(f32)
        m = dict(shared)
        m['hin'] = np.ascontiguousarray(np.concatenate(rows, axis=0)).astype(f32)
        m['cmod'] = cmod
        in_maps.append(m)
    return in_maps


def kernel(**inputs):
    inputs = {k: np.asarray(v) for k, v in inputs.items()}
    in_maps = _prep_inputs(inputs)
    nc = build_program()
    res = run_bass_kernel_spmd(nc, in_maps, core_ids=list(range(NCORES)))
    outs = [r["out"].reshape(NS, SEQ, D) for r in res.results]
    return np.concatenate(outs, axis=0).astype(np.float32)
```
